# Optimizing a Trainium2 kernel written in Bass

```python
import jax, jax.numpy as jnp
from jax import lax
import numpy as np

D_MODEL = 1024
BATCH = 8
SEQ = 2048
DEPTH = 2

CHUNK = 64
Q_BLOCK = 128
N_A_LAYERS = (DEPTH + 1) // 2
N_B_LAYERS = DEPTH - N_A_LAYERS
A_HEADS = 16
A_HEAD_DIM = D_MODEL // A_HEADS
B_HEADS = 16
NOPE_DIM = 64
ROPE_DIM = 32
V_DIM = 64
Q_RANK = 768
KV_RANK = 256
ROPE_THETA = 10000.0
N_EXPERTS = 16
N_GROUPS = 4
EXPERTS_PER_GROUP = N_EXPERTS // N_GROUPS
GROUP_SCORE_TOP = 2
TOP_K = 2
D_EXPERT = 512
EPS = 1e-6

kernel_name = "fox_mla_yoco_grouped_moe_adaln"


def rmsnorm(x, g):
    xf = x.astype(jnp.float32)
    y = xf * lax.rsqrt(jnp.mean(xf * xf, axis=-1, keepdims=True) + EPS)
    return y.astype(x.dtype) * g


def modulate(h, shift, scale):
    return h * (1 + scale[:, None, :]) + shift[:, None, :]


def rope_tables(positions, dtype):
    half = ROPE_DIM // 2
    inv_freq = ROPE_THETA ** (-jnp.arange(half, dtype=jnp.float32) / half)
    ang = positions.astype(jnp.float32)[..., None] * inv_freq
    return jnp.cos(ang).astype(dtype), jnp.sin(ang).astype(dtype)


def apply_rope(x, cos, sin):
    x1, x2 = jnp.split(x, 2, axis=-1)
    return jnp.concatenate([x1 * cos - x2 * sin, x2 * cos + x1 * sin], axis=-1)


def block_sweep(attend, seq):
    return jnp.concatenate([attend(i * Q_BLOCK, (i + 1) * Q_BLOCK) for i in range(seq // Q_BLOCK)], axis=1)


def forgetting_attention(h, w_in, b_f, w_o):
    B, S, D = h.shape
    proj = h @ w_in
    q, k, v, f_logit = jnp.split(proj, [D, 2 * D, 3 * D], axis=-1)
    q = q.reshape(B, S, A_HEADS, A_HEAD_DIM)
    k = k.reshape(B, S, A_HEADS, A_HEAD_DIM)
    v = v.reshape(B, S, A_HEADS, A_HEAD_DIM)
    log_f = jax.nn.log_sigmoid((f_logit + b_f).astype(jnp.float32))
    cum = jnp.cumsum(log_f, axis=1).transpose(0, 2, 1)
    scale = A_HEAD_DIM ** -0.5
    pos = jnp.arange(S)

    def attend(t0, t1):
        s = jnp.einsum('bqhd,bkhd->bhqk', q[:, t0:t1], k[:, :t1], preferred_element_type=jnp.float32) * scale
        s = s + cum[:, :, t0:t1, None] - cum[:, :, None, :t1]
        allowed = pos[t0:t1, None] >= pos[None, :t1]
        p = jax.nn.softmax(jnp.where(allowed, s, -jnp.inf), axis=-1)
        return jnp.einsum('bhqk,bkhd->bqhd', p.astype(v.dtype), v[:, :t1])

    o = block_sweep(attend, S).reshape(B, S, D)
    return o @ w_o


def mla_shared_kv(x, c_act, norm_g, w_mod, b_mod, w_down, latent_g, w_up, cos, sin):
    B, S, _ = x.shape
    shift, scale = jnp.split(c_act @ w_mod + b_mod, 2, axis=-1)
    h = modulate(rmsnorm(x, norm_g), shift, scale)
    c_kv, k_rope = jnp.split(h @ w_down, [KV_RANK], axis=-1)
    c_kv = rmsnorm(c_kv, latent_g)
    kv = (c_kv @ w_up).reshape(B, S, B_HEADS, NOPE_DIM + V_DIM)
    k_nope, v = jnp.split(kv, [NOPE_DIM], axis=-1)
    k_rope = apply_rope(k_rope, cos, sin)
    return k_nope, k_rope, v


def latent_attention(h, k_nope, k_rope, v, w_dq, q_g, w_uq, w_o, cos, sin):
    B, S, _ = h.shape
    cq = rmsnorm(h @ w_dq, q_g)
    q = (cq @ w_uq).reshape(B, S, B_HEADS, NOPE_DIM + ROPE_DIM)
    q_nope, q_rope = jnp.split(q, [NOPE_DIM], axis=-1)
    q_rope = apply_rope(q_rope, cos[:, :, None, :], sin[:, :, None, :])
    scale = (NOPE_DIM + ROPE_DIM) ** -0.5
    chunk_id = jnp.arange(S) // CHUNK

    def attend(t0, t1):
        s = jnp.einsum('bqhd,bkhd->bhqk', q_nope[:, t0:t1], k_nope[:, :t1], preferred_element_type=jnp.float32)
        s = s + jnp.einsum('bqhr,bkr->bhqk', q_rope[:, t0:t1], k_rope[:, :t1], preferred_element_type=jnp.float32)
        allowed = chunk_id[t0:t1, None] >= chunk_id[None, :t1]
        p = jax.nn.softmax(jnp.where(allowed, s * scale, -jnp.inf), axis=-1)
        return jnp.einsum('bhqk,bkhd->bqhd', p.astype(v.dtype), v[:, :t1])

    o = block_sweep(attend, S).reshape(B, S, B_HEADS * V_DIM)
    return o @ w_o


def grouped_moe(h, router_w, router_bias, w_gate, w_up, w_down):
    B, S, D = h.shape
    hf = h.reshape(-1, D)
    n = hf.shape[0]
    scores = jax.nn.sigmoid(jnp.matmul(hf, router_w, preferred_element_type=jnp.float32))
    sel = (scores + router_bias.astype(jnp.float32)).reshape(n, N_GROUPS, EXPERTS_PER_GROUP)
    group_score = lax.top_k(sel, GROUP_SCORE_TOP)[0].sum(-1)
    g = jnp.argmax(group_score, axis=-1)
    g_idx = jnp.broadcast_to(g[:, None, None], (n, 1, EXPERTS_PER_GROUP))
    in_group = jnp.take_along_axis(sel, g_idx, axis=1)[:, 0]
    _, local = lax.top_k(in_group, TOP_K)
    idx = g[:, None] * EXPERTS_PER_GROUP + local
    w = jnp.take_along_axis(scores, idx, axis=1)
    w = w / jnp.sum(w, axis=-1, keepdims=True)
    combine = jnp.sum(jax.nn.one_hot(idx, N_EXPERTS, dtype=jnp.float32) * w[..., None], axis=1).astype(h.dtype)
    y = jnp.zeros_like(hf)
    for e in range(N_EXPERTS):
        a = jax.nn.silu(hf @ w_gate[e]) * (hf @ w_up[e])
        y = y + combine[:, e:e + 1] * (a @ w_down[e])
    return y.reshape(B, S, D)


def setup_inputs(seed: int = 0) -> dict:
    key = jax.random.key(seed)
    ks = iter(jax.random.split(key, 40))
    f32 = jnp.float32
    D = D_MODEL

    def w(shape, fan_in, gain=1.0):
        return gain * fan_in ** -0.5 * jax.random.normal(next(ks), shape, f32)

    def gain(shape):
        return 1.0 + 0.05 * jax.random.normal(next(ks), shape, f32)

    def small(shape, s):
        return s * jax.random.normal(next(ks), shape, f32)

    x = jax.random.normal(next(ks), (BATCH, SEQ, D), f32)
    c = jax.random.normal(next(ks), (BATCH, D), f32)
    offset = jax.random.randint(next(ks), (BATCH, 1), 0, 4096, dtype=jnp.int32)
    positions = (jnp.arange(SEQ, dtype=jnp.int32)[None, :] + offset).astype(jnp.int32)
    return {
        "x": x,
        "c": c,
        "positions": positions,
        "a_norm_g": gain((N_A_LAYERS, D)),
        "a_w_in": w((N_A_LAYERS, D, 3 * D + A_HEADS), D),
        "a_b_f": jax.random.uniform(next(ks), (N_A_LAYERS, A_HEADS), f32, 1.0, 5.0),
        "a_w_o": w((N_A_LAYERS, D, D), D),
        "kv_norm_g": gain((D,)),
        "kv_w_mod": w((D, 2 * D), D, 0.5),
        "kv_b_mod": small((2 * D,), 0.02),
        "kv_w_down": w((D, KV_RANK + ROPE_DIM), D),
        "kv_latent_g": gain((KV_RANK,)),
        "kv_w_up": w((KV_RANK, B_HEADS * (NOPE_DIM + V_DIM)), KV_RANK),
        "b_norm_g": gain((N_B_LAYERS, D)),
        "b_w_dq": w((N_B_LAYERS, D, Q_RANK), D),
        "b_q_norm_g": gain((N_B_LAYERS, Q_RANK)),
        "b_w_uq": w((N_B_LAYERS, Q_RANK, B_HEADS * (NOPE_DIM + ROPE_DIM)), Q_RANK),
        "b_w_o": w((N_B_LAYERS, B_HEADS * V_DIM, D), B_HEADS * V_DIM),
        "w_mod": w((DEPTH, D, 6 * D), D, 0.5),
        "b_mod": small((DEPTH, 6 * D), 0.02),
        "ffn_norm_g": gain((DEPTH, D)),
        "router_w": w((D, N_EXPERTS), D),
        "router_bias": small((N_EXPERTS,), 0.01),
        "exp_w_gate": w((DEPTH, N_EXPERTS, D, D_EXPERT), D),
        "exp_w_up": w((DEPTH, N_EXPERTS, D, D_EXPERT), D),
        "exp_w_down": w((DEPTH, N_EXPERTS, D_EXPERT, D), D_EXPERT),
        "final_norm_g": gain((D,)),
    }


def reference(x, c, positions, a_norm_g, a_w_in, a_b_f, a_w_o, kv_norm_g, kv_w_mod, kv_b_mod,
              kv_w_down, kv_latent_g, kv_w_up, b_norm_g, b_w_dq, b_q_norm_g, b_w_uq, b_w_o,
              w_mod, b_mod, ffn_norm_g, router_w, router_bias, exp_w_gate, exp_w_up, exp_w_down,
              final_norm_g):
    c_act = jax.nn.silu(c)
    cos, sin = rope_tables(positions, x.dtype)
    shared_kv = None
    for layer in range(DEPTH):
        sh1, sc1, g1, sh2, sc2, g2 = jnp.split(c_act @ w_mod[layer] + b_mod[layer], 6, axis=-1)
        if layer < N_A_LAYERS:
            h = modulate(rmsnorm(x, a_norm_g[layer]), sh1, sc1)
            mix = forgetting_attention(h, a_w_in[layer], a_b_f[layer], a_w_o[layer])
        else:
            j = layer - N_A_LAYERS
            h = modulate(rmsnorm(x, b_norm_g[j]), sh1, sc1)
            k_nope, k_rope, v = shared_kv
            mix = latent_attention(h, k_nope, k_rope, v, b_w_dq[j], b_q_norm_g[j], b_w_uq[j], b_w_o[j], cos, sin)
        x = x + g1[:, None, :] * mix
        h = modulate(rmsnorm(x, ffn_norm_g[layer]), sh2, sc2)
        x = x + g2[:, None, :] * grouped_moe(h, router_w, router_bias, exp_w_gate[layer], exp_w_up[layer], exp_w_down[layer])
        if layer == N_A_LAYERS - 1:
            shared_kv = mla_shared_kv(x, c_act, kv_norm_g, kv_w_mod, kv_b_mod, kv_w_down, kv_latent_g, kv_w_up, cos, sin)
    return rmsnorm(x, final_norm_g)
```

```python
import numpy as np
from contextlib import ExitStack
import concourse.bass as bass
import concourse.mybir as mybir
from concourse.bass_utils import run_bass_kernel_spmd

F32 = mybir.dt.float32
BF16 = mybir.dt.bfloat16
I32 = mybir.dt.int32
AF = mybir.ActivationFunctionType
ALU = mybir.AluOpType
AX = mybir.AxisListType

S = 2048
D = 1024
NT = 16
NB = 4
DC = 8
NE = 16
EPS = 1e-6
SAME_ENGINE_SYNC = True
STOP_AFTER = "final"


class _Op:
    __slots__ = ("eng", "idx", "fn", "deps", "need_sig", "sig_val", "dma", "dma_val")

    def __init__(self, eng, idx, fn, dma):
        self.eng = eng
        self.idx = idx
        self.fn = fn
        self.deps = []
        self.need_sig = False
        self.sig_val = 0
        self.dma = dma
        self.dma_val = 0


class Prog:
    ENGS = ("pe", "act", "dve", "pool", "sp")

    def __init__(self, nc):
        self.nc = nc
        self.ops = {e: [] for e in self.ENGS}
        self.last_w = {}
        self.readers = {}
        self.dma_cnt = {}

    def op(self, eng, fn, r=(), w=(), dma=None):
        o = _Op(eng, len(self.ops[eng]), fn, dma)
        deps = {}

        def add(d):
            if d.dma is not None:
                key = ("d", d.dma)
                if key not in deps or deps[key].dma_val < d.dma_val:
                    deps[key] = d
            else:
                if d.eng == eng and (eng == "pe" or not SAME_ENGINE_SYNC):
                    return
                key = ("c", d.eng)
                if key not in deps or deps[key].idx < d.idx:
                    deps[key] = d

        for k in r:
            d = self.last_w.get(k)
            if d is not None:
                add(d)
        for k in w:
            d = self.last_w.get(k)
            if d is not None:
                add(d)
            rd = self.readers.get(k)
            if rd:
                for d in rd.values():
                    add(d)
        for d in deps.values():
            if d.dma is None:
                d.need_sig = True
            o.deps.append(d)
        for k in w:
            self.last_w[k] = o
            self.readers[k] = {}
        for k in r:
            rd = self.readers.setdefault(k, {})
            if dma is not None:
                rd[("d", dma, o.idx)] = o
            else:
                rd[("c", eng)] = o
        if dma is not None:
            c = self.dma_cnt.get(dma, 0) + 1
            self.dma_cnt[dma] = c
            o.dma_val = 16 * c
        self.ops[eng].append(o)
        return o

    def emit(self, pool, final_waits=()):
        nc = self.nc
        for e in self.ENGS:
            c = 0
            for o in self.ops[e]:
                if o.dma is None and o.need_sig:
                    c += 1
                    o.sig_val = pool.ebase[e] + c
            pool.ebase[e] += c
        swk = set()
        for o in self.ops["pool"]:
            if o.dma is not None:
                swk.add(o.dma)
        slot = {}
        n_sw, n_hw = 0, 0
        for k in self.dma_cnt:
            if k in swk:
                slot[k] = n_sw
                n_sw += 1
            else:
                slot[k] = pool.n_sw + n_hw
                n_hw += 1
        assert n_sw <= pool.n_sw and n_hw <= len(pool.dsem) - pool.n_sw, (n_sw, n_hw)
        for e in self.ENGS:
            for o in self.ops[e]:
                if o.dma is not None:
                    o.dma_val += pool.dbase[slot[o.dma]]
        esem = pool.esem
        dsem = {k: pool.dsem[i] for k, i in slot.items()}
        all_final = {k: pool.dbase[slot[k]] + 16 * self.dma_cnt[k] for k in slot}
        for k, i in slot.items():
            pool.dbase[i] += 16 * self.dma_cnt[k]
        with nc.Block() as block:

            def run(e, engobj):
                waited = {}
                for o in self.ops[e]:
                    ws = []
                    for d in o.deps:
                        if d.dma is not None:
                            sem, val, sk = dsem[d.dma], d.dma_val, ("d", d.dma)
                        else:
                            sem, val, sk = esem[d.eng], d.sig_val, ("c", d.eng)
                        if waited.get(sk, 0) >= val:
                            continue
                        waited[sk] = val
                        ws.append((sem, val))
                    for sem, val in ws[:-1]:
                        engobj.wait_ge(sem, val)
                    ins = o.fn(engobj)
                    if ws:
                        ins._wait_ge(ws[-1][0], ws[-1][1])
                    if o.dma is not None:
                        ins.then_inc(dsem[o.dma], 16)
                    elif o.need_sig:
                        ins.then_inc(esem[e], 1)
                if e == "sp":
                    for k, v in all_final.items():
                        engobj.wait_ge(dsem[k], v)

            @block.tensor
            def _(pe):
                run("pe", pe)

            @block.scalar
            def _(act):
                run("act", act)

            @block.vector
            def _(dve):
                run("dve", dve)

            @block.gpsimd
            def _(pool_):
                run("pool", pool_)

            @block.sync
            def _(sp):
                run("sp", sp)


class SemPool:
    def __init__(self, nc, st, n_dma=54, n_sw=32):
        self.n_sw = n_sw
        self.esem = {e: st.enter_context(nc.semaphore("g_" + e)) for e in Prog.ENGS}
        self.ebase = {e: 0 for e in Prog.ENGS}
        self.dsem = [st.enter_context(nc.semaphore("gd%d" % i)) for i in range(n_dma)]
        self.dbase = [0] * n_dma


class H:
    def __init__(self, nc):
        self.nc = nc
        self.P = None
        self.pool = None

    def begin(self):
        self.P = Prog(self.nc)

    def end(self, final_waits=()):
        self.P.emit(self.pool, final_waits)
        self.P = None

    def mm(self, out, lhsT, rhs, start=True, stop=True, r=(), w=()):
        self.P.op("pe", lambda e: e.matmul(out, lhsT, rhs, start=start, stop=stop), r, w)

    def tr(self, out, in_, ident, r=(), w=()):
        self.P.op("pe", lambda e: e.transpose(out, in_, ident), r, w)

    def act(self, out, in_, func, bias=0.0, scale=1.0, r=(), w=()):
        self.P.op("act", lambda e: e.activation(out, in_, func, bias=bias, scale=scale), r, w)

    def tt(self, eng, out, in0, in1, op, r=(), w=()):
        self.P.op(eng, lambda e: e.tensor_tensor(out, in0, in1, op), r, w)

    def ts(self, eng, out, in0, s1, s2, op0, op1=None, r=(), w=()):
        if op1 is None:
            self.P.op(eng, lambda e: e.tensor_scalar(out, in0, s1, None, op0), r, w)
        else:
            self.P.op(eng, lambda e: e.tensor_scalar(out, in0, s1, s2, op0, op1), r, w)

    def stt(self, eng, out, in0, sc, in1, op0, op1, r=(), w=()):
        self.P.op(eng, lambda e: e.scalar_tensor_tensor(out, in0, sc, in1, op0, op1), r, w)

    def cp(self, eng, out, in_, r=(), w=()):
        if eng == "act":
            self.P.op("act", lambda e: e.copy(out, in_), r, w)
        else:
            self.P.op(eng, lambda e: e.tensor_copy(out, in_), r, w)

    def rcp(self, out, in_, r=(), w=()):
        self.P.op("dve", lambda e: e.reciprocal(out, in_), r, w)

    def red(self, out, in_, op, r=(), w=()):
        self.P.op("dve", lambda e: e.tensor_reduce(out, in_, AX.X, op), r, w)

    def memset(self, eng, ap, val, w=()):
        self.P.op(eng, lambda e: e.memset(ap, val), (), w)

    def dma(self, q, out, in_, r=(), w=(), sem=None):
        self.P.op(q, lambda e: e.dma_start(out=out, in_=in_), r, w, dma=sem)


def build_program(lo=0, hi=5):
    nc = bass.Bass("TRN2", target_bir_lowering=False)
    h = H(nc)

    def din(name, shape, dt=F32):
        return nc.dram_tensor(name, list(shape), dt, kind="ExternalInput").ap()

    xT_d = din("xT", [D, S])
    c_d = din("c_l", [128, DC])
    pos_d = din("pos", [1, S], I32)
    invf_d = din("invf", [128, 2])
    gvec_d = din("gvec", [128, 6 * DC])
    bmod_d = din("bmod", [128, 112])
    bf_d = din("bf_rep", [128, 256])
    rb_d = din("rb_rep", [128, 256])
    lg_d = din("lat_g", [128, 2])
    qg_d = din("q_g", [128, 6])
    w_mod_d = din("w_mod", [2, D, 6 * D])
    kv_w_mod_d = din("kv_w_mod", [D, 2 * D])
    a_w_in_d = din("a_w_in", [D, 3 * D + 16])
    a_w_o_d = din("a_w_o", [D, D])
    kv_w_down_d = din("kv_w_down", [D, 288])
    kv_w_up_d = din("kv_w_up", [256, 2048])
    b_w_dq_d = din("b_w_dq", [D, 768])
    b_w_uq_d = din("b_w_uq", [768, 1536])
    b_w_o_d = din("b_w_o", [D, D])
    router_w_d = din("router_w", [D, NE])
    wg_d = [din("exp_w_gate%d" % l_, [NE, D, 512]) for l_ in range(2)]
    wu_d = [din("exp_w_up%d" % l_, [NE, D, 512]) for l_ in range(2)]
    wd_d = [din("exp_w_down%d" % l_, [NE, 512, D]) for l_ in range(2)]
    outT_d = nc.dram_tensor("outT", [D, S], F32, kind="ExternalOutput").ap()

    with ExitStack() as G:
        _uid = [0]

        def sb(name, shape, dt, st=G):
            _uid[0] += 1
            return st.enter_context(nc.sbuf_tensor("s%d_%s" % (_uid[0], name), list(shape), dt))

        pb = [G.enter_context(nc.psum_tensor("pb%d" % i, [128, 512], F32)) for i in range(8)]
        h.pool = SemPool(nc, G)

        def PK(i):
            return ("ps", i)

        xT = sb("xT", [128, DC, S], F32)
        hT = sb("hT", [128, DC, S], BF16)
        ident = sb("ident", [128, 128], F32)
        ones_f = sb("ones_f", [128, 128], F32)
        ones_b = sb("ones_b", [128, 128], BF16)
        triu_f = sb("triu_f", [128, 128], F32)
        triu_b = sb("triu_b", [128, 128], BF16)
        cmask_b = sb("cmask_b", [128, 128], BF16)
        epst = sb("epst", [128, 1], F32)
        mod0 = sb("mod0", [128, 48], F32)
        mod1 = sb("mod1", [128, 48], F32)
        modkv = sb("modkv", [128, 16], F32)
        gvec = sb("gvec_s", [128, 6 * DC], F32)
        bmod = sb("bmod_s", [128, 112], F32)
        bf_rep = sb("bf_rep_s", [128, 256], F32)
        rb_rep = sb("rb_rep_s", [128, 256], F32)
        lat_g = sb("lat_g_s", [128, 2], F32)
        q_g = sb("q_g_s", [128, 6], F32)
        csT = sb("csT", [128, S], BF16)
        ckvT = sb("ckvT", [128, 2, S], BF16)
        krope = sb("krope", [128, S], BF16)
        ncoef = sb("ncoef", [128, 2 * DC], F32)
        cact = sb("cact", [128, DC], BF16)

        def tbs(tb):
            return slice(tb * 512, (tb + 1) * 512)

        def norm_phase(st, gidx, modt, sh0, sc0, nchunks=DC, src=None, dst=None, dst_keyf=None,
                       router=None):
            sq = [sb("sq%d" % i, [128, 512], BF16, st) for i in range(3)]
            rs = [sb("rs%d" % i, [128, 512], F32, st) for i in range(2)]
            t2 = [sb("t2_%d" % i, [128, 512], F32, st) for i in range(3)]
            h.stt("dve", ncoef[:, 0:DC], modt[:, sc0:sc0 + DC], 1.0, gvec[:, gidx * DC:(gidx + 1) * DC],
                  ALU.add, ALU.mult, r=["mod", "gvec"], w=["ncoef"])
            n = 0
            for tb in range(NB):
                pss = pb[tb % 2]
                for dc in range(DC):
                    q = sq[n % 3]
                    n += 1
                    h.act(q[:], xT[:, dc, tbs(tb)], AF.Square, r=[("xT", dc, tb)], w=[("sq", id(q))])
                    h.mm(pss[:, :], ones_b[:, :], q[:], start=(dc == 0), stop=(dc == DC - 1),
                         r=[("sq", id(q)), "ones_b"], w=[PK(tb % 2)])
                r_ = rs[tb % 2]
                h.act(r_[:], pss[:, :], AF.Ln, bias=epst[:, 0:1], scale=1.0 / D, r=[PK(tb % 2), "epst"], w=[("rs", tb % 2)])
                h.act(r_[:], r_[:], AF.Exp, scale=-0.5, r=[("rs", tb % 2)], w=[("rs", tb % 2)])
                for dc in range(DC):
                    t = t2[n % 3]
                    n += 1
                    h.tt("dve", t[:], xT[:, dc, tbs(tb)], r_[:], ALU.mult, r=[("xT", dc, tb), ("rs", tb % 2)], w=[("t2", id(t))])
                    h.act(hT[:, dc, tbs(tb)], t[:], AF.Identity, bias=modt[:, sh0 + dc:sh0 + dc + 1],
                          scale=ncoef[:, dc:dc + 1], r=[("t2", id(t)), "ncoef", "mod"], w=[("hT", tb)])
                    if router is not None:
                        router(tb, dc, t)

        with ExitStack() as st:
            cs = sb("c_s", [128, DC], F32, st)
            wm = [sb("wm%d" % i, [128, 6 * D], BF16, st) for i in range(3)]
            posi = sb("posi", [128, S], I32, st)
            invf = sb("invf_s", [128, 2], F32, st)
            sinT = sb("sinT", [128, S], BF16, st)
            cosT = sb("cosT", [128, S], BF16, st)
            ang = sb("ang", [128, S], F32, st)
            ta = sb("ta", [128, S], F32, st)
            tb_ = sb("tb_", [128, S], F32, st)
            ti = sb("ti", [128, S], I32, st)
            h.begin()
            h.dma("sp", cs[:], c_d[:, :], w=["cs"], sem="cs")
            h.dma("sp", gvec[:], gvec_d[:, :], w=["gvec"], sem="ld_gvec")
            h.dma("sp", bmod[:], bmod_d[:, :], w=["bmod"], sem="ld_bmod")
            h.dma("sp", bf_rep[:], bf_d[:, :], w=["bf_rep"], sem="ld_bf_rep")
            h.dma("sp", rb_rep[:], rb_d[:, :], w=["rb_rep"], sem="ld_rb_rep")
            h.dma("sp", lat_g[:], lg_d[:, :], w=["lat_g"], sem="ld_lat_g")
            h.dma("sp", q_g[:], qg_d[:, :], w=["q_g"], sem="ld_q_g")
            h.dma("sp", invf[:], invf_d[:, :], w=["invf"], sem="ld_invf")
            h.dma("sp", posi[:], pos_d.partition_broadcast(128), w=["posi"], sem="ld_posi")
            h.memset("pool", ones_f[:], 1.0, w=["ones_f"])
            h.memset("pool", ones_b[:], 1.0, w=["ones_b"])
            h.memset("pool", epst[:], EPS, w=["epst"])
            h.P.op("pool", lambda e: e.affine_select(out=ident[:], in_=ones_f[:], pattern=[[-1, 128]], compare_op=ALU.is_equal,
                                                     fill=0.0, base=0, channel_multiplier=1), ["ones_f"], ["ident"])
            h.P.op("pool", lambda e: e.affine_select(out=triu_f[:], in_=ones_f[:], pattern=[[1, 128]], compare_op=ALU.is_ge,
                                                     fill=0.0, base=0, channel_multiplier=-1), ["ones_f"], ["triu_f"])
            h.cp("pool", triu_b[:], triu_f[:], r=["triu_f"], w=["triu_b"])
            h.memset("pool", cmask_b[:], 1.0, w=["cmask_b"])
            h.memset("pool", cmask_b[64:128, 0:64], 0.0, w=["cmask_b"])
            h.memset("pool", krope[:], 0.0, w=["krope"])
            for dc in range(DC):
                h.dma("sp", xT[:, dc, :], xT_d[dc * 128:(dc + 1) * 128, :], w=[("xT", dc, t) for t in range(NB)], sem=("xload", dc))
            h.act(cact[:], cs[:], AF.Silu, r=["cs"], w=["cact"])
            wi = 0
            h.cp("dve", mod1[:, :], bmod[:, 48:96], r=["bmod"], w=["mod1acc"])
            h.cp("dve", modkv[:, :], bmod[:, 96:112], r=["bmod"], w=["modkvacc"])
            for (wsrc, ncol, modt, boff) in ((w_mod_d[0], 48, mod0, 0),):
                psm = pb[2 + (wi % 2)]
                for kc in range(DC):
                    wt = wm[wi % 3]
                    wkey = ("wm", wi % 3)
                    wi += 1
                    h.dma("pool", wt[:, 0:ncol * 128], wsrc[kc * 128:(kc + 1) * 128, :], w=[wkey], sem=wkey)
                    for j in range(ncol):
                        h.mm(psm[:, kc * ncol + j:kc * ncol + j + 1], wt[:, j * 128:(j + 1) * 128], cact[:, kc:kc + 1],
                             r=[wkey, "cact"], w=[("psm", id(psm))])
                h.red(modt[:, 0:ncol], psm[:, 0:DC * ncol].rearrange("p (k j) -> p j k", k=DC), ALU.add, r=[("psm", id(psm))], w=["mod"])
                h.tt("dve", modt[:, 0:ncol], modt[:, 0:ncol], bmod[:, boff:boff + ncol], ALU.add, r=["mod", "bmod"], w=["mod"])
            h.cp("dve", ang[:], posi[:], r=["posi"], w=["ang"])
            h.ts("dve", ang[:], ang[:], invf[:, 0:1], None, ALU.mult, r=["ang", "invf"], w=["ang"])
            for (dst, shift) in ((sinT, 0.0), (cosT, float(np.pi / 2))):
                if shift != 0.0:
                    h.ts("dve", ang[:], ang[:], shift, None, ALU.add, r=["ang"], w=["ang"])
                h.ts("dve", ta[:], ang[:], float(1.0 / (2 * np.pi)), None, ALU.mult, r=["ang"], w=["ta"])
                h.cp("dve", ti[:], ta[:], r=["ta"], w=["ti"])
                h.cp("dve", ta[:], ti[:], r=["ti"], w=["ta"])
                h.stt("dve", tb_[:], ta[:], float(-2 * np.pi), ang[:], ALU.mult, ALU.add, r=["ta", "ang"], w=["tb_"])
                h.ts("dve", ta[:], tb_[:], float(np.pi), float(-2 * np.pi), ALU.is_gt, ALU.mult, r=["tb_"], w=["ta"])
                h.tt("dve", tb_[:], tb_[:], ta[:], ALU.add, r=["tb_", "ta"], w=["tb_"])
                h.ts("dve", ta[:], tb_[:], float(-np.pi), float(2 * np.pi), ALU.is_lt, ALU.mult, r=["tb_"], w=["ta"])
                h.tt("dve", tb_[:], tb_[:], ta[:], ALU.add, r=["tb_", "ta"], w=["tb_"])
                h.act(dst[:], tb_[:], AF.Sin, r=["tb_"], w=[("trig", id(dst))])
            h.memset("pool", csT[0:64, :], 0.0, w=["cs_lo"])
            h.cp("pool", csT[64:96, :], cosT[64:96, :], r=[("trig", id(cosT))], w=["rope_tab"])
            h.ts("dve", csT[96:128, :], sinT[96:128, :], invf[96:128, 1:2], None, ALU.mult, r=[("trig", id(sinT)), "invf"], w=["rope_tab2"])
            h.end()

        def attn_phase(layer):
            fox = layer == 0
            modt = mod0 if fox else mod1
            with ExitStack() as st0:
                h.begin()
                norm_phase(st0, 0 if fox else 3, modt, 0, DC)
                h.end()
            if not fox:
                with ExitStack() as st1:
                    wdq = sb("wdq", [128, DC, 768], BF16, st1)
                    craw = sb("craw", [128, 6, 512], F32, st1)
                    csq = [sb("csq%d" % i, [128, 512], BF16, st1) for i in range(2)]
                    crs = sb("crs", [128, 512], F32, st1)
                    h.begin()
                    h.dma("pool", wdq[:], b_w_dq_d[:, :].rearrange("(dc p) c -> p dc c", p=128), w=["wdq"], sem="wdq")
                    for tb in range(NB):
                        for c in range(6):
                            bk = 2 + c % 2
                            for dc in range(DC):
                                h.mm(pb[bk][:, :], wdq[:, dc, c * 128:(c + 1) * 128], hT[:, dc, tbs(tb)],
                                     start=(dc == 0), stop=(dc == DC - 1), r=["wdq", ("hT", tb)], w=[PK(bk)])
                            h.cp("act", craw[:, c, :], pb[bk][:, :], r=[PK(bk)], w=[("craw", c)])
                            q_ = csq[c % 2]
                            h.tt("dve", q_[:], craw[:, c, :], craw[:, c, :], ALU.mult, r=[("craw", c)], w=[("csq", c % 2)])
                            h.mm(pb[4][:, :], ones_b[:, :], q_[:], start=(c == 0), stop=(c == 5), r=[("csq", c % 2), "ones_b"], w=[PK(4)])
                        h.act(crs[:], pb[4][:, :], AF.Ln, bias=epst[:, 0:1], scale=1.0 / 768, r=[PK(4), "epst"], w=["crs"])
                        h.act(crs[:], crs[:], AF.Exp, scale=-0.5, r=["crs"], w=["crs"])
                        for c in range(6):
                            h.stt("dve", hT[:, c, tbs(tb)], craw[:, c, :], q_g[:, c:c + 1], crs[:], ALU.mult, ALU.mult,
                                  r=[("craw", c), "crs", "q_g"], w=[("hT", tb)])
                    h.end()
            with ExitStack() as st:
                h.begin()
                cqT = hT
                NSET = 2
                qh = [[sb("qh%d_%d" % (s_, i), [128, S], BF16, st) for i in range(2)] for s_ in range(NSET)]
                kh = [[sb("kh%d_%d" % (s_, i), [128, S], BF16, st) for i in range(2)] for s_ in range(NSET)]
                Vp = [sb("Vp%d" % s_, [128, NT, 192], BF16, st) for s_ in range(NSET)]
                oT = [sb("oT%d" % i, [128, S], BF16, st) for i in range(2)]
                wo = [sb("wo%d" % i, [128, D], BF16, st) for i in range(3)]
                PT = [sb("PT%d" % i, [128, 512], BF16, st) for i in range(6)]
                rec = [sb("rec%d" % i, [128, 512], F32, st) for i in range(2)]
                zero_bias = sb("zero_bias", [128, 1], F32, st)
                h.memset("pool", zero_bias[:], 0.0, w=["zero_bias"])
                for s_ in range(NSET):
                    h.memset("pool", Vp[s_][:, :, 64:128], 1.0, w=[("Vp", s_)])
                    for i in range(2):
                        h.memset("pool", qh[s_][i][:], 0.0, w=[("qh", s_, i)])
                        if fox:
                            h.memset("pool", kh[s_][i][:], 0.0, w=[("kh", s_, i)])
                            h.memset("pool", kh[s_][i][64:65, :], 1.0, w=[("kh", s_, i)])
                        else:
                            h.cp("pool", kh[s_][i][:], krope[:], r=["krope"], w=[("kh", s_, i)])
                if fox:
                    wpair = [sb("wpair%d" % i, [128, DC, 384], BF16, st) for i in range(2)]
                    wf = sb("wf", [128, DC, 16], BF16, st)
                    lf = sb("lf", [128, 256], F32, st)
                    tot = sb("tot", [128, 256], F32, st)
                    off = sb("off", [128, 256], F32, st)
                    negcum = sb("negcum", [128, 256], F32, st)
                    cum8 = sb("cum8", [128, 256], F32, st)
                    cumT = sb("cumT", [16, S], BF16, st)
                    h.dma("pool", wf[:], a_w_in_d[:, 3 * D:3 * D + 16].rearrange("(dc p) c -> p dc c", p=128), w=["wf"], sem="wf")
                    for i in range(NT):
                        for dc in range(DC):
                            h.mm(pb[2][:, i * 16:(i + 1) * 16], hT[:, dc, i * 128:(i + 1) * 128], wf[:, dc, :],
                                 start=(dc == 0), stop=(dc == DC - 1), r=[("hT", i // 4), "wf"], w=[PK(2)])
                    h.tt("dve", lf[:], pb[2][:, 0:256], bf_rep[:], ALU.add, r=[PK(2), "bf_rep"], w=["lf"])
                    h.act(lf[:], lf[:], AF.Sigmoid, r=["lf"], w=["lf"])
                    h.act(lf[:], lf[:], AF.Ln, r=["lf"], w=["lf"])
                    h.mm(pb[3][:, 0:256], triu_f[:], lf[:], r=["triu_f", "lf"], w=[PK(3)])
                    h.mm(pb[2][:, 0:256], ones_f[:], lf[:], r=["ones_f", "lf"], w=[PK(2)])
                    h.cp("dve", tot[:], pb[2][:, 0:256], r=[PK(2)], w=["tot"])
                    h.memset("dve", off[:, 0:16], 0.0, w=["off"])
                    for i in range(1, NT):
                        h.tt("dve", off[:, i * 16:(i + 1) * 16], off[:, (i - 1) * 16:i * 16], tot[:, (i - 1) * 16:i * 16], ALU.add,
                             r=["off", "tot"], w=["off"])
                    h.tt("dve", cum8[:], pb[3][:, 0:256], off[:], ALU.add, r=[PK(3), "off"], w=["cum8"])
                    h.ts("dve", negcum[:], cum8[:], -1.0, None, ALU.mult, r=["cum8"], w=["negcum"])
                    h.ts("dve", cum8[:], cum8[:], 8.0, None, ALU.mult, r=["cum8"], w=["cum8"])
                    for i in range(NT):
                        bk = 4 + (i // 4) % 2
                        h.tr(pb[bk][0:16, (i % 4) * 128:(i % 4 + 1) * 128], cum8[:, i * 16:(i + 1) * 16], ident[:],
                             r=["cum8", "ident"], w=[PK(bk)])
                        if i % 4 == 3:
                            h.cp("act", cumT[:, (i // 4) * 512:(i // 4 + 1) * 512], pb[bk][0:16, :], r=[PK(bk)], w=["cumT"])
                else:
                    wq = [sb("wq%d" % i, [128, 6, 256], BF16, st) for i in range(2)]
                    wkv = [sb("wkv%d" % i, [128, 2, 256], BF16, st) for i in range(2)]

                g1c = 2 * DC
                w_o_d = a_w_o_d if fox else b_w_o_d
                scale = 0.125 if fox else float(96 ** -0.5)
                mask = triu_b if fox else cmask_b
                Kc = 65 if fox else 128
                cnt = {"ps_s": 0, "pt": 0, "ps_o": 0, "rec": 0, "op": 0}
                NPT = 6

                bgc = {"bk": 0}

                def bgbank():
                    bgc["bk"] += 1
                    return 5 + bgc["bk"] % 3

                def issue_weights(p):
                    wb = p % 2
                    h.dma("pool", wo[p % 3][:], w_o_d[p * 128:(p + 1) * 128, :], w=[("wo", p % 3)], sem=("wo", p % 3))
                    if fox:
                        for part in range(3):
                            c0 = part * D + p * 128
                            h.dma("pool", wpair[wb][:, :, part * 128:(part + 1) * 128],
                                  a_w_in_d[:, c0:c0 + 128].rearrange("(dc p) c -> p dc c", p=128), w=[("wpair", wb, part)], sem=("wpair", wb, part))
                    else:
                        for hh in range(2):
                            hd = 2 * p + hh
                            uq = b_w_uq_d[:, hd * 96:(hd + 1) * 96].rearrange("(kc p) c -> p kc c", p=128)
                            h.dma("pool", wq[wb][:, :, hh * 128:hh * 128 + 64], uq[:, :, 0:64], w=[("wq", wb, hh, 0)], sem=("wq", wb, hh, 0))
                            h.dma("pool", wq[wb][:, :, hh * 128 + 64:hh * 128 + 96], uq[:, :, 64:96], w=[("wq", wb, hh, 1)], sem=("wq", wb, hh, 1))
                            h.dma("pool", wq[wb][:, :, hh * 128 + 96:hh * 128 + 112], uq[:, :, 80:96], w=[("wq", wb, hh, 2)], sem=("wq", wb, hh, 2))
                            h.dma("pool", wq[wb][:, :, hh * 128 + 112:hh * 128 + 128], uq[:, :, 64:80], w=[("wq", wb, hh, 3)], sem=("wq", wb, hh, 3))
                            up = kv_w_up_d[:, hd * 128:(hd + 1) * 128].rearrange("(c p) n -> p c n", p=128)
                            h.dma("pool", wkv[wb][:, :, hh * 64:(hh + 1) * 64], up[:, :, 0:64], w=[("wkv", wb, hh, 0)], sem=("wkv", wb, hh, 0))
                            h.dma("pool", wkv[wb][:, :, 128 + hh * 64:128 + (hh + 1) * 64], up[:, :, 64:128], w=[("wkv", wb, hh, 1)], sem=("wkv", wb, hh, 1))

                def proj_items(p):
                    s_ = p % NSET
                    wb = p % 2
                    items = []

                    def v_item(g):
                        def f():
                            bk = bgbank()
                            for i in range(4 * g, 4 * g + 4):
                                if fox:
                                    for dc in range(DC):
                                        h.mm(pb[bk][:, (i % 4) * 128:(i % 4 + 1) * 128], hT[:, dc, i * 128:(i + 1) * 128], wpair[wb][:, dc, 256:384],
                                             start=(dc == 0), stop=(dc == DC - 1), r=[("wpair", wb, 2), ("hT", i // 4)], w=[PK(bk)])
                                else:
                                    for c in range(2):
                                        h.mm(pb[bk][:, (i % 4) * 128:(i % 4 + 1) * 128], ckvT[:, c, i * 128:(i + 1) * 128], wkv[wb][:, c, 128:256],
                                             start=(c == 0), stop=(c == 1), r=[("wkv", wb, 0, 1), ("wkv", wb, 1, 1), "ckvT"], w=[PK(bk)])
                            pv = pb[bk][:, :].rearrange("p (i c) -> p i c", c=128)
                            h.cp("act", Vp[s_][:, 4 * g:4 * g + 4, 0:64], pv[:, :, 0:64], r=[PK(bk)], w=[("Vp", s_)])
                            h.cp("dve", Vp[s_][:, 4 * g:4 * g + 4, 128:192], pv[:, :, 64:128], r=[PK(bk)], w=[("Vp", s_)])
                        return f

                    if fox:
                        def qk_item(which, tb):
                            def f():
                                nm = "qh" if which == 0 else "kh"
                                dsts = qh[s_] if which == 0 else kh[s_]
                                bk = bgbank()
                                for dc in range(DC):
                                    h.mm(pb[bk][:, :], wpair[wb][:, dc, which * 128:(which + 1) * 128], hT[:, dc, tbs(tb)],
                                         start=(dc == 0), stop=(dc == DC - 1), r=[("wpair", wb, which), ("hT", tb)], w=[PK(bk)])
                                h.cp("act", dsts[0][0:64, tbs(tb)], pb[bk][0:64, :], r=[PK(bk)], w=[(nm, s_, 0)])
                                h.cp("dve", dsts[1][0:64, tbs(tb)], pb[bk][64:128, :], r=[PK(bk)], w=[(nm, s_, 1)])
                            return f

                        def aug_item():
                            for hh in range(2):
                                h.dma("sp", qh[s_][hh][64:65, :], cumT[2 * p + hh:2 * p + hh + 1, :], r=["cumT"], w=[("qh", s_, hh)], sem=("aug", s_, hh))
                        items.append(aug_item)
                        for which in range(2):
                            for tb in range(NB):
                                items.append(qk_item(which, tb))
                    else:
                        def q_item(hh, tb):
                            def f():
                                bk = bgbank()
                                for kc in range(6):
                                    h.mm(pb[bk][:, :], wq[wb][:, kc, hh * 128:(hh + 1) * 128], cqT[:, kc, tbs(tb)],
                                         start=(kc == 0), stop=(kc == 5),
                                         r=[("wq", wb, hh, 0), ("wq", wb, hh, 1), ("wq", wb, hh, 2), ("wq", wb, hh, 3), ("hT", tb)], w=[PK(bk)])
                                h.cp("act", qh[s_][hh][0:64, tbs(tb)], pb[bk][0:64, :], r=[PK(bk)], w=[("qh", s_, hh)])
                                h.tt("dve", qh[s_][hh][64:128, tbs(tb)], pb[bk][64:128, :], csT[64:128, tbs(tb)], ALU.mult,
                                     r=[PK(bk), "rope_tab", "rope_tab2"], w=[("qh", s_, hh)])
                            return f

                        def k_item(tb):
                            def f():
                                bk = bgbank()
                                for c in range(2):
                                    h.mm(pb[bk][:, :], wkv[wb][:, c, 0:128], ckvT[:, c, tbs(tb)], start=(c == 0), stop=(c == 1),
                                         r=[("wkv", wb, 0, 0), ("wkv", wb, 1, 0), "ckvT"], w=[PK(bk)])
                                h.cp("act", kh[s_][0][0:64, tbs(tb)], pb[bk][0:64, :], r=[PK(bk)], w=[("kh", s_, 0)])
                                h.cp("dve", kh[s_][1][0:64, tbs(tb)], pb[bk][64:128, :], r=[PK(bk)], w=[("kh", s_, 1)])
                            return f
                        for hh in range(2):
                            for tb in range(NB):
                                items.append(q_item(hh, tb))
                        for tb in range(NB):
                            items.append(k_item(tb))
                    for g in range(4):
                        items.append(v_item(g))
                    return items

                def outproj_items(p):
                    wb = p % 3
                    ob = p % 2
                    items = []

                    def o_item(tb, dcol):
                        def f():
                            bo = bgbank()
                            h.mm(pb[bo][:, :], wo[wb][:, dcol * 128:(dcol + 1) * 128], oT[ob][:, tbs(tb)],
                                 r=[("wo", wb), ("oT", ob, tb)], w=[PK(bo)])
                            h.stt("dve", xT[:, dcol, tbs(tb)], pb[bo][:, :], modt[:, g1c + dcol:g1c + dcol + 1], xT[:, dcol, tbs(tb)],
                                  ALU.mult, ALU.add, r=[PK(bo), "mod", ("xT", dcol, tb)], w=[("xT", dcol, tb)])
                        return f
                    for tb in range(NB):
                        for dcol in range(DC):
                            items.append(o_item(tb, dcol))
                    return items

                def attn_steps(p):
                    s_ = p % NSET
                    ob = p % 2
                    steps = []
                    for hh in range(2):
                        for qb in range(NB):
                            nkt = 4 * qb + 4
                            ob_k = 3 + cnt["ps_o"] % 2
                            cnt["ps_o"] += 1
                            for kt in range(nkt):
                                steps.append((hh, qb, kt, nkt, ob_k, cnt["ps_s"] % 3, cnt["pt"] % NPT))
                                cnt["ps_s"] += 1
                                cnt["pt"] += 1

                    def emit_S(stp):
                        hh, qb, kt, nkt, ob_k, sk, pk = stp
                        hd = 2 * p + hh
                        qt, kt_ = qh[s_][hh], kh[s_][hh]
                        jj = kt - 4 * qb
                        n0 = max(0, jj) * 128
                        h.mm(pb[sk][:, n0:512], kt_[0:Kc, kt * 128:(kt + 1) * 128], qt[0:Kc, qb * 512 + n0:(qb + 1) * 512],
                             r=[("kh", s_, hh), ("qh", s_, hh)], w=[PK(sk)])
                        if fox:
                            bias = negcum[:, kt * 16 + hd:kt * 16 + hd + 1]
                            rk = [PK(sk), "negcum"]
                        else:
                            bias = zero_bias[:, 0:1]
                            rk = [PK(sk), "zero_bias"]
                        h.act(PT[pk][:, n0:512], pb[sk][:, n0:512], AF.Exp, bias=bias, scale=scale, r=rk, w=[("PT", pk)])
                        if jj >= 0:
                            h.tt("pool", PT[pk][:, n0:n0 + 128], PT[pk][:, n0:n0 + 128], mask[:], ALU.mult,
                                 r=[("PT", pk), "mask"], w=[("PT", pk)])

                    def emit_PV(stp):
                        hh, qb, kt, nkt, ob_k, sk, pk = stp
                        jj = kt - 4 * qb
                        n0 = max(0, jj) * 128
                        vl = Vp[s_][:, kt, 0:128] if hh == 0 else Vp[s_][:, kt, 64:192]
                        h.mm(pb[ob_k][:, n0:512], vl, PT[pk][:, n0:512], start=(kt == 0), stop=(kt == nkt - 1),
                             r=[("Vp", s_), ("PT", pk)], w=[PK(ob_k)])
                        if kt == nkt - 1:
                            rc = cnt["rec"] % 2
                            cnt["rec"] += 1
                            if hh == 0:
                                h.act(rec[rc][0:64, :], pb[ob_k][64:128, :], AF.Ln, r=[PK(ob_k)], w=[("rec", rc)])
                                h.act(rec[rc][0:64, :], rec[rc][0:64, :], AF.Exp, scale=-1.0, r=[("rec", rc)], w=[("rec", rc)])
                                h.tt("dve", oT[ob][0:64, tbs(qb)], pb[ob_k][0:64, :], rec[rc][0:64, :], ALU.mult,
                                     r=[PK(ob_k), ("rec", rc)], w=[("oT", ob, qb)])
                            else:
                                h.act(rec[rc][64:128, :], pb[ob_k][0:64, :], AF.Ln, r=[PK(ob_k)], w=[("rec", rc)])
                                h.act(rec[rc][64:128, :], rec[rc][64:128, :], AF.Exp, scale=-1.0, r=[("rec", rc)], w=[("rec", rc)])
                                h.tt("dve", oT[ob][64:128, tbs(qb)], pb[ob_k][64:128, :], rec[rc][64:128, :], ALU.mult,
                                     r=[PK(ob_k), ("rec", rc)], w=[("oT", ob, qb)])
                    return steps, emit_S, emit_PV

                LOOK = 2
                issue_weights(0)
                for it in proj_items(0):
                    it()
                bg = []
                for p in range(8):
                    if p + 1 < 8:
                        issue_weights(p + 1)
                        bg = bg + proj_items(p + 1)
                    steps, emit_S, emit_PV = attn_steps(p)
                    nst = len(steps)
                    nbg = len(bg)
                    done = 0
                    for i_ in range(nst + LOOK):
                        if i_ < nst:
                            emit_S(steps[i_])
                        if i_ >= LOOK:
                            emit_PV(steps[i_ - LOOK])
                        tgt = (nbg * (i_ + 1)) // (nst + LOOK)
                        while done < tgt:
                            bg[done]()
                            done += 1
                    while done < nbg:
                        bg[done]()
                        done += 1
                    bg = outproj_items(p)
                for it in bg:
                    it()
                h.end()

        def moe_phase(layer):
            modt = mod0 if layer == 0 else mod1
            g2c = 5 * DC
            with ExitStack() as st:
                selE = sb("selE", [16, NE, 128], BF16, st)
                combT = sb("combT", [16, S], BF16, st)
                with ExitStack() as st2:
                    rw = sb("rw", [128, DC, NE], F32, st2)
                    h32 = sb("h32", [128, DC, 512], F32, st2)
                    R = {n: sb("r_" + n, [128, 256], F32, st2) for n in ("sc", "sel", "a", "b", "c", "top2", "w")}
                    gs = sb("gs", [128, 64], F32, st2)
                    gtmp = sb("gtmp", [128, 64], F32, st2)
                    gmax = sb("gmax", [128, 16], F32, st2)
                    ohg = sb("ohg", [128, 64], F32, st2)
                    wsum = sb("wsum", [128, 16], F32, st2)
                    h.begin()
                    h.dma("sp", rw[:], router_w_d[:, :].rearrange("(dc p) e -> p dc e", p=128), w=["rw"], sem="rw")
                    for e_ in range(NE):
                        h.ts("pool", selE[:, e_, :], ones_f[0:16, :], ident[0:16, e_:e_ + 1], None, ALU.mult, r=["ones_f", "ident"], w=["selE"])

                    def router(tb, dc, t):
                        h.ts("dve", h32[:, dc, :], t[:], ncoef[:, dc:dc + 1], modt[:, 3 * DC + dc:3 * DC + dc + 1], ALU.mult, ALU.add,
                             r=[("t2", id(t)), "ncoef", "mod"], w=[("h32", dc)])
                        if dc == DC - 1:
                            for i in range(4):
                                ti_ = tb * 4 + i
                                for d2 in range(DC):
                                    h.mm(pb[2][:, ti_ * 16:(ti_ + 1) * 16], h32[:, d2, i * 128:(i + 1) * 128], rw[:, d2, :],
                                         start=(d2 == 0), stop=(d2 == DC - 1), r=[("h32", d2), "rw"], w=[PK(2)])

                    norm_phase(st2, 1 if layer == 0 else 4, modt, 3 * DC, 4 * DC, router=router)
                    sc, sel, A_, B_, C_, top2, w_ = (R[n] for n in ("sc", "sel", "a", "b", "c", "top2", "w"))
                    h.act(sc[:], pb[2][:, 0:256], AF.Sigmoid, r=[PK(2)], w=["r_sc"])
                    h.tt("dve", sel[:], sc[:], rb_rep[:], ALU.add, r=["r_sc", "rb_rep"], w=["r_sel"])
                    X = sel[:].rearrange("p (t e) -> p t e", e=4)
                    first = True
                    for (a, b) in ((0, 1), (0, 2), (0, 3), (1, 2), (1, 3), (2, 3)):
                        if first:
                            h.tt("dve", gs[:], X[:, :, a], X[:, :, b], ALU.add, r=["r_sel"], w=["gs"])
                            first = False
                        else:
                            h.tt("dve", gtmp[:], X[:, :, a], X[:, :, b], ALU.add, r=["r_sel"], w=["gtmp"])
                            h.tt("dve", gs[:], gs[:], gtmp[:], ALU.max, r=["gs", "gtmp"], w=["gs"])
                    G4 = gs[:].rearrange("p (t g) -> p t g", g=4)
                    h.tt("dve", gmax[:], G4[:, :, 0], G4[:, :, 1], ALU.max, r=["gs"], w=["gmax"])
                    h.tt("dve", gmax[:], gmax[:], G4[:, :, 2], ALU.max, r=["gs", "gmax"], w=["gmax"])
                    h.tt("dve", gmax[:], gmax[:], G4[:, :, 3], ALU.max, r=["gs", "gmax"], w=["gmax"])
                    O4 = ohg[:].rearrange("p (t g) -> p t g", g=4)
                    for g in range(4):
                        h.tt("dve", O4[:, :, g], G4[:, :, g], gmax[:], ALU.is_equal, r=["gs", "gmax"], w=["ohg"])
                    T2 = top2[:].rearrange("p (t e) -> p t e", e=4)
                    A3 = A_[:].rearrange("p (t e) -> p t e", e=4)
                    B3 = B_[:].rearrange("p (t e) -> p t e", e=4)
                    for e_ in range(4):
                        oth = [x for x in range(4) if x != e_]
                        h.tt("dve", A3[:, :, e_], X[:, :, oth[0]], X[:, :, e_], ALU.is_gt, r=["r_sel"], w=["r_a"])
                        h.tt("dve", B3[:, :, e_], X[:, :, oth[1]], X[:, :, e_], ALU.is_gt, r=["r_sel"], w=["r_b"])
                        h.tt("dve", A3[:, :, e_], A3[:, :, e_], B3[:, :, e_], ALU.add, r=["r_a", "r_b"], w=["r_a"])
                        h.tt("dve", B3[:, :, e_], X[:, :, oth[2]], X[:, :, e_], ALU.is_gt, r=["r_sel"], w=["r_b"])
                        h.tt("dve", A3[:, :, e_], A3[:, :, e_], B3[:, :, e_], ALU.add, r=["r_a", "r_b"], w=["r_a"])
                        h.ts("dve", T2[:, :, e_], A3[:, :, e_], 1.5, None, ALU.is_lt, r=["r_a"], w=["r_top2"])
                        h.tt("dve", T2[:, :, e_], T2[:, :, e_], ohg[:], ALU.mult, r=["r_top2", "ohg"], w=["r_top2"])
                    h.tt("dve", w_[:], sc[:], top2[:], ALU.mult, r=["r_sc", "r_top2"], w=["r_w"])
                    h.red(wsum[:], w_[:].rearrange("p (t e) -> p t e", e=16), ALU.add, r=["r_w"], w=["wsum"])
                    h.rcp(wsum[:], wsum[:], r=["wsum"], w=["wsum"])
                    for i in range(NT):
                        h.ts("dve", C_[:, i * 16:(i + 1) * 16], w_[:, i * 16:(i + 1) * 16], wsum[:, i:i + 1], None, ALU.mult,
                             r=["r_w", "wsum"], w=["r_c"])
                    for i in range(NT):
                        bk = 4 + (i // 4) % 2
                        h.tr(pb[bk][0:16, (i % 4) * 128:(i % 4 + 1) * 128], C_[:, i * 16:(i + 1) * 16], ident[:],
                             r=["r_c", "ident"], w=[PK(bk)])
                        if i % 4 == 3:
                            h.cp("act", combT[:, (i // 4) * 512:(i // 4 + 1) * 512], pb[bk][0:16, :], r=[PK(bk)], w=["combT"])
                    h.end()
                if layer == 1 and globals().get("_MOE1_SKIP_B", False):
                    return
                wg = [sb("wg%d" % i, [128, DC, 512], BF16, st) for i in range(2)]
                wu = [sb("wu%d" % i, [128, DC, 512], BF16, st) for i in range(2)]
                wd = [sb("wd%d" % i, [128, 4, D], BF16, st) for i in range(2)]
                aT = [sb("aT%d" % i, [128, 4, 512], BF16, st) for i in range(2)]
                sg = [sb("sg%d" % i, [128, 512], F32, st) for i in range(3)]
                cmb = [sb("cmb%d" % i, [128, 512], F32, st) for i in range(2)]
                h.begin()
                n_g = 0
                n_y = 0
                n_it = 0
                mod_items = []
                if layer == 0:
                    wmc = [sb("wmc%d" % i, [128, 1024], BF16, st) for i in range(2)]
                    for (wsrc, nblk, modt_, mkey) in ((w_mod_d[1], 6, mod1, "mod1acc"), (kv_w_mod_d, 2, modkv, "modkvacc")):
                        for blk in range(nblk):
                            for kc in range(DC):
                                mod_items.append((wsrc, blk, kc, modt_, mkey))

                def issue_mod_dma(n):
                    wsrc, blk, kc, modt_, mkey = mod_items[n]
                    wkey = ("wmc", n % 2)
                    h.dma("pool", wmc[n % 2][:], wsrc[kc * 128:(kc + 1) * 128, blk * 1024:(blk + 1) * 1024], w=[wkey], sem=wkey)

                if mod_items:
                    issue_mod_dma(0)

                def run_mod_item(n):
                    wsrc, blk, kc, modt_, mkey = mod_items[n]
                    wt = wmc[n % 2]
                    wkey = ("wmc", n % 2)
                    if n + 1 < len(mod_items):
                        issue_mod_dma(n + 1)
                    for j in range(8):
                        h.mm(pb[0][:, j:j + 1], wt[:, j * 128:(j + 1) * 128], cact[:, kc:kc + 1], r=[wkey, "cact"], w=[PK(0)])
                    h.tt("dve", modt_[:, blk * 8:(blk + 1) * 8], modt_[:, blk * 8:(blk + 1) * 8], pb[0][:, 0:8], ALU.add,
                         r=[PK(0), mkey], w=[mkey])

                def issue_expert(e2):
                    w2 = e2 % 2
                    h.dma("pool", wg[w2][:], wg_d[layer][e2].rearrange("(dc p) f -> p dc f", p=128), w=[("wg", w2)], sem=("wg", w2))
                    h.dma("pool", wu[w2][:], wu_d[layer][e2].rearrange("(dc p) f -> p dc f", p=128), w=[("wu", w2)], sem=("wu", w2))
                    h.dma("pool", wd[w2][:], wd_d[layer][e2].rearrange("(fc p) d -> p fc d", p=128), w=[("wd", w2)], sem=("wd", w2))

                issue_expert(0)
                for e_ in range(NE):
                    wb = e_ % 2
                    if e_ + 1 < NE:
                        issue_expert(e_ + 1)
                    for tb in range(NB):
                        ab = n_it % 2
                        n_it += 1
                        h.mm(pb[0][:, :], selE[:, e_, :], combT[:, tbs(tb)], r=["selE", "combT"], w=[PK(0)])
                        h.cp("act", cmb[ab][:], pb[0][:, :], r=[PK(0)], w=[("cmb", ab)])
                        for fc in range(4):
                            bg = 1 + n_g % 2
                            bu = 3 + n_g % 2
                            sgi = n_g % 3
                            n_g += 1
                            for dc in range(DC):
                                h.mm(pb[bg][:, :], wg[wb][:, dc, fc * 128:(fc + 1) * 128], hT[:, dc, tbs(tb)],
                                     start=(dc == 0), stop=(dc == DC - 1), r=[("wg", wb), ("hT", tb)], w=[PK(bg)])
                            for dc in range(DC):
                                h.mm(pb[bu][:, :], wu[wb][:, dc, fc * 128:(fc + 1) * 128], hT[:, dc, tbs(tb)],
                                     start=(dc == 0), stop=(dc == DC - 1), r=[("wu", wb), ("hT", tb)], w=[PK(bu)])
                            h.act(sg[sgi][:], pb[bg][:, :], AF.Silu, r=[PK(bg)], w=[("sg", sgi)])
                            h.tt("dve", sg[sgi][:], sg[sgi][:], cmb[ab][:], ALU.mult, r=[("sg", sgi), ("cmb", ab)], w=[("sg", sgi)])
                            h.tt("dve", aT[ab][:, fc, :], pb[bu][:, :], sg[sgi][:], ALU.mult, r=[PK(bu), ("sg", sgi)], w=[("aT", ab, fc)])
                            if fc == 1 and n_it - 1 < len(mod_items):
                                run_mod_item(n_it - 1)
                        for dcol in range(DC):
                            by = 5 + n_y % 3
                            n_y += 1
                            for fc in range(4):
                                h.mm(pb[by][:, :], wd[wb][:, fc, dcol * 128:(dcol + 1) * 128], aT[ab][:, fc, :],
                                     start=(fc == 0), stop=(fc == 3), r=[("wd", wb), ("aT", ab, fc)], w=[PK(by)])
                            h.stt("dve", xT[:, dcol, tbs(tb)], pb[by][:, :], modt[:, g2c + dcol:g2c + dcol + 1], xT[:, dcol, tbs(tb)],
                                  ALU.mult, ALU.add, r=[PK(by), "mod", ("xT", dcol, tb)], w=[("xT", dcol, tb)])
                h.end()

        def kv_phase():
            with ExitStack() as st:
                wkd = sb("wkd", [128, DC, 256], BF16, st)
                wkr = sb("wkr", [128, DC, 128], BF16, st)
                craw = sb("kraw", [128, 2, 512], F32, st)
                csq = [sb("ksq%d" % i, [128, 512], BF16, st) for i in range(2)]
                crs = sb("krs", [128, 512], F32, st)
                ra = [sb("kra%d" % i, [128, 512], F32, st) for i in range(2)]
                rb = [sb("krb%d" % i, [128, 512], F32, st) for i in range(2)]
                h.begin()
                h.memset("pool", wkr[:, :, 0:64], 0.0, w=["wkr_z"])
                dn = kv_w_down_d[:, :].rearrange("(dc p) c -> p dc c", p=128)
                h.dma("pool", wkd[:], dn[:, :, 0:256], w=["wkd"], sem="wkd")
                h.dma("pool", wkr[:, :, 64:96], dn[:, :, 256:288], w=["wkr0"], sem="wkr0")
                h.dma("pool", wkr[:, :, 96:112], dn[:, :, 272:288], w=["wkr1"], sem="wkr1")
                h.dma("pool", wkr[:, :, 112:128], dn[:, :, 256:272], w=["wkr2"], sem="wkr2")
                norm_phase(st, 2, modkv, 0, DC)
                for tb in range(NB):
                    for c in range(2):
                        bk = 2 + c
                        for dc in range(DC):
                            h.mm(pb[bk][:, :], wkd[:, dc, c * 128:(c + 1) * 128], hT[:, dc, tbs(tb)], start=(dc == 0), stop=(dc == DC - 1),
                                 r=["wkd", ("hT", tb)], w=[PK(bk)])
                        h.cp("act", craw[:, c, :], pb[bk][:, :], r=[PK(bk)], w=[("kraw", c)])
                        h.tt("dve", csq[c][:], craw[:, c, :], craw[:, c, :], ALU.mult, r=[("kraw", c)], w=[("ksq", c)])
                        h.mm(pb[4][:, :], ones_b[:, :], csq[c][:], start=(c == 0), stop=(c == 1), r=[("ksq", c), "ones_b"], w=[PK(4)])
                    h.act(crs[:], pb[4][:, :], AF.Ln, bias=epst[:, 0:1], scale=1.0 / 256, r=[PK(4), "epst"], w=["krs"])
                    h.act(crs[:], crs[:], AF.Exp, scale=-0.5, r=["krs"], w=["krs"])
                    for c in range(2):
                        h.stt("dve", ckvT[:, c, tbs(tb)], craw[:, c, :], lat_g[:, c:c + 1], crs[:], ALU.mult, ALU.mult,
                              r=[("kraw", c), "krs", "lat_g"], w=["ckvT"])
                    bk = 5 + tb % 2
                    for dc in range(DC):
                        h.mm(pb[bk][:, :], wkr[:, dc, :], hT[:, dc, tbs(tb)], start=(dc == 0), stop=(dc == DC - 1),
                             r=["wkr_z", "wkr0", "wkr1", "wkr2", ("hT", tb)], w=[PK(bk)])
                    cs_ = tbs(tb)
                    a_, b_ = ra[tb % 2], rb[tb % 2]
                    h.tt("dve", a_[64:128, :], pb[bk][64:128, :], csT[64:128, cs_], ALU.mult, r=[PK(bk), "rope_tab", "rope_tab2"], w=[("kra", tb % 2)])
                    h.cp("act", b_[64:96, :], a_[96:128, :], r=[("kra", tb % 2)], w=[("krb", tb % 2)])
                    h.tt("dve", krope[64:96, cs_], a_[64:96, :], b_[64:96, :], ALU.add, r=[("kra", tb % 2), ("krb", tb % 2)], w=[("krope", tb)])
                    h.cp("act", krope[96:128, cs_], krope[64:96, cs_], r=[("krope", tb)], w=[("kropeB", tb)])
                h.end()

        def final_phase(do_norm):
            with ExitStack() as st:
                sq = [sb("fsq%d" % i, [128, 512], BF16, st) for i in range(3)]
                rs = [sb("frs%d" % i, [128, 512], F32, st) for i in range(2)]
                ob = [sb("fob%d" % i, [128, 512], F32, st) for i in range(4)]
                h.begin()
                for _i in range(globals().get("_DUMMY", 0)):
                    _de = globals().get("_DUMMY_ENG", "pe")
                    if _de == "pe":
                        h.mm(pb[7][:, 0:16], ones_b[:, :], ones_b[:, 0:16], r=["ones_b"], w=[PK(7)])
                    elif _de == "dve":
                        h.memset("dve", sq[0][:, 0:8], 0.0, w=[])
                    elif _de == "actbig":
                        h.act(hT[:, 0, :], xT[:, 0, :], AF.Copy, r=[], w=[])
                    elif _de == "pooldma":
                        h.dma("pool", sq[2][:, 0:16], a_w_o_d[0:128, 0:16], w=["dummy_dma"], sem="dummy_dma")
                    else:
                        h.act(sq[1][:, 0:8], ones_b[:, 0:8], AF.Copy, r=[], w=[])
                n = 0
                for tb in range(NB):
                    if do_norm:
                        pss = pb[tb % 2]
                        for dc in range(DC):
                            q = sq[n % 3]
                            n += 1
                            h.act(q[:], xT[:, dc, tbs(tb)], AF.Square, r=[("xT", dc, tb)], w=[("sq", id(q))])
                            h.mm(pss[:, :], ones_b[:, :], q[:], start=(dc == 0), stop=(dc == DC - 1), r=[("sq", id(q)), "ones_b"], w=[PK(tb % 2)])
                        r_ = rs[tb % 2]
                        h.act(r_[:], pss[:, :], AF.Ln, bias=epst[:, 0:1], scale=1.0 / D, r=[PK(tb % 2), "epst"], w=[("rs", tb % 2)])
                        h.act(r_[:], r_[:], AF.Exp, scale=-0.5, r=[("rs", tb % 2)], w=[("rs", tb % 2)])
                    for dc in range(DC):
                        o = ob[n % 4]
                        ok = ("ob", n % 4)
                        n += 1
                        if do_norm:
                            h.stt("dve", o[:], xT[:, dc, tbs(tb)], gvec[:, 5 * DC + dc:5 * DC + dc + 1], r_[:], ALU.mult, ALU.mult,
                                  r=[("xT", dc, tb), "gvec", ("rs", tb % 2)], w=[ok])
                        else:
                            h.cp("dve", o[:], xT[:, dc, tbs(tb)], r=[("xT", dc, tb)], w=[ok])
                        h.dma("sp", outT_d[dc * 128:(dc + 1) * 128, tbs(tb)], o[:], r=[ok], w=[("outd", dc, tb)], sem=ok)
                h.end()

        fns = [lambda: attn_phase(0), lambda: moe_phase(0), kv_phase, lambda: attn_phase(1), lambda: moe_phase(1)]
        for i_ in range(5):
            if lo <= i_ <= hi and i_ not in globals().get("_SKIP", []):
                fns[i_]()
        for _i in range(globals().get("_DUMMY_BLOCKS", 0)):
            h.begin()
            h.memset("dve", ncoef[:, 8:9], 0.0, w=["x"])
            h.end()
        final_phase(hi >= 5)
    return nc


_CACHE = {}


def _lay(v, k):
    return np.ascontiguousarray(np.asarray(v, np.float32).reshape(k, 128).T)


def kernel(x, c, positions, a_norm_g, a_w_in, a_b_f, a_w_o, kv_norm_g, kv_w_mod, kv_b_mod,
           kv_w_down, kv_latent_g, kv_w_up, b_norm_g, b_w_dq, b_q_norm_g, b_w_uq, b_w_o,
           w_mod, b_mod, ffn_norm_g, router_w, router_bias, exp_w_gate, exp_w_up, exp_w_down,
           final_norm_g):
    f = lambda a: np.ascontiguousarray(np.asarray(a, np.float32))
    x = f(x)
    B = x.shape[0]
    if "nc" not in _CACHE:
        _CACHE["nc"] = build_program(0, 5)
    gvec = np.concatenate([_lay(a_norm_g[0], 8), _lay(ffn_norm_g[0], 8), _lay(kv_norm_g, 8), _lay(b_norm_g[0], 8),
                           _lay(ffn_norm_g[1], 8), _lay(final_norm_g, 8)], axis=1)
    bmod = np.concatenate([_lay(b_mod[0], 48), _lay(b_mod[1], 48), _lay(kv_b_mod, 16)], axis=1)
    bf_rep = np.ascontiguousarray(np.tile(np.asarray(a_b_f[0], np.float32)[None, :], (128, 16)))
    rb_rep = np.ascontiguousarray(np.tile(np.asarray(router_bias, np.float32)[None, :], (128, 16)))
    half = 16
    inv_freq = (10000.0 ** (-np.arange(half, dtype=np.float32) / half)).astype(np.float32)
    invf = np.zeros((128, 2), np.float32)
    for p in range(128):
        invf[p, 0] = inv_freq[p % 16]
        invf[p, 1] = -1.0 if (p % 32) < 16 else 1.0
    shared = {
        "invf": invf, "gvec": np.ascontiguousarray(gvec), "bmod": np.ascontiguousarray(bmod), "bf_rep": bf_rep, "rb_rep": rb_rep,
        "lat_g": _lay(kv_latent_g, 2), "q_g": _lay(b_q_norm_g[0], 6),
        "w_mod": f(w_mod), "kv_w_mod": f(kv_w_mod), "a_w_in": f(a_w_in[0]), "a_w_o": f(a_w_o[0]),
        "kv_w_down": f(kv_w_down), "kv_w_up": f(kv_w_up), "b_w_dq": f(b_w_dq[0]), "b_w_uq": f(b_w_uq[0]),
        "b_w_o": f(b_w_o[0]), "router_w": f(router_w),
        "exp_w_gate0": f(exp_w_gate[0]), "exp_w_gate1": f(exp_w_gate[1]), "exp_w_up0": f(exp_w_up[0]), "exp_w_up1": f(exp_w_up[1]),
        "exp_w_down0": f(exp_w_down[0]), "exp_w_down1": f(exp_w_down[1]),
    }
    in_maps = []
    for b in range(B):
        m = dict(shared)
        m["xT"] = np.ascontiguousarray(x[b].T)
        m["c_l"] = _lay(c[b], 8)
        m["pos"] = np.ascontiguousarray(np.asarray(positions[b], np.int32)[None, :])
        in_maps.append(m)
    res = run_bass_kernel_spmd(_CACHE["nc"], in_maps, core_ids=list(range(B)))
    out = np.stack([np.asarray(r["outT"], np.float32).T for r in res.results], axis=0)
    return np.ascontiguousarray(out)
```

```python
import numpy as np
from contextlib import ExitStack
import concourse.bass as bass
import concourse.mybir as mybir
from concourse.bass_utils import run_bass_kernel_spmd

F32 = mybir.dt.float32
BF16 = mybir.dt.bfloat16
I32 = mybir.dt.int32
AF = mybir.ActivationFunctionType
ALU = mybir.AluOpType
AX = mybir.AxisListType

S = 2048
D = 1024
NT = 16
NB = 4
DC = 8
NE = 16
EPS = 1e-6
SAME_ENGINE_SYNC = True
STOP_AFTER = "final"


class _Op:
    __slots__ = ("eng", "idx", "fn", "deps", "need_sig", "sig_val", "dma", "dma_val")

    def __init__(self, eng, idx, fn, dma):
        self.eng = eng
        self.idx = idx
        self.fn = fn
        self.deps = []
        self.need_sig = False
        self.sig_val = 0
        self.dma = dma
        self.dma_val = 0


class Prog:
    ENGS = ("pe", "act", "dve", "pool", "sp")

    def __init__(self, nc):
        self.nc = nc
        self.ops = {e: [] for e in self.ENGS}
        self.last_w = {}
        self.readers = {}
        self.dma_cnt = {}

    def op(self, eng, fn, r=(), w=(), dma=None):
        o = _Op(eng, len(self.ops[eng]), fn, dma)
        deps = {}

        def add(d):
            if d.dma is not None:
                key = ("d", d.dma)
                if key not in deps or deps[key].dma_val < d.dma_val:
                    deps[key] = d
            else:
                if d.eng == eng and (eng == "pe" or not SAME_ENGINE_SYNC):
                    return
                key = ("c", d.eng)
                if key not in deps or deps[key].idx < d.idx:
                    deps[key] = d

        for k in r:
            d = self.last_w.get(k)
            if d is not None:
                add(d)
        for k in w:
            d = self.last_w.get(k)
            if d is not None:
                add(d)
            rd = self.readers.get(k)
            if rd:
                for d in rd.values():
                    add(d)
        for d in deps.values():
            if d.dma is None:
                d.need_sig = True
            o.deps.append(d)
        for k in w:
            self.last_w[k] = o
            self.readers[k] = {}
        for k in r:
            rd = self.readers.setdefault(k, {})
            if dma is not None:
                rd[("d", dma, o.idx)] = o
            else:
                rd[("c", eng)] = o
        if dma is not None:
            c = self.dma_cnt.get(dma, 0) + 1
            self.dma_cnt[dma] = c
            o.dma_val = 16 * c
        self.ops[eng].append(o)
        return o

    def emit(self, pool, final_waits=()):
        nc = self.nc
        for e in self.ENGS:
            c = 0
            for o in self.ops[e]:
                if o.dma is None and o.need_sig:
                    c += 1
                    o.sig_val = pool.ebase[e] + c
            pool.ebase[e] += c
        swk = set()
        for o in self.ops["pool"]:
            if o.dma is not None:
                swk.add(o.dma)
        slot = {}
        n_sw, n_hw = 0, 0
        for k in self.dma_cnt:
            if k in swk:
                slot[k] = n_sw
                n_sw += 1
            else:
                slot[k] = pool.n_sw + n_hw
                n_hw += 1
        assert n_sw <= pool.n_sw and n_hw <= len(pool.dsem) - pool.n_sw, (n_sw, n_hw)
        for e in self.ENGS:
            for o in self.ops[e]:
                if o.dma is not None:
                    o.dma_val += pool.dbase[slot[o.dma]]
        esem = pool.esem
        dsem = {k: pool.dsem[i] for k, i in slot.items()}
        all_final = {k: pool.dbase[slot[k]] + 16 * self.dma_cnt[k] for k in slot}
        for k, i in slot.items():
            pool.dbase[i] += 16 * self.dma_cnt[k]
        with nc.Block() as block:

            def run(e, engobj):
                waited = {}
                for o in self.ops[e]:
                    ws = []
                    for d in o.deps:
                        if d.dma is not None:
                            sem, val, sk = dsem[d.dma], d.dma_val, ("d", d.dma)
                        else:
                            sem, val, sk = esem[d.eng], d.sig_val, ("c", d.eng)
                        if waited.get(sk, 0) >= val:
                            continue
                        waited[sk] = val
                        ws.append((sem, val))
                    for sem, val in ws[:-1]:
                        engobj.wait_ge(sem, val)
                    ins = o.fn(engobj)
                    if ws:
                        ins._wait_ge(ws[-1][0], ws[-1][1])
                    if o.dma is not None:
                        ins.then_inc(dsem[o.dma], 16)
                    elif o.need_sig:
                        ins.then_inc(esem[e], 1)
                if e == "sp":
                    for k, v in all_final.items():
                        engobj.wait_ge(dsem[k], v)

            @block.tensor
            def _(pe):
                run("pe", pe)

            @block.scalar
            def _(act):
                run("act", act)

            @block.vector
            def _(dve):
                run("dve", dve)

            @block.gpsimd
            def _(pool_):
                run("pool", pool_)

            @block.sync
            def _(sp):
                run("sp", sp)


class SemPool:
    def __init__(self, nc, st, n_dma=54, n_sw=32):
        self.n_sw = n_sw
        self.esem = {e: st.enter_context(nc.semaphore("g_" + e)) for e in Prog.ENGS}
        self.ebase = {e: 0 for e in Prog.ENGS}
        self.dsem = [st.enter_context(nc.semaphore("gd%d" % i)) for i in range(n_dma)]
        self.dbase = [0] * n_dma


class H:
    def __init__(self, nc):
        self.nc = nc
        self.P = None
        self.pool = None

    def begin(self):
        self.P = Prog(self.nc)

    def end(self, final_waits=()):
        self.P.emit(self.pool, final_waits)
        self.P = None

    def mm(self, out, lhsT, rhs, start=True, stop=True, r=(), w=()):
        self.P.op("pe", lambda e: e.matmul(out, lhsT, rhs, start=start, stop=stop), r, w)

    def tr(self, out, in_, ident, r=(), w=()):
        self.P.op("pe", lambda e: e.transpose(out, in_, ident), r, w)

    def act(self, out, in_, func, bias=0.0, scale=1.0, r=(), w=()):
        self.P.op("act", lambda e: e.activation(out, in_, func, bias=bias, scale=scale), r, w)

    def tt(self, eng, out, in0, in1, op, r=(), w=()):
        self.P.op(eng, lambda e: e.tensor_tensor(out, in0, in1, op), r, w)

    def ts(self, eng, out, in0, s1, s2, op0, op1=None, r=(), w=()):
        if op1 is None:
            self.P.op(eng, lambda e: e.tensor_scalar(out, in0, s1, None, op0), r, w)
        else:
            self.P.op(eng, lambda e: e.tensor_scalar(out, in0, s1, s2, op0, op1), r, w)

    def stt(self, eng, out, in0, sc, in1, op0, op1, r=(), w=()):
        self.P.op(eng, lambda e: e.scalar_tensor_tensor(out, in0, sc, in1, op0, op1), r, w)

    def cp(self, eng, out, in_, r=(), w=()):
        if eng == "act":
            self.P.op("act", lambda e: e.copy(out, in_), r, w)
        else:
            self.P.op(eng, lambda e: e.tensor_copy(out, in_), r, w)

    def rcp(self, out, in_, r=(), w=()):
        self.P.op("dve", lambda e: e.reciprocal(out, in_), r, w)

    def red(self, out, in_, op, r=(), w=()):
        self.P.op("dve", lambda e: e.tensor_reduce(out, in_, AX.X, op), r, w)

    def memset(self, eng, ap, val, w=()):
        self.P.op(eng, lambda e: e.memset(ap, val), (), w)

    def dma(self, q, out, in_, r=(), w=(), sem=None):
        self.P.op(q, lambda e: e.dma_start(out=out, in_=in_), r, w, dma=sem)


def build_program(lo=0, hi=5):
    nc = bass.Bass("TRN2", target_bir_lowering=False)
    h = H(nc)

    def din(name, shape, dt=F32):
        return nc.dram_tensor(name, list(shape), dt, kind="ExternalInput").ap()

    xT_d = din("xT", [D, S])
    c_d = din("c_l", [128, DC])
    pos_d = din("pos", [1, S], I32)
    invf_d = din("invf", [128, 2])
    gvec_d = din("gvec", [128, 6 * DC])
    bmod_d = din("bmod", [128, 112])
    bf_d = din("bf_rep", [128, 256])
    rb_d = din("rb_rep", [128, 256])
    lg_d = din("lat_g", [128, 2])
    qg_d = din("q_g", [128, 6])
    w_mod_d = din("w_mod", [2, D, 6 * D])
    kv_w_mod_d = din("kv_w_mod", [D, 2 * D])
    a_w_in_d = din("a_w_in", [D, 3 * D + 16])
    a_w_o_d = din("a_w_o", [D, D])
    kv_w_down_d = din("kv_w_down", [D, 288])
    kv_w_up_d = din("kv_w_up", [256, 2048])
    b_w_dq_d = din("b_w_dq", [D, 768])
    b_w_uq_d = din("b_w_uq", [768, 1536])
    b_w_o_d = din("b_w_o", [D, D])
    router_w_d = din("router_w", [D, NE])
    wg_d = [din("exp_w_gate%d" % l_, [NE, D, 512]) for l_ in range(2)]
    wu_d = [din("exp_w_up%d" % l_, [NE, D, 512]) for l_ in range(2)]
    wd_d = [din("exp_w_down%d" % l_, [NE, 512, D]) for l_ in range(2)]
    outT_d = nc.dram_tensor("outT", [D, S], F32, kind="ExternalOutput").ap()

    with ExitStack() as G:
        _uid = [0]

        def sb(name, shape, dt, st=G):
            _uid[0] += 1
            return st.enter_context(nc.sbuf_tensor("s%d_%s" % (_uid[0], name), list(shape), dt))

        pb = [G.enter_context(nc.psum_tensor("pb%d" % i, [128, 512], F32)) for i in range(8)]
        h.pool = SemPool(nc, G)

        def PK(i):
            return ("ps", i)

        xT = sb("xT", [128, DC, S], F32)
        hT = sb("hT", [128, DC, S], BF16)
        ident = sb("ident", [128, 128], F32)
        ones_f = sb("ones_f", [128, 128], F32)
        ones_b = sb("ones_b", [128, 128], BF16)
        triu_f = sb("triu_f", [128, 128], F32)
        triu_b = sb("triu_b", [128, 128], BF16)
        cmask_b = sb("cmask_b", [128, 128], BF16)
        epst = sb("epst", [128, 1], F32)
        mod0 = sb("mod0", [128, 48], F32)
        mod1 = sb("mod1", [128, 48], F32)
        modkv = sb("modkv", [128, 16], F32)
        gvec = sb("gvec_s", [128, 6 * DC], F32)
        bmod = sb("bmod_s", [128, 112], F32)
        bf_rep = sb("bf_rep_s", [128, 256], F32)
        rb_rep = sb("rb_rep_s", [128, 256], F32)
        lat_g = sb("lat_g_s", [128, 2], F32)
        q_g = sb("q_g_s", [128, 6], F32)
        csT = sb("csT", [128, S], BF16)
        ckvT = sb("ckvT", [128, 2, S], BF16)
        krope = sb("krope", [128, S], BF16)
        ncoef = sb("ncoef", [128, 2 * DC], F32)
        cact = sb("cact", [128, DC], BF16)

        def tbs(tb):
            return slice(tb * 512, (tb + 1) * 512)

        def norm_phase(st, gidx, modt, sh0, sc0, nchunks=DC, src=None, dst=None, dst_keyf=None,
                       router=None):
            sq = [sb("sq%d" % i, [128, 512], BF16, st) for i in range(3)]
            rs = [sb("rs%d" % i, [128, 512], F32, st) for i in range(2)]
            t2 = [sb("t2_%d" % i, [128, 512], F32, st) for i in range(3)]
            h.stt("dve", ncoef[:, 0:DC], modt[:, sc0:sc0 + DC], 1.0, gvec[:, gidx * DC:(gidx + 1) * DC],
                  ALU.add, ALU.mult, r=["mod", "gvec"], w=["ncoef"])
            n = 0
            for tb in range(NB):
                pss = pb[tb % 2]
                for dc in range(DC):
                    q = sq[n % 3]
                    n += 1
                    h.act(q[:], xT[:, dc, tbs(tb)], AF.Square, r=[("xT", dc, tb)], w=[("sq", id(q))])
                    h.mm(pss[:, :], ones_b[:, :], q[:], start=(dc == 0), stop=(dc == DC - 1),
                         r=[("sq", id(q)), "ones_b"], w=[PK(tb % 2)])
                r_ = rs[tb % 2]
                h.act(r_[:], pss[:, :], AF.Ln, bias=epst[:, 0:1], scale=1.0 / D, r=[PK(tb % 2), "epst"], w=[("rs", tb % 2)])
                h.act(r_[:], r_[:], AF.Exp, scale=-0.5, r=[("rs", tb % 2)], w=[("rs", tb % 2)])
                for dc in range(DC):
                    t = t2[n % 3]
                    n += 1
                    h.tt("dve", t[:], xT[:, dc, tbs(tb)], r_[:], ALU.mult, r=[("xT", dc, tb), ("rs", tb % 2)], w=[("t2", id(t))])
                    h.act(hT[:, dc, tbs(tb)], t[:], AF.Identity, bias=modt[:, sh0 + dc:sh0 + dc + 1],
                          scale=ncoef[:, dc:dc + 1], r=[("t2", id(t)), "ncoef", "mod"], w=[("hT", tb)])
                    if router is not None:
                        router(tb, dc, t)

        with ExitStack() as st:
            cs = sb("c_s", [128, DC], F32, st)
            wm = [sb("wm%d" % i, [128, 6 * D], BF16, st) for i in range(3)]
            posi = sb("posi", [128, S], I32, st)
            invf = sb("invf_s", [128, 2], F32, st)
            sinT = sb("sinT", [128, S], BF16, st)
            cosT = sb("cosT", [128, S], BF16, st)
            ang = sb("ang", [128, S], F32, st)
            ta = sb("ta", [128, S], F32, st)
            tb_ = sb("tb_", [128, S], F32, st)
            ti = sb("ti", [128, S], I32, st)
            h.begin()
            h.dma("sp", cs[:], c_d[:, :], w=["cs"], sem="cs")
            h.dma("sp", gvec[:], gvec_d[:, :], w=["gvec"], sem="ld_gvec")
            h.dma("sp", bmod[:], bmod_d[:, :], w=["bmod"], sem="ld_bmod")
            h.dma("sp", bf_rep[:], bf_d[:, :], w=["bf_rep"], sem="ld_bf_rep")
            h.dma("sp", rb_rep[:], rb_d[:, :], w=["rb_rep"], sem="ld_rb_rep")
            h.dma("sp", lat_g[:], lg_d[:, :], w=["lat_g"], sem="ld_lat_g")
            h.dma("sp", q_g[:], qg_d[:, :], w=["q_g"], sem="ld_q_g")
            h.dma("sp", invf[:], invf_d[:, :], w=["invf"], sem="ld_invf")
            h.dma("sp", posi[:], pos_d.partition_broadcast(128), w=["posi"], sem="ld_posi")
            h.memset("pool", ones_f[:], 1.0, w=["ones_f"])
            h.memset("pool", ones_b[:], 1.0, w=["ones_b"])
            h.memset("pool", epst[:], EPS, w=["epst"])
            h.P.op("pool", lambda e: e.affine_select(out=ident[:], in_=ones_f[:], pattern=[[-1, 128]], compare_op=ALU.is_equal,
                                                     fill=0.0, base=0, channel_multiplier=1), ["ones_f"], ["ident"])
            h.P.op("pool", lambda e: e.affine_select(out=triu_f[:], in_=ones_f[:], pattern=[[1, 128]], compare_op=ALU.is_ge,
                                                     fill=0.0, base=0, channel_multiplier=-1), ["ones_f"], ["triu_f"])
            h.cp("pool", triu_b[:], triu_f[:], r=["triu_f"], w=["triu_b"])
            h.memset("pool", cmask_b[:], 1.0, w=["cmask_b"])
            h.memset("pool", cmask_b[64:128, 0:64], 0.0, w=["cmask_b"])
            h.memset("pool", krope[:], 0.0, w=["krope"])
            for dc in range(DC):
                h.dma("sp", xT[:, dc, :], xT_d[dc * 128:(dc + 1) * 128, :], w=[("xT", dc, t) for t in range(NB)], sem=("xload", dc))
            h.act(cact[:], cs[:], AF.Silu, r=["cs"], w=["cact"])
            wi = 0
            h.cp("dve", mod1[:, :], bmod[:, 48:96], r=["bmod"], w=["mod1acc"])
            h.cp("dve", modkv[:, :], bmod[:, 96:112], r=["bmod"], w=["modkvacc"])
            for (wsrc, ncol, modt, boff) in ((w_mod_d[0], 48, mod0, 0),):
                psm = pb[2 + (wi % 2)]
                for kc in range(DC):
                    wt = wm[wi % 3]
                    wkey = ("wm", wi % 3)
                    wi += 1
                    h.dma("pool", wt[:, 0:ncol * 128], wsrc[kc * 128:(kc + 1) * 128, :], w=[wkey], sem=wkey)
                    for j in range(ncol):
                        h.mm(psm[:, kc * ncol + j:kc * ncol + j + 1], wt[:, j * 128:(j + 1) * 128], cact[:, kc:kc + 1],
                             r=[wkey, "cact"], w=[("psm", id(psm))])
                h.red(modt[:, 0:ncol], psm[:, 0:DC * ncol].rearrange("p (k j) -> p j k", k=DC), ALU.add, r=[("psm", id(psm))], w=["mod"])
                h.tt("dve", modt[:, 0:ncol], modt[:, 0:ncol], bmod[:, boff:boff + ncol], ALU.add, r=["mod", "bmod"], w=["mod"])
            h.cp("dve", ang[:], posi[:], r=["posi"], w=["ang"])
            h.ts("dve", ang[:], ang[:], invf[:, 0:1], None, ALU.mult, r=["ang", "invf"], w=["ang"])
            for (dst, shift) in ((sinT, 0.0), (cosT, float(np.pi / 2))):
                if shift != 0.0:
                    h.ts("dve", ang[:], ang[:], shift, None, ALU.add, r=["ang"], w=["ang"])
                h.ts("dve", ta[:], ang[:], float(1.0 / (2 * np.pi)), None, ALU.mult, r=["ang"], w=["ta"])
                h.cp("dve", ti[:], ta[:], r=["ta"], w=["ti"])
                h.cp("dve", ta[:], ti[:], r=["ti"], w=["ta"])
                h.stt("dve", tb_[:], ta[:], float(-2 * np.pi), ang[:], ALU.mult, ALU.add, r=["ta", "ang"], w=["tb_"])
                h.ts("dve", ta[:], tb_[:], float(np.pi), float(-2 * np.pi), ALU.is_gt, ALU.mult, r=["tb_"], w=["ta"])
                h.tt("dve", tb_[:], tb_[:], ta[:], ALU.add, r=["tb_", "ta"], w=["tb_"])
                h.ts("dve", ta[:], tb_[:], float(-np.pi), float(2 * np.pi), ALU.is_lt, ALU.mult, r=["tb_"], w=["ta"])
                h.tt("dve", tb_[:], tb_[:], ta[:], ALU.add, r=["tb_", "ta"], w=["tb_"])
                h.act(dst[:], tb_[:], AF.Sin, r=["tb_"], w=[("trig", id(dst))])
            h.memset("pool", csT[0:64, :], 0.0, w=["cs_lo"])
            h.cp("pool", csT[64:96, :], cosT[64:96, :], r=[("trig", id(cosT))], w=["rope_tab"])
            h.ts("dve", csT[96:128, :], sinT[96:128, :], invf[96:128, 1:2], None, ALU.mult, r=[("trig", id(sinT)), "invf"], w=["rope_tab2"])
            h.end()

        def attn_phase(layer):
            fox = layer == 0
            modt = mod0 if fox else mod1
            if fox:
                with ExitStack() as st0:
                    h.begin()
                    norm_phase(st0, 0, modt, 0, DC)
                    h.end()
            with ExitStack() as st:
                h.begin()
                cqT = hT
                NSET = 2
                qh = [[sb("qh%d_%d" % (s_, i), [128, S], BF16, st) for i in range(2)] for s_ in range(NSET)]
                kh = [[sb("kh%d_%d" % (s_, i), [128, S], BF16, st) for i in range(2)] for s_ in range(NSET)]
                Vp = [sb("Vp%d" % s_, [128, NT, 192], BF16, st) for s_ in range(NSET)]
                oT = [sb("oT%d" % i, [128, S], BF16, st) for i in range(2)]
                wo = [sb("wo%d" % i, [128, D], BF16, st) for i in range(3)]
                PT = [sb("PT%d" % i, [128, 512], BF16, st) for i in range(6)]
                rec = [sb("rec%d" % i, [128, 512], F32, st) for i in range(2)]
                zero_bias = sb("zero_bias", [128, 1], F32, st)
                h.memset("pool", zero_bias[:], 0.0, w=["zero_bias"])
                for s_ in range(NSET):
                    h.memset("pool", Vp[s_][:, :, 64:128], 1.0, w=[("Vp", s_)])
                    for i in range(2):
                        h.memset("pool", qh[s_][i][:], 0.0, w=[("qh", s_, i)])
                        if fox:
                            h.memset("pool", kh[s_][i][:], 0.0, w=[("kh", s_, i)])
                            h.memset("pool", kh[s_][i][64:65, :], 1.0, w=[("kh", s_, i)])
                        else:
                            h.cp("pool", kh[s_][i][:], krope[:], r=["krope"], w=[("kh", s_, i)])
                if fox:
                    wpair = [sb("wpair%d" % i, [128, DC, 384], BF16, st) for i in range(2)]
                    wf = sb("wf", [128, DC, 16], BF16, st)
                    lf = sb("lf", [128, 256], F32, st)
                    tot = sb("tot", [128, 256], F32, st)
                    off = sb("off", [128, 256], F32, st)
                    negcum = sb("negcum", [128, 256], F32, st)
                    cum8 = sb("cum8", [128, 256], F32, st)
                    cumT = sb("cumT", [16, S], BF16, st)
                    h.dma("pool", wf[:], a_w_in_d[:, 3 * D:3 * D + 16].rearrange("(dc p) c -> p dc c", p=128), w=["wf"], sem="wf")
                    for i in range(NT):
                        for dc in range(DC):
                            h.mm(pb[2][:, i * 16:(i + 1) * 16], hT[:, dc, i * 128:(i + 1) * 128], wf[:, dc, :],
                                 start=(dc == 0), stop=(dc == DC - 1), r=[("hT", i // 4), "wf"], w=[PK(2)])
                    h.tt("dve", lf[:], pb[2][:, 0:256], bf_rep[:], ALU.add, r=[PK(2), "bf_rep"], w=["lf"])
                    h.act(lf[:], lf[:], AF.Sigmoid, r=["lf"], w=["lf"])
                    h.act(lf[:], lf[:], AF.Ln, r=["lf"], w=["lf"])
                    h.mm(pb[3][:, 0:256], triu_f[:], lf[:], r=["triu_f", "lf"], w=[PK(3)])
                    h.mm(pb[2][:, 0:256], ones_f[:], lf[:], r=["ones_f", "lf"], w=[PK(2)])
                    h.cp("dve", tot[:], pb[2][:, 0:256], r=[PK(2)], w=["tot"])
                    h.memset("dve", off[:, 0:16], 0.0, w=["off"])
                    for i in range(1, NT):
                        h.tt("dve", off[:, i * 16:(i + 1) * 16], off[:, (i - 1) * 16:i * 16], tot[:, (i - 1) * 16:i * 16], ALU.add,
                             r=["off", "tot"], w=["off"])
                    h.tt("dve", cum8[:], pb[3][:, 0:256], off[:], ALU.add, r=[PK(3), "off"], w=["cum8"])
                    h.ts("dve", negcum[:], cum8[:], -1.0, None, ALU.mult, r=["cum8"], w=["negcum"])
                    h.ts("dve", cum8[:], cum8[:], 8.0, None, ALU.mult, r=["cum8"], w=["cum8"])
                    for i in range(NT):
                        bk = 4 + (i // 4) % 2
                        h.tr(pb[bk][0:16, (i % 4) * 128:(i % 4 + 1) * 128], cum8[:, i * 16:(i + 1) * 16], ident[:],
                             r=["cum8", "ident"], w=[PK(bk)])
                        if i % 4 == 3:
                            h.cp("act", cumT[:, (i // 4) * 512:(i // 4 + 1) * 512], pb[bk][0:16, :], r=[PK(bk)], w=["cumT"])
                else:
                    wq = [sb("wq%d" % i, [128, 6, 256], BF16, st) for i in range(2)]
                    wkv = [sb("wkv%d" % i, [128, 2, 256], BF16, st) for i in range(2)]

                g1c = 2 * DC
                w_o_d = a_w_o_d if fox else b_w_o_d
                scale = 0.125 if fox else float(96 ** -0.5)
                mask = triu_b if fox else cmask_b
                Kc = 65 if fox else 128
                cnt = {"ps_s": 0, "pt": 0, "ps_o": 0, "rec": 0, "op": 0}
                NPT = 6

                bgc = {"bk": 0}

                def bgbank():
                    bgc["bk"] += 1
                    return 5 + bgc["bk"] % 3

                def issue_weights(p):
                    wb = p % 2
                    h.dma("pool", wo[p % 3][:], w_o_d[p * 128:(p + 1) * 128, :], w=[("wo", p % 3)], sem=("wo", p % 3))
                    if fox:
                        for part in range(3):
                            c0 = part * D + p * 128
                            h.dma("pool", wpair[wb][:, :, part * 128:(part + 1) * 128],
                                  a_w_in_d[:, c0:c0 + 128].rearrange("(dc p) c -> p dc c", p=128), w=[("wpair", wb, part)], sem=("wpair", wb, part))
                    else:
                        for hh in range(2):
                            hd = 2 * p + hh
                            uq = b_w_uq_d[:, hd * 96:(hd + 1) * 96].rearrange("(kc p) c -> p kc c", p=128)
                            h.dma("pool", wq[wb][:, :, hh * 128:hh * 128 + 64], uq[:, :, 0:64], w=[("wq", wb, hh, 0)], sem=("wq", wb, hh, 0))
                            h.dma("pool", wq[wb][:, :, hh * 128 + 64:hh * 128 + 96], uq[:, :, 64:96], w=[("wq", wb, hh, 1)], sem=("wq", wb, hh, 1))
                            h.dma("pool", wq[wb][:, :, hh * 128 + 96:hh * 128 + 112], uq[:, :, 80:96], w=[("wq", wb, hh, 2)], sem=("wq", wb, hh, 2))
                            h.dma("pool", wq[wb][:, :, hh * 128 + 112:hh * 128 + 128], uq[:, :, 64:80], w=[("wq", wb, hh, 3)], sem=("wq", wb, hh, 3))
                            up = kv_w_up_d[:, hd * 128:(hd + 1) * 128].rearrange("(c p) n -> p c n", p=128)
                            h.dma("pool", wkv[wb][:, :, hh * 64:(hh + 1) * 64], up[:, :, 0:64], w=[("wkv", wb, hh, 0)], sem=("wkv", wb, hh, 0))
                            h.dma("pool", wkv[wb][:, :, 128 + hh * 64:128 + (hh + 1) * 64], up[:, :, 64:128], w=[("wkv", wb, hh, 1)], sem=("wkv", wb, hh, 1))

                def proj_items(p):
                    s_ = p % NSET
                    wb = p % 2
                    items = []

                    def v_item(g):
                        def f():
                            bk = bgbank()
                            for i in range(4 * g, 4 * g + 4):
                                if fox:
                                    for dc in range(DC):
                                        h.mm(pb[bk][:, (i % 4) * 128:(i % 4 + 1) * 128], hT[:, dc, i * 128:(i + 1) * 128], wpair[wb][:, dc, 256:384],
                                             start=(dc == 0), stop=(dc == DC - 1), r=[("wpair", wb, 2), ("hT", i // 4)], w=[PK(bk)])
                                else:
                                    for c in range(2):
                                        h.mm(pb[bk][:, (i % 4) * 128:(i % 4 + 1) * 128], ckvT[:, c, i * 128:(i + 1) * 128], wkv[wb][:, c, 128:256],
                                             start=(c == 0), stop=(c == 1), r=[("wkv", wb, 0, 1), ("wkv", wb, 1, 1), "ckvT"], w=[PK(bk)])
                            pv = pb[bk][:, :].rearrange("p (i c) -> p i c", c=128)
                            h.cp("act", Vp[s_][:, 4 * g:4 * g + 4, 0:64], pv[:, :, 0:64], r=[PK(bk)], w=[("Vp", s_)])
                            h.cp("dve", Vp[s_][:, 4 * g:4 * g + 4, 128:192], pv[:, :, 64:128], r=[PK(bk)], w=[("Vp", s_)])
                        return f

                    if fox:
                        def qk_item(which, tb):
                            def f():
                                nm = "qh" if which == 0 else "kh"
                                dsts = qh[s_] if which == 0 else kh[s_]
                                bk = bgbank()
                                for dc in range(DC):
                                    h.mm(pb[bk][:, :], wpair[wb][:, dc, which * 128:(which + 1) * 128], hT[:, dc, tbs(tb)],
                                         start=(dc == 0), stop=(dc == DC - 1), r=[("wpair", wb, which), ("hT", tb)], w=[PK(bk)])
                                h.cp("act", dsts[0][0:64, tbs(tb)], pb[bk][0:64, :], r=[PK(bk)], w=[(nm, s_, 0)])
                                h.cp("dve", dsts[1][0:64, tbs(tb)], pb[bk][64:128, :], r=[PK(bk)], w=[(nm, s_, 1)])
                            return f

                        def aug_item():
                            for hh in range(2):
                                h.dma("sp", qh[s_][hh][64:65, :], cumT[2 * p + hh:2 * p + hh + 1, :], r=["cumT"], w=[("qh", s_, hh)], sem=("aug", s_, hh))
                        items.append(aug_item)
                        for which in range(2):
                            for tb in range(NB):
                                items.append(qk_item(which, tb))
                    else:
                        def q_item(hh, tb):
                            def f():
                                bk = bgbank()
                                for kc in range(6):
                                    h.mm(pb[bk][:, :], wq[wb][:, kc, hh * 128:(hh + 1) * 128], cqT[:, kc, tbs(tb)],
                                         start=(kc == 0), stop=(kc == 5),
                                         r=[("wq", wb, hh, 0), ("wq", wb, hh, 1), ("wq", wb, hh, 2), ("wq", wb, hh, 3), ("hT", tb)], w=[PK(bk)])
                                h.cp("act", qh[s_][hh][0:64, tbs(tb)], pb[bk][0:64, :], r=[PK(bk)], w=[("qh", s_, hh)])
                                h.tt("dve", qh[s_][hh][64:128, tbs(tb)], pb[bk][64:128, :], csT[64:128, tbs(tb)], ALU.mult,
                                     r=[PK(bk), "rope_tab", "rope_tab2"], w=[("qh", s_, hh)])
                            return f

                        def k_item(tb):
                            def f():
                                bk = bgbank()
                                for c in range(2):
                                    h.mm(pb[bk][:, :], wkv[wb][:, c, 0:128], ckvT[:, c, tbs(tb)], start=(c == 0), stop=(c == 1),
                                         r=[("wkv", wb, 0, 0), ("wkv", wb, 1, 0), "ckvT"], w=[PK(bk)])
                                h.cp("act", kh[s_][0][0:64, tbs(tb)], pb[bk][0:64, :], r=[PK(bk)], w=[("kh", s_, 0)])
                                h.cp("dve", kh[s_][1][0:64, tbs(tb)], pb[bk][64:128, :], r=[PK(bk)], w=[("kh", s_, 1)])
                            return f
                        for hh in range(2):
                            for tb in range(NB):
                                items.append(q_item(hh, tb))
                        for tb in range(NB):
                            items.append(k_item(tb))
                    for g in range(4):
                        items.append(v_item(g))
                    return items

                def outproj_items(p):
                    wb = p % 3
                    ob = p % 2
                    items = []

                    def o_item(tb, dcol):
                        def f():
                            bo = bgbank()
                            h.mm(pb[bo][:, :], wo[wb][:, dcol * 128:(dcol + 1) * 128], oT[ob][:, tbs(tb)],
                                 r=[("wo", wb), ("oT", ob, tb)], w=[PK(bo)])
                            h.stt("dve", xT[:, dcol, tbs(tb)], pb[bo][:, :], modt[:, g1c + dcol:g1c + dcol + 1], xT[:, dcol, tbs(tb)],
                                  ALU.mult, ALU.add, r=[PK(bo), "mod", ("xT", dcol, tb)], w=[("xT", dcol, tb)])
                        return f
                    for tb in range(NB):
                        for dcol in range(DC):
                            items.append(o_item(tb, dcol))
                    return items

                def attn_steps(p):
                    s_ = p % NSET
                    ob = p % 2
                    steps = []
                    for hh in range(2):
                        for qb in range(NB):
                            nkt = 4 * qb + 4
                            ob_k = 3 + cnt["ps_o"] % 2
                            cnt["ps_o"] += 1
                            for kt in range(nkt):
                                steps.append((hh, qb, kt, nkt, ob_k, cnt["ps_s"] % 3, cnt["pt"] % NPT))
                                cnt["ps_s"] += 1
                                cnt["pt"] += 1

                    def emit_S(stp):
                        hh, qb, kt, nkt, ob_k, sk, pk = stp
                        hd = 2 * p + hh
                        qt, kt_ = qh[s_][hh], kh[s_][hh]
                        jj = kt - 4 * qb
                        n0 = max(0, jj) * 128
                        h.mm(pb[sk][:, n0:512], kt_[0:Kc, kt * 128:(kt + 1) * 128], qt[0:Kc, qb * 512 + n0:(qb + 1) * 512],
                             r=[("kh", s_, hh), ("qh", s_, hh)], w=[PK(sk)])
                        if fox:
                            bias = negcum[:, kt * 16 + hd:kt * 16 + hd + 1]
                            rk = [PK(sk), "negcum"]
                        else:
                            bias = zero_bias[:, 0:1]
                            rk = [PK(sk), "zero_bias"]
                        h.act(PT[pk][:, n0:512], pb[sk][:, n0:512], AF.Exp, bias=bias, scale=scale, r=rk, w=[("PT", pk)])
                        if jj >= 0:
                            h.tt("pool", PT[pk][:, n0:n0 + 128], PT[pk][:, n0:n0 + 128], mask[:], ALU.mult,
                                 r=[("PT", pk), "mask"], w=[("PT", pk)])

                    def emit_PV(stp):
                        hh, qb, kt, nkt, ob_k, sk, pk = stp
                        jj = kt - 4 * qb
                        n0 = max(0, jj) * 128
                        vl = Vp[s_][:, kt, 0:128] if hh == 0 else Vp[s_][:, kt, 64:192]
                        h.mm(pb[ob_k][:, n0:512], vl, PT[pk][:, n0:512], start=(kt == 0), stop=(kt == nkt - 1),
                             r=[("Vp", s_), ("PT", pk)], w=[PK(ob_k)])
                        if kt == nkt - 1:
                            rc = cnt["rec"] % 2
                            cnt["rec"] += 1
                            if hh == 0:
                                h.act(rec[rc][0:64, :], pb[ob_k][64:128, :], AF.Ln, r=[PK(ob_k)], w=[("rec", rc)])
                                h.act(rec[rc][0:64, :], rec[rc][0:64, :], AF.Exp, scale=-1.0, r=[("rec", rc)], w=[("rec", rc)])
                                h.tt("dve", oT[ob][0:64, tbs(qb)], pb[ob_k][0:64, :], rec[rc][0:64, :], ALU.mult,
                                     r=[PK(ob_k), ("rec", rc)], w=[("oT", ob, qb)])
                            else:
                                h.act(rec[rc][64:128, :], pb[ob_k][0:64, :], AF.Ln, r=[PK(ob_k)], w=[("rec", rc)])
                                h.act(rec[rc][64:128, :], rec[rc][64:128, :], AF.Exp, scale=-1.0, r=[("rec", rc)], w=[("rec", rc)])
                                h.tt("dve", oT[ob][64:128, tbs(qb)], pb[ob_k][64:128, :], rec[rc][64:128, :], ALU.mult,
                                     r=[PK(ob_k), ("rec", rc)], w=[("oT", ob, qb)])
                    return steps, emit_S, emit_PV

                LOOK = 2
                issue_weights(0)
                for it in proj_items(0):
                    it()
                bg = []
                for p in range(8):
                    if p + 1 < 8:
                        issue_weights(p + 1)
                        bg = bg + proj_items(p + 1)
                    steps, emit_S, emit_PV = attn_steps(p)
                    nst = len(steps)
                    nbg = len(bg)
                    done = 0
                    for i_ in range(nst + LOOK):
                        if i_ < nst:
                            emit_S(steps[i_])
                        if i_ >= LOOK:
                            emit_PV(steps[i_ - LOOK])
                        tgt = (nbg * (i_ + 1)) // (nst + LOOK)
                        while done < tgt:
                            bg[done]()
                            done += 1
                    while done < nbg:
                        bg[done]()
                        done += 1
                    bg = outproj_items(p)
                for it in bg:
                    it()
                h.end()

        def moe_phase(layer):
            modt = mod0 if layer == 0 else mod1
            g2c = 5 * DC
            with ExitStack() as st:
                selE = sb("selE", [16, NE, 128], BF16, st)
                combT = sb("combT", [16, S], BF16, st)
                with ExitStack() as st2:
                    rw = sb("rw", [128, DC, NE], F32, st2)
                    h32 = sb("h32", [128, DC, 512], F32, st2)
                    R = {n: sb("r_" + n, [128, 256], F32, st2) for n in ("sc", "sel", "a", "b", "c", "top2", "w")}
                    gs = sb("gs", [128, 64], F32, st2)
                    gtmp = sb("gtmp", [128, 64], F32, st2)
                    gmax = sb("gmax", [128, 16], F32, st2)
                    ohg = sb("ohg", [128, 64], F32, st2)
                    wsum = sb("wsum", [128, 16], F32, st2)
                    h.begin()
                    h.dma("sp", rw[:], router_w_d[:, :].rearrange("(dc p) e -> p dc e", p=128), w=["rw"], sem="rw")
                    for e_ in range(NE):
                        h.ts("pool", selE[:, e_, :], ones_f[0:16, :], ident[0:16, e_:e_ + 1], None, ALU.mult, r=["ones_f", "ident"], w=["selE"])

                    def router(tb, dc, t):
                        h.ts("dve", h32[:, dc, :], t[:], ncoef[:, dc:dc + 1], modt[:, 3 * DC + dc:3 * DC + dc + 1], ALU.mult, ALU.add,
                             r=[("t2", id(t)), "ncoef", "mod"], w=[("h32", dc)])
                        if dc == DC - 1:
                            for i in range(4):
                                ti_ = tb * 4 + i
                                for d2 in range(DC):
                                    h.mm(pb[2][:, ti_ * 16:(ti_ + 1) * 16], h32[:, d2, i * 128:(i + 1) * 128], rw[:, d2, :],
                                         start=(d2 == 0), stop=(d2 == DC - 1), r=[("h32", d2), "rw"], w=[PK(2)])

                    norm_phase(st2, 1 if layer == 0 else 4, modt, 3 * DC, 4 * DC, router=router)
                    sc, sel, A_, B_, C_, top2, w_ = (R[n] for n in ("sc", "sel", "a", "b", "c", "top2", "w"))
                    h.act(sc[:], pb[2][:, 0:256], AF.Sigmoid, r=[PK(2)], w=["r_sc"])
                    h.tt("dve", sel[:], sc[:], rb_rep[:], ALU.add, r=["r_sc", "rb_rep"], w=["r_sel"])
                    X = sel[:].rearrange("p (t e) -> p t e", e=4)
                    first = True
                    for (a, b) in ((0, 1), (0, 2), (0, 3), (1, 2), (1, 3), (2, 3)):
                        if first:
                            h.tt("dve", gs[:], X[:, :, a], X[:, :, b], ALU.add, r=["r_sel"], w=["gs"])
                            first = False
                        else:
                            h.tt("dve", gtmp[:], X[:, :, a], X[:, :, b], ALU.add, r=["r_sel"], w=["gtmp"])
                            h.tt("dve", gs[:], gs[:], gtmp[:], ALU.max, r=["gs", "gtmp"], w=["gs"])
                    G4 = gs[:].rearrange("p (t g) -> p t g", g=4)
                    h.tt("dve", gmax[:], G4[:, :, 0], G4[:, :, 1], ALU.max, r=["gs"], w=["gmax"])
                    h.tt("dve", gmax[:], gmax[:], G4[:, :, 2], ALU.max, r=["gs", "gmax"], w=["gmax"])
                    h.tt("dve", gmax[:], gmax[:], G4[:, :, 3], ALU.max, r=["gs", "gmax"], w=["gmax"])
                    O4 = ohg[:].rearrange("p (t g) -> p t g", g=4)
                    for g in range(4):
                        h.tt("dve", O4[:, :, g], G4[:, :, g], gmax[:], ALU.is_equal, r=["gs", "gmax"], w=["ohg"])
                    T2 = top2[:].rearrange("p (t e) -> p t e", e=4)
                    A3 = A_[:].rearrange("p (t e) -> p t e", e=4)
                    B3 = B_[:].rearrange("p (t e) -> p t e", e=4)
                    for e_ in range(4):
                        oth = [x for x in range(4) if x != e_]
                        h.tt("dve", A3[:, :, e_], X[:, :, oth[0]], X[:, :, e_], ALU.is_gt, r=["r_sel"], w=["r_a"])
                        h.tt("dve", B3[:, :, e_], X[:, :, oth[1]], X[:, :, e_], ALU.is_gt, r=["r_sel"], w=["r_b"])
                        h.tt("dve", A3[:, :, e_], A3[:, :, e_], B3[:, :, e_], ALU.add, r=["r_a", "r_b"], w=["r_a"])
                        h.tt("dve", B3[:, :, e_], X[:, :, oth[2]], X[:, :, e_], ALU.is_gt, r=["r_sel"], w=["r_b"])
                        h.tt("dve", A3[:, :, e_], A3[:, :, e_], B3[:, :, e_], ALU.add, r=["r_a", "r_b"], w=["r_a"])
                        h.ts("dve", T2[:, :, e_], A3[:, :, e_], 1.5, None, ALU.is_lt, r=["r_a"], w=["r_top2"])
                        h.tt("dve", T2[:, :, e_], T2[:, :, e_], ohg[:], ALU.mult, r=["r_top2", "ohg"], w=["r_top2"])
                    h.tt("dve", w_[:], sc[:], top2[:], ALU.mult, r=["r_sc", "r_top2"], w=["r_w"])
                    h.red(wsum[:], w_[:].rearrange("p (t e) -> p t e", e=16), ALU.add, r=["r_w"], w=["wsum"])
                    h.rcp(wsum[:], wsum[:], r=["wsum"], w=["wsum"])
                    for i in range(NT):
                        h.ts("dve", C_[:, i * 16:(i + 1) * 16], w_[:, i * 16:(i + 1) * 16], wsum[:, i:i + 1], None, ALU.mult,
                             r=["r_w", "wsum"], w=["r_c"])
                    for i in range(NT):
                        bk = 4 + (i // 4) % 2
                        h.tr(pb[bk][0:16, (i % 4) * 128:(i % 4 + 1) * 128], C_[:, i * 16:(i + 1) * 16], ident[:],
                             r=["r_c", "ident"], w=[PK(bk)])
                        if i % 4 == 3:
                            h.cp("act", combT[:, (i // 4) * 512:(i // 4 + 1) * 512], pb[bk][0:16, :], r=[PK(bk)], w=["combT"])
                    h.end()
                if layer == 1 and globals().get("_MOE1_SKIP_B", False):
                    return
                wg = [sb("wg%d" % i, [128, DC, 512], BF16, st) for i in range(2)]
                wu = [sb("wu%d" % i, [128, DC, 512], BF16, st) for i in range(2)]
                wd = [sb("wd%d" % i, [128, 4, D], BF16, st) for i in range(2)]
                aT = [sb("aT%d" % i, [128, 4, 512], BF16, st) for i in range(2)]
                sg = [sb("sg%d" % i, [128, 512], F32, st) for i in range(3)]
                cmb = [sb("cmb%d" % i, [128, 512], F32, st) for i in range(2)]
                h.begin()
                n_g = 0
                n_y = 0
                n_it = 0
                mod_items = []
                if layer == 0:
                    wmc = [sb("wmc%d" % i, [128, 1024], BF16, st) for i in range(2)]
                    for (wsrc, nblk, modt_, mkey) in ((w_mod_d[1], 6, mod1, "mod1acc"), (kv_w_mod_d, 2, modkv, "modkvacc")):
                        for blk in range(nblk):
                            for kc in range(DC):
                                mod_items.append((wsrc, blk, kc, modt_, mkey))

                def issue_mod_dma(n):
                    wsrc, blk, kc, modt_, mkey = mod_items[n]
                    wkey = ("wmc", n % 2)
                    h.dma("pool", wmc[n % 2][:], wsrc[kc * 128:(kc + 1) * 128, blk * 1024:(blk + 1) * 1024], w=[wkey], sem=wkey)

                if mod_items:
                    issue_mod_dma(0)

                def run_mod_item(n):
                    wsrc, blk, kc, modt_, mkey = mod_items[n]
                    wt = wmc[n % 2]
                    wkey = ("wmc", n % 2)
                    if n + 1 < len(mod_items):
                        issue_mod_dma(n + 1)
                    for j in range(8):
                        h.mm(pb[0][:, j:j + 1], wt[:, j * 128:(j + 1) * 128], cact[:, kc:kc + 1], r=[wkey, "cact"], w=[PK(0)])
                    h.tt("dve", modt_[:, blk * 8:(blk + 1) * 8], modt_[:, blk * 8:(blk + 1) * 8], pb[0][:, 0:8], ALU.add,
                         r=[PK(0), mkey], w=[mkey])

                def issue_expert(e2):
                    w2 = e2 % 2
                    h.dma("pool", wg[w2][:], wg_d[layer][e2].rearrange("(dc p) f -> p dc f", p=128), w=[("wg", w2)], sem=("wg", w2))
                    h.dma("pool", wu[w2][:], wu_d[layer][e2].rearrange("(dc p) f -> p dc f", p=128), w=[("wu", w2)], sem=("wu", w2))
                    h.dma("pool", wd[w2][:], wd_d[layer][e2].rearrange("(fc p) d -> p fc d", p=128), w=[("wd", w2)], sem=("wd", w2))

                issue_expert(0)
                for e_ in range(NE):
                    wb = e_ % 2
                    if e_ + 1 < NE:
                        issue_expert(e_ + 1)
                    for tb in range(NB):
                        ab = n_it % 2
                        n_it += 1
                        h.mm(pb[0][:, :], selE[:, e_, :], combT[:, tbs(tb)], r=["selE", "combT"], w=[PK(0)])
                        h.cp("act", cmb[ab][:], pb[0][:, :], r=[PK(0)], w=[("cmb", ab)])
                        for fc in range(4):
                            bg = 1 + n_g % 2
                            bu = 3 + n_g % 2
                            sgi = n_g % 3
                            n_g += 1
                            for dc in range(DC):
                                h.mm(pb[bg][:, :], wg[wb][:, dc, fc * 128:(fc + 1) * 128], hT[:, dc, tbs(tb)],
                                     start=(dc == 0), stop=(dc == DC - 1), r=[("wg", wb), ("hT", tb)], w=[PK(bg)])
                            for dc in range(DC):
                                h.mm(pb[bu][:, :], wu[wb][:, dc, fc * 128:(fc + 1) * 128], hT[:, dc, tbs(tb)],
                                     start=(dc == 0), stop=(dc == DC - 1), r=[("wu", wb), ("hT", tb)], w=[PK(bu)])
                            h.act(sg[sgi][:], pb[bg][:, :], AF.Silu, r=[PK(bg)], w=[("sg", sgi)])
                            h.tt("dve", sg[sgi][:], sg[sgi][:], cmb[ab][:], ALU.mult, r=[("sg", sgi), ("cmb", ab)], w=[("sg", sgi)])
                            h.tt("dve", aT[ab][:, fc, :], pb[bu][:, :], sg[sgi][:], ALU.mult, r=[PK(bu), ("sg", sgi)], w=[("aT", ab, fc)])
                            if fc == 1 and n_it - 1 < len(mod_items):
                                run_mod_item(n_it - 1)
                        for dcol in range(DC):
                            by = 5 + n_y % 3
                            n_y += 1
                            for fc in range(4):
                                h.mm(pb[by][:, :], wd[wb][:, fc, dcol * 128:(dcol + 1) * 128], aT[ab][:, fc, :],
                                     start=(fc == 0), stop=(fc == 3), r=[("wd", wb), ("aT", ab, fc)], w=[PK(by)])
                            h.stt("dve", xT[:, dcol, tbs(tb)], pb[by][:, :], modt[:, g2c + dcol:g2c + dcol + 1], xT[:, dcol, tbs(tb)],
                                  ALU.mult, ALU.add, r=[PK(by), "mod", ("xT", dcol, tb)], w=[("xT", dcol, tb)])
                h.end()

        def kv_phase():
            with ExitStack() as st:
                wkd = sb("wkd", [128, DC, 256], BF16, st)
                wkr = sb("wkr", [128, DC, 128], BF16, st)
                craw = sb("kraw", [128, 2, 512], F32, st)
                csq = [sb("ksq%d" % i, [128, 512], BF16, st) for i in range(2)]
                crs = sb("krs", [128, 512], F32, st)
                ra = [sb("kra%d" % i, [128, 512], F32, st) for i in range(2)]
                rb = [sb("krb%d" % i, [128, 512], F32, st) for i in range(2)]
                h.begin()
                h.memset("pool", wkr[:, :, 0:64], 0.0, w=["wkr_z"])
                dn = kv_w_down_d[:, :].rearrange("(dc p) c -> p dc c", p=128)
                h.dma("pool", wkd[:], dn[:, :, 0:256], w=["wkd"], sem="wkd")
                h.dma("pool", wkr[:, :, 64:96], dn[:, :, 256:288], w=["wkr0"], sem="wkr0")
                h.dma("pool", wkr[:, :, 96:112], dn[:, :, 272:288], w=["wkr1"], sem="wkr1")
                h.dma("pool", wkr[:, :, 112:128], dn[:, :, 256:272], w=["wkr2"], sem="wkr2")
                norm_phase(st, 2, modkv, 0, DC)
                for tb in range(NB):
                    for c in range(2):
                        bk = 2 + c
                        for dc in range(DC):
                            h.mm(pb[bk][:, :], wkd[:, dc, c * 128:(c + 1) * 128], hT[:, dc, tbs(tb)], start=(dc == 0), stop=(dc == DC - 1),
                                 r=["wkd", ("hT", tb)], w=[PK(bk)])
                        h.cp("act", craw[:, c, :], pb[bk][:, :], r=[PK(bk)], w=[("kraw", c)])
                        h.tt("dve", csq[c][:], craw[:, c, :], craw[:, c, :], ALU.mult, r=[("kraw", c)], w=[("ksq", c)])
                        h.mm(pb[4][:, :], ones_b[:, :], csq[c][:], start=(c == 0), stop=(c == 1), r=[("ksq", c), "ones_b"], w=[PK(4)])
                    h.act(crs[:], pb[4][:, :], AF.Ln, bias=epst[:, 0:1], scale=1.0 / 256, r=[PK(4), "epst"], w=["krs"])
                    h.act(crs[:], crs[:], AF.Exp, scale=-0.5, r=["krs"], w=["krs"])
                    for c in range(2):
                        h.stt("dve", ckvT[:, c, tbs(tb)], craw[:, c, :], lat_g[:, c:c + 1], crs[:], ALU.mult, ALU.mult,
                              r=[("kraw", c), "krs", "lat_g"], w=["ckvT"])
                    bk = 5 + tb % 2
                    for dc in range(DC):
                        h.mm(pb[bk][:, :], wkr[:, dc, :], hT[:, dc, tbs(tb)], start=(dc == 0), stop=(dc == DC - 1),
                             r=["wkr_z", "wkr0", "wkr1", "wkr2", ("hT", tb)], w=[PK(bk)])
                    cs_ = tbs(tb)
                    a_, b_ = ra[tb % 2], rb[tb % 2]
                    h.tt("dve", a_[64:128, :], pb[bk][64:128, :], csT[64:128, cs_], ALU.mult, r=[PK(bk), "rope_tab", "rope_tab2"], w=[("kra", tb % 2)])
                    h.cp("act", b_[64:96, :], a_[96:128, :], r=[("kra", tb % 2)], w=[("krb", tb % 2)])
                    h.tt("dve", krope[64:96, cs_], a_[64:96, :], b_[64:96, :], ALU.add, r=[("kra", tb % 2), ("krb", tb % 2)], w=[("krope", tb)])
                    h.cp("act", krope[96:128, cs_], krope[64:96, cs_], r=[("krope", tb)], w=[("kropeB", tb)])
                norm_phase(st, 3, mod1, 0, DC)
                wdq = sb("wdq", [128, DC, 768], BF16, st)
                craw = sb("craw", [128, 6, 512], F32, st)
                csq = [sb("csq%d" % i, [128, 512], BF16, st) for i in range(2)]
                crs = sb("crs", [128, 512], F32, st)
                h.dma("pool", wdq[:], b_w_dq_d[:, :].rearrange("(dc p) c -> p dc c", p=128), w=["wdq"], sem="wdq")
                for tb in range(NB):
                    for c in range(6):
                        bk = 2 + c % 2
                        for dc in range(DC):
                            h.mm(pb[bk][:, :], wdq[:, dc, c * 128:(c + 1) * 128], hT[:, dc, tbs(tb)],
                                 start=(dc == 0), stop=(dc == DC - 1), r=["wdq", ("hT", tb)], w=[PK(bk)])
                        h.cp("act", craw[:, c, :], pb[bk][:, :], r=[PK(bk)], w=[("craw", c)])
                        q_ = csq[c % 2]
                        h.tt("dve", q_[:], craw[:, c, :], craw[:, c, :], ALU.mult, r=[("craw", c)], w=[("csq", c % 2)])
                        h.mm(pb[4][:, :], ones_b[:, :], q_[:], start=(c == 0), stop=(c == 5), r=[("csq", c % 2), "ones_b"], w=[PK(4)])
                    h.act(crs[:], pb[4][:, :], AF.Ln, bias=epst[:, 0:1], scale=1.0 / 768, r=[PK(4), "epst"], w=["crs"])
                    h.act(crs[:], crs[:], AF.Exp, scale=-0.5, r=["crs"], w=["crs"])
                    for c in range(6):
                        h.stt("dve", hT[:, c, tbs(tb)], craw[:, c, :], q_g[:, c:c + 1], crs[:], ALU.mult, ALU.mult,
                              r=[("craw", c), "crs", "q_g"], w=[("hT", tb)])
                h.end()

        def final_phase(do_norm):
            with ExitStack() as st:
                sq = [sb("fsq%d" % i, [128, 512], BF16, st) for i in range(3)]
                rs = [sb("frs%d" % i, [128, 512], F32, st) for i in range(2)]
                ob = [sb("fob%d" % i, [128, 512], F32, st) for i in range(4)]
                h.begin()
                for _i in range(globals().get("_DUMMY", 0)):
                    _de = globals().get("_DUMMY_ENG", "pe")
                    if _de == "pe":
                        h.mm(pb[7][:, 0:16], ones_b[:, :], ones_b[:, 0:16], r=["ones_b"], w=[PK(7)])
                    elif _de == "dve":
                        h.memset("dve", sq[0][:, 0:8], 0.0, w=[])
                    elif _de == "actbig":
                        h.act(hT[:, 0, :], xT[:, 0, :], AF.Copy, r=[], w=[])
                    elif _de == "pooldma":
                        h.dma("pool", sq[2][:, 0:16], a_w_o_d[0:128, 0:16], w=["dummy_dma"], sem="dummy_dma")
                    else:
                        h.act(sq[1][:, 0:8], ones_b[:, 0:8], AF.Copy, r=[], w=[])
                n = 0
                for tb in range(NB):
                    if do_norm:
                        pss = pb[tb % 2]
                        for dc in range(DC):
                            q = sq[n % 3]
                            n += 1
                            h.act(q[:], xT[:, dc, tbs(tb)], AF.Square, r=[("xT", dc, tb)], w=[("sq", id(q))])
                            h.mm(pss[:, :], ones_b[:, :], q[:], start=(dc == 0), stop=(dc == DC - 1), r=[("sq", id(q)), "ones_b"], w=[PK(tb % 2)])
                        r_ = rs[tb % 2]
                        h.act(r_[:], pss[:, :], AF.Ln, bias=epst[:, 0:1], scale=1.0 / D, r=[PK(tb % 2), "epst"], w=[("rs", tb % 2)])
                        h.act(r_[:], r_[:], AF.Exp, scale=-0.5, r=[("rs", tb % 2)], w=[("rs", tb % 2)])
                    for dc in range(DC):
                        o = ob[n % 4]
                        ok = ("ob", n % 4)
                        n += 1
                        if do_norm:
                            h.stt("dve", o[:], xT[:, dc, tbs(tb)], gvec[:, 5 * DC + dc:5 * DC + dc + 1], r_[:], ALU.mult, ALU.mult,
                                  r=[("xT", dc, tb), "gvec", ("rs", tb % 2)], w=[ok])
                        else:
                            h.cp("dve", o[:], xT[:, dc, tbs(tb)], r=[("xT", dc, tb)], w=[ok])
                        h.dma("sp", outT_d[dc * 128:(dc + 1) * 128, tbs(tb)], o[:], r=[ok], w=[("outd", dc, tb)], sem=ok)
                h.end()

        fns = [lambda: attn_phase(0), lambda: moe_phase(0), kv_phase, lambda: attn_phase(1), lambda: moe_phase(1)]
        for i_ in range(5):
            if lo <= i_ <= hi and i_ not in globals().get("_SKIP", []):
                fns[i_]()
        for _i in range(globals().get("_DUMMY_BLOCKS", 0)):
            h.begin()
            h.memset("dve", ncoef[:, 8:9], 0.0, w=["x"])
            h.end()
        final_phase(hi >= 5)
    return nc


_CACHE = {}


def _lay(v, k):
    return np.ascontiguousarray(np.asarray(v, np.float32).reshape(k, 128).T)


def kernel(x, c, positions, a_norm_g, a_w_in, a_b_f, a_w_o, kv_norm_g, kv_w_mod, kv_b_mod,
           kv_w_down, kv_latent_g, kv_w_up, b_norm_g, b_w_dq, b_q_norm_g, b_w_uq, b_w_o,
           w_mod, b_mod, ffn_norm_g, router_w, router_bias, exp_w_gate, exp_w_up, exp_w_down,
           final_norm_g):
    f = lambda a: np.ascontiguousarray(np.asarray(a, np.float32))
    x = f(x)
    B = x.shape[0]
    if "nc" not in _CACHE:
        _CACHE["nc"] = build_program(0, 5)
    gvec = np.concatenate([_lay(a_norm_g[0], 8), _lay(ffn_norm_g[0], 8), _lay(kv_norm_g, 8), _lay(b_norm_g[0], 8),
                           _lay(ffn_norm_g[1], 8), _lay(final_norm_g, 8)], axis=1)
    bmod = np.concatenate([_lay(b_mod[0], 48), _lay(b_mod[1], 48), _lay(kv_b_mod, 16)], axis=1)
    bf_rep = np.ascontiguousarray(np.tile(np.asarray(a_b_f[0], np.float32)[None, :], (128, 16)))
    rb_rep = np.ascontiguousarray(np.tile(np.asarray(router_bias, np.float32)[None, :], (128, 16)))
    half = 16
    inv_freq = (10000.0 ** (-np.arange(half, dtype=np.float32) / half)).astype(np.float32)
    invf = np.zeros((128, 2), np.float32)
    for p in range(128):
        invf[p, 0] = inv_freq[p % 16]
        invf[p, 1] = -1.0 if (p % 32) < 16 else 1.0
    shared = {
        "invf": invf, "gvec": np.ascontiguousarray(gvec), "bmod": np.ascontiguousarray(bmod), "bf_rep": bf_rep, "rb_rep": rb_rep,
        "lat_g": _lay(kv_latent_g, 2), "q_g": _lay(b_q_norm_g[0], 6),
        "w_mod": f(w_mod), "kv_w_mod": f(kv_w_mod), "a_w_in": f(a_w_in[0]), "a_w_o": f(a_w_o[0]),
        "kv_w_down": f(kv_w_down), "kv_w_up": f(kv_w_up), "b_w_dq": f(b_w_dq[0]), "b_w_uq": f(b_w_uq[0]),
        "b_w_o": f(b_w_o[0]), "router_w": f(router_w),
        "exp_w_gate0": f(exp_w_gate[0]), "exp_w_gate1": f(exp_w_gate[1]), "exp_w_up0": f(exp_w_up[0]), "exp_w_up1": f(exp_w_up[1]),
        "exp_w_down0": f(exp_w_down[0]), "exp_w_down1": f(exp_w_down[1]),
    }
    in_maps = []
    for b in range(B):
        m = dict(shared)
        m["xT"] = np.ascontiguousarray(x[b].T)
        m["c_l"] = _lay(c[b], 8)
        m["pos"] = np.ascontiguousarray(np.asarray(positions[b], np.int32)[None, :])
        in_maps.append(m)
    res = run_bass_kernel_spmd(_CACHE["nc"], in_maps, core_ids=list(range(B)))
    out = np.stack([np.asarray(r["outT"], np.float32).T for r in res.results], axis=0)
    return np.ascontiguousarray(out)
```

```python
import numpy as np
from contextlib import ExitStack
import concourse.bass as bass
import concourse.mybir as mybir
from concourse.bass_utils import run_bass_kernel_spmd

F32 = mybir.dt.float32
BF16 = mybir.dt.bfloat16
I32 = mybir.dt.int32
AF = mybir.ActivationFunctionType
ALU = mybir.AluOpType
AX = mybir.AxisListType

S = 2048
D = 1024
NT = 16
NB = 4
DC = 8
NE = 16
EPS = 1e-6
SAME_ENGINE_SYNC = True
STOP_AFTER = "final"


class _Op:
    __slots__ = ("eng", "idx", "fn", "deps", "need_sig", "sig_val", "dma", "dma_val")

    def __init__(self, eng, idx, fn, dma):
        self.eng = eng
        self.idx = idx
        self.fn = fn
        self.deps = []
        self.need_sig = False
        self.sig_val = 0
        self.dma = dma
        self.dma_val = 0


class Prog:
    ENGS = ("pe", "act", "dve", "pool", "sp")

    def __init__(self, nc):
        self.nc = nc
        self.ops = {e: [] for e in self.ENGS}
        self.last_w = {}
        self.readers = {}
        self.dma_cnt = {}

    def op(self, eng, fn, r=(), w=(), dma=None):
        o = _Op(eng, len(self.ops[eng]), fn, dma)
        deps = {}

        def add(d):
            if d.dma is not None:
                key = ("d", d.dma)
                if key not in deps or deps[key].dma_val < d.dma_val:
                    deps[key] = d
            else:
                if d.eng == eng and (eng == "pe" or not SAME_ENGINE_SYNC):
                    return
                key = ("c", d.eng)
                if key not in deps or deps[key].idx < d.idx:
                    deps[key] = d

        for k in r:
            d = self.last_w.get(k)
            if d is not None:
                add(d)
        for k in w:
            d = self.last_w.get(k)
            if d is not None:
                add(d)
            rd = self.readers.get(k)
            if rd:
                for d in rd.values():
                    add(d)
        for d in deps.values():
            if d.dma is None:
                d.need_sig = True
            o.deps.append(d)
        for k in w:
            self.last_w[k] = o
            self.readers[k] = {}
        for k in r:
            rd = self.readers.setdefault(k, {})
            if dma is not None:
                rd[("d", dma, o.idx)] = o
            else:
                rd[("c", eng)] = o
        if dma is not None:
            c = self.dma_cnt.get(dma, 0) + 1
            self.dma_cnt[dma] = c
            o.dma_val = 16 * c
        self.ops[eng].append(o)
        return o

    def emit(self, pool, final_waits=()):
        nc = self.nc
        for e in self.ENGS:
            c = 0
            for o in self.ops[e]:
                if o.dma is None and o.need_sig:
                    c += 1
                    o.sig_val = pool.ebase[e] + c
            pool.ebase[e] += c
        swk = set()
        for o in self.ops["pool"]:
            if o.dma is not None:
                swk.add(o.dma)
        slot = {}
        n_sw, n_hw = 0, 0
        for k in self.dma_cnt:
            if k in swk:
                slot[k] = n_sw
                n_sw += 1
            else:
                slot[k] = pool.n_sw + n_hw
                n_hw += 1
        assert n_sw <= pool.n_sw and n_hw <= len(pool.dsem) - pool.n_sw, (n_sw, n_hw)
        for e in self.ENGS:
            for o in self.ops[e]:
                if o.dma is not None:
                    o.dma_val += pool.dbase[slot[o.dma]]
        esem = pool.esem
        dsem = {k: pool.dsem[i] for k, i in slot.items()}
        all_final = {k: pool.dbase[slot[k]] + 16 * self.dma_cnt[k] for k in slot}
        for k, i in slot.items():
            pool.dbase[i] += 16 * self.dma_cnt[k]
        with nc.Block() as block:

            def run(e, engobj):
                waited = {}
                for o in self.ops[e]:
                    ws = []
                    for d in o.deps:
                        if d.dma is not None:
                            sem, val, sk = dsem[d.dma], d.dma_val, ("d", d.dma)
                        else:
                            sem, val, sk = esem[d.eng], d.sig_val, ("c", d.eng)
                        if waited.get(sk, 0) >= val:
                            continue
                        waited[sk] = val
                        ws.append((sem, val))
                    for sem, val in ws[:-1]:
                        engobj.wait_ge(sem, val)
                    ins = o.fn(engobj)
                    if ws:
                        ins._wait_ge(ws[-1][0], ws[-1][1])
                    if o.dma is not None:
                        ins.then_inc(dsem[o.dma], 16)
                    elif o.need_sig:
                        ins.then_inc(esem[e], 1)
                if e == "sp":
                    for k, v in all_final.items():
                        engobj.wait_ge(dsem[k], v)

            @block.tensor
            def _(pe):
                run("pe", pe)

            @block.scalar
            def _(act):
                run("act", act)

            @block.vector
            def _(dve):
                run("dve", dve)

            @block.gpsimd
            def _(pool_):
                run("pool", pool_)

            @block.sync
            def _(sp):
                run("sp", sp)


class SemPool:
    def __init__(self, nc, st, n_dma=54, n_sw=32):
        self.n_sw = n_sw
        self.esem = {e: st.enter_context(nc.semaphore("g_" + e)) for e in Prog.ENGS}
        self.ebase = {e: 0 for e in Prog.ENGS}
        self.dsem = [st.enter_context(nc.semaphore("gd%d" % i)) for i in range(n_dma)]
        self.dbase = [0] * n_dma


class H:
    def __init__(self, nc):
        self.nc = nc
        self.P = None
        self.pool = None

    def begin(self):
        self.P = Prog(self.nc)

    def end(self, final_waits=()):
        self.P.emit(self.pool, final_waits)
        self.P = None

    def mm(self, out, lhsT, rhs, start=True, stop=True, r=(), w=()):
        self.P.op("pe", lambda e: e.matmul(out, lhsT, rhs, start=start, stop=stop), r, w)

    def tr(self, out, in_, ident, r=(), w=()):
        self.P.op("pe", lambda e: e.transpose(out, in_, ident), r, w)

    def act(self, out, in_, func, bias=0.0, scale=1.0, r=(), w=()):
        self.P.op("act", lambda e: e.activation(out, in_, func, bias=bias, scale=scale), r, w)

    def tt(self, eng, out, in0, in1, op, r=(), w=()):
        self.P.op(eng, lambda e: e.tensor_tensor(out, in0, in1, op), r, w)

    def ts(self, eng, out, in0, s1, s2, op0, op1=None, r=(), w=()):
        if op1 is None:
            self.P.op(eng, lambda e: e.tensor_scalar(out, in0, s1, None, op0), r, w)
        else:
            self.P.op(eng, lambda e: e.tensor_scalar(out, in0, s1, s2, op0, op1), r, w)

    def stt(self, eng, out, in0, sc, in1, op0, op1, r=(), w=()):
        self.P.op(eng, lambda e: e.scalar_tensor_tensor(out, in0, sc, in1, op0, op1), r, w)

    def cp(self, eng, out, in_, r=(), w=()):
        if eng == "act":
            self.P.op("act", lambda e: e.copy(out, in_), r, w)
        else:
            self.P.op(eng, lambda e: e.tensor_copy(out, in_), r, w)

    def rcp(self, out, in_, r=(), w=()):
        self.P.op("dve", lambda e: e.reciprocal(out, in_), r, w)

    def red(self, out, in_, op, r=(), w=()):
        self.P.op("dve", lambda e: e.tensor_reduce(out, in_, AX.X, op), r, w)

    def memset(self, eng, ap, val, w=()):
        self.P.op(eng, lambda e: e.memset(ap, val), (), w)

    def dma(self, q, out, in_, r=(), w=(), sem=None):
        self.P.op(q, lambda e: e.dma_start(out=out, in_=in_), r, w, dma=sem)


def build_program(lo=0, hi=5):
    nc = bass.Bass("TRN2", target_bir_lowering=False)
    h = H(nc)

    def din(name, shape, dt=F32):
        return nc.dram_tensor(name, list(shape), dt, kind="ExternalInput").ap()

    xT_d = din("xT", [D, S])
    c_d = din("c_l", [128, DC])
    pos_d = din("pos", [1, S], I32)
    invf_d = din("invf", [128, 2])
    gvec_d = din("gvec", [128, 6 * DC])
    bmod_d = din("bmod", [128, 112])
    bf_d = din("bf_rep", [128, 256])
    rb_d = din("rb_rep", [128, 256])
    lg_d = din("lat_g", [128, 2])
    qg_d = din("q_g", [128, 6])
    w_mod_d = din("w_mod", [2, D, 6 * D])
    kv_w_mod_d = din("kv_w_mod", [D, 2 * D])
    a_w_in_d = din("a_w_in", [D, 3 * D + 16])
    a_w_o_d = din("a_w_o", [D, D])
    kv_w_down_d = din("kv_w_down", [D, 288])
    kv_w_up_d = din("kv_w_up", [256, 2048])
    b_w_dq_d = din("b_w_dq", [D, 768])
    b_w_uq_d = din("b_w_uq", [768, 1536])
    b_w_o_d = din("b_w_o", [D, D])
    router_w_d = din("router_w", [D, NE])
    wg_d = [din("exp_w_gate%d" % l_, [NE, D, 512]) for l_ in range(2)]
    wu_d = [din("exp_w_up%d" % l_, [NE, D, 512]) for l_ in range(2)]
    wd_d = [din("exp_w_down%d" % l_, [NE, 512, D]) for l_ in range(2)]
    outT_d = nc.dram_tensor("outT", [D, S], F32, kind="ExternalOutput").ap()

    with ExitStack() as G:
        _uid = [0]

        def sb(name, shape, dt, st=G):
            _uid[0] += 1
            return st.enter_context(nc.sbuf_tensor("s%d_%s" % (_uid[0], name), list(shape), dt))

        pb = [G.enter_context(nc.psum_tensor("pb%d" % i, [128, 512], F32)) for i in range(8)]
        h.pool = SemPool(nc, G)

        def PK(i):
            return ("ps", i)

        xT = sb("xT", [128, DC, S], F32)
        hT = sb("hT", [128, DC, S], BF16)
        ident = sb("ident", [128, 128], F32)
        ones_f = sb("ones_f", [128, 128], F32)
        ones_b = sb("ones_b", [128, 128], BF16)
        triu_f = sb("triu_f", [128, 128], F32)
        triu_b = sb("triu_b", [128, 128], BF16)
        cmask_b = sb("cmask_b", [128, 128], BF16)
        epst = sb("epst", [128, 1], F32)
        mod0 = sb("mod0", [128, 48], F32)
        mod1 = sb("mod1", [128, 48], F32)
        modkv = sb("modkv", [128, 16], F32)
        gvec = sb("gvec_s", [128, 6 * DC], F32)
        bmod = sb("bmod_s", [128, 112], F32)
        bf_rep = sb("bf_rep_s", [128, 256], F32)
        rb_rep = sb("rb_rep_s", [128, 256], F32)
        lat_g = sb("lat_g_s", [128, 2], F32)
        q_g = sb("q_g_s", [128, 6], F32)
        csT = sb("csT", [128, S], BF16)
        ckvT = sb("ckvT", [128, 2, S], BF16)
        krope = sb("krope", [128, S], BF16)
        ncoef = sb("ncoef", [128, 2 * DC], F32)
        cact = sb("cact", [128, DC], BF16)

        def tbs(tb):
            return slice(tb * 512, (tb + 1) * 512)

        def norm_phase(st, gidx, modt, sh0, sc0, nchunks=DC, src=None, dst=None, dst_keyf=None,
                       router=None):
            sq = [sb("sq%d" % i, [128, 512], BF16, st) for i in range(4)]
            rs = [sb("rs%d" % i, [128, 512], F32, st) for i in range(2)]
            t2 = [sb("t2_%d" % i, [128, 512], F32, st) for i in range(3)]
            h.stt("dve", ncoef[:, 0:DC], modt[:, sc0:sc0 + DC], 1.0, gvec[:, gidx * DC:(gidx + 1) * DC],
                  ALU.add, ALU.mult, r=["mod", "gvec"], w=["ncoef"])
            n = 0
            for tb in range(NB):
                pss = pb[tb % 2]
                for dc in range(DC):
                    q = sq[n % 4]
                    n += 1
                    if dc % 2 == 0:
                        h.act(q[:], xT[:, dc, tbs(tb)], AF.Square, r=[("xT", dc, tb)], w=[("sq", id(q))])
                    else:
                        h.tt("dve", q[:], xT[:, dc, tbs(tb)], xT[:, dc, tbs(tb)], ALU.mult, r=[("xT", dc, tb)], w=[("sq", id(q))])
                    h.mm(pss[:, :], ones_b[:, :], q[:], start=(dc == 0), stop=(dc == DC - 1),
                         r=[("sq", id(q)), "ones_b"], w=[PK(tb % 2)])
                r_ = rs[tb % 2]
                h.act(r_[:], pss[:, :], AF.Ln, bias=epst[:, 0:1], scale=1.0 / D, r=[PK(tb % 2), "epst"], w=[("rs", tb % 2)])
                h.act(r_[:], r_[:], AF.Exp, scale=-0.5, r=[("rs", tb % 2)], w=[("rs", tb % 2)])
                for dc in range(DC):
                    t = t2[n % 3]
                    n += 1
                    h.tt("dve", t[:], xT[:, dc, tbs(tb)], r_[:], ALU.mult, r=[("xT", dc, tb), ("rs", tb % 2)], w=[("t2", id(t))])
                    h.act(hT[:, dc, tbs(tb)], t[:], AF.Identity, bias=modt[:, sh0 + dc:sh0 + dc + 1],
                          scale=ncoef[:, dc:dc + 1], r=[("t2", id(t)), "ncoef", "mod"], w=[("hT", tb)])
                    if router is not None:
                        router(tb, dc, t)

        with ExitStack() as st:
            cs = sb("c_s", [128, DC], F32, st)
            wm = [sb("wm%d" % i, [128, 6 * D], BF16, st) for i in range(3)]
            posi = sb("posi", [128, S], I32, st)
            invf = sb("invf_s", [128, 2], F32, st)
            sinT = sb("sinT", [128, S], BF16, st)
            cosT = sb("cosT", [128, S], BF16, st)
            ang = sb("ang", [128, S], F32, st)
            ta = sb("ta", [128, S], F32, st)
            tb_ = sb("tb_", [128, S], F32, st)
            ti = sb("ti", [128, S], I32, st)
            h.begin()
            h.dma("sp", cs[:], c_d[:, :], w=["cs"], sem="cs")
            h.dma("sp", gvec[:], gvec_d[:, :], w=["gvec"], sem="ld_gvec")
            h.dma("sp", bmod[:], bmod_d[:, :], w=["bmod"], sem="ld_bmod")
            h.dma("sp", bf_rep[:], bf_d[:, :], w=["bf_rep"], sem="ld_bf_rep")
            h.dma("sp", rb_rep[:], rb_d[:, :], w=["rb_rep"], sem="ld_rb_rep")
            h.dma("sp", lat_g[:], lg_d[:, :], w=["lat_g"], sem="ld_lat_g")
            h.dma("sp", q_g[:], qg_d[:, :], w=["q_g"], sem="ld_q_g")
            h.dma("sp", invf[:], invf_d[:, :], w=["invf"], sem="ld_invf")
            h.dma("sp", posi[:], pos_d.partition_broadcast(128), w=["posi"], sem="ld_posi")
            h.memset("pool", ones_f[:], 1.0, w=["ones_f"])
            h.memset("pool", ones_b[:], 1.0, w=["ones_b"])
            h.memset("pool", epst[:], EPS, w=["epst"])
            h.P.op("pool", lambda e: e.affine_select(out=ident[:], in_=ones_f[:], pattern=[[-1, 128]], compare_op=ALU.is_equal,
                                                     fill=0.0, base=0, channel_multiplier=1), ["ones_f"], ["ident"])
            h.P.op("pool", lambda e: e.affine_select(out=triu_f[:], in_=ones_f[:], pattern=[[1, 128]], compare_op=ALU.is_ge,
                                                     fill=0.0, base=0, channel_multiplier=-1), ["ones_f"], ["triu_f"])
            h.cp("pool", triu_b[:], triu_f[:], r=["triu_f"], w=["triu_b"])
            h.memset("pool", cmask_b[:], 1.0, w=["cmask_b"])
            h.memset("pool", cmask_b[64:128, 0:64], 0.0, w=["cmask_b"])
            h.memset("pool", krope[:], 0.0, w=["krope"])
            for dc in range(DC):
                h.dma("sp", xT[:, dc, :], xT_d[dc * 128:(dc + 1) * 128, :], w=[("xT", dc, t) for t in range(NB)], sem=("xload", dc))
            h.act(cact[:], cs[:], AF.Silu, r=["cs"], w=["cact"])
            wi = 0
            h.cp("dve", mod1[:, :], bmod[:, 48:96], r=["bmod"], w=["mod1acc"])
            h.cp("dve", modkv[:, :], bmod[:, 96:112], r=["bmod"], w=["modkvacc"])
            for (wsrc, ncol, modt, boff) in ((w_mod_d[0], 48, mod0, 0),):
                psm = pb[2 + (wi % 2)]
                for kc in range(DC):
                    wt = wm[wi % 3]
                    wkey = ("wm", wi % 3)
                    wi += 1
                    h.dma("pool", wt[:, 0:ncol * 128], wsrc[kc * 128:(kc + 1) * 128, :], w=[wkey], sem=wkey)
                    for j in range(ncol):
                        h.mm(psm[:, kc * ncol + j:kc * ncol + j + 1], wt[:, j * 128:(j + 1) * 128], cact[:, kc:kc + 1],
                             r=[wkey, "cact"], w=[("psm", id(psm))])
                h.red(modt[:, 0:ncol], psm[:, 0:DC * ncol].rearrange("p (k j) -> p j k", k=DC), ALU.add, r=[("psm", id(psm))], w=["mod"])
                h.tt("dve", modt[:, 0:ncol], modt[:, 0:ncol], bmod[:, boff:boff + ncol], ALU.add, r=["mod", "bmod"], w=["mod"])
            h.cp("dve", ang[:], posi[:], r=["posi"], w=["ang"])
            h.ts("dve", ang[:], ang[:], invf[:, 0:1], None, ALU.mult, r=["ang", "invf"], w=["ang"])
            for (dst, shift) in ((sinT, 0.0), (cosT, float(np.pi / 2))):
                if shift != 0.0:
                    h.ts("dve", ang[:], ang[:], shift, None, ALU.add, r=["ang"], w=["ang"])
                h.ts("dve", ta[:], ang[:], float(1.0 / (2 * np.pi)), None, ALU.mult, r=["ang"], w=["ta"])
                h.cp("dve", ti[:], ta[:], r=["ta"], w=["ti"])
                h.cp("dve", ta[:], ti[:], r=["ti"], w=["ta"])
                h.stt("dve", tb_[:], ta[:], float(-2 * np.pi), ang[:], ALU.mult, ALU.add, r=["ta", "ang"], w=["tb_"])
                h.ts("dve", ta[:], tb_[:], float(np.pi), float(-2 * np.pi), ALU.is_gt, ALU.mult, r=["tb_"], w=["ta"])
                h.tt("dve", tb_[:], tb_[:], ta[:], ALU.add, r=["tb_", "ta"], w=["tb_"])
                h.ts("dve", ta[:], tb_[:], float(-np.pi), float(2 * np.pi), ALU.is_lt, ALU.mult, r=["tb_"], w=["ta"])
                h.tt("dve", tb_[:], tb_[:], ta[:], ALU.add, r=["tb_", "ta"], w=["tb_"])
                h.act(dst[:], tb_[:], AF.Sin, r=["tb_"], w=[("trig", id(dst))])
            h.memset("pool", csT[0:64, :], 0.0, w=["cs_lo"])
            h.cp("pool", csT[64:96, :], cosT[64:96, :], r=[("trig", id(cosT))], w=["rope_tab"])
            h.ts("dve", csT[96:128, :], sinT[96:128, :], invf[96:128, 1:2], None, ALU.mult, r=[("trig", id(sinT)), "invf"], w=["rope_tab2"])
            h.end()

        def attn_phase(layer):
            fox = layer == 0
            modt = mod0 if fox else mod1
            if fox:
                with ExitStack() as st0:
                    h.begin()
                    norm_phase(st0, 0, modt, 0, DC)
                    h.end()
            with ExitStack() as st:
                h.begin()
                cqT = hT
                NSET = 2
                qh = [[sb("qh%d_%d" % (s_, i), [128, S], BF16, st) for i in range(2)] for s_ in range(NSET)]
                kh = [[sb("kh%d_%d" % (s_, i), [128, S], BF16, st) for i in range(2)] for s_ in range(NSET)]
                Vp = [sb("Vp%d" % s_, [128, NT, 192], BF16, st) for s_ in range(NSET)]
                oT = [sb("oT%d" % i, [128, S], BF16, st) for i in range(2)]
                wo = [sb("wo%d" % i, [128, D], BF16, st) for i in range(3)]
                PT = [sb("PT%d" % i, [128, 512], BF16, st) for i in range(6)]
                rec = [sb("rec%d" % i, [128, 512], F32, st) for i in range(2)]
                zero_bias = sb("zero_bias", [128, 1], F32, st)
                h.memset("pool", zero_bias[:], 0.0, w=["zero_bias"])
                for s_ in range(NSET):
                    h.memset("pool", Vp[s_][:, :, 64:128], 1.0, w=[("Vp", s_)])
                    for i in range(2):
                        if fox:
                            h.memset("pool", kh[s_][i][64:65, :], 1.0, w=[("kh", s_, i)])
                        else:
                            h.cp("act" if i == 0 else "dve", kh[s_][i][64:128, :], krope[64:128, :], r=["krope"], w=[("kh", s_, i)])
                if fox:
                    wpair = [sb("wpair%d" % i, [128, DC, 384], BF16, st) for i in range(2)]
                    wf = sb("wf", [128, DC, 16], BF16, st)
                    lf = sb("lf", [128, 256], F32, st)
                    tot = sb("tot", [128, 256], F32, st)
                    off = sb("off", [128, 256], F32, st)
                    negcum = sb("negcum", [128, 256], F32, st)
                    cum8 = sb("cum8", [128, 256], F32, st)
                    cumT = sb("cumT", [16, S], BF16, st)
                    h.dma("pool", wf[:], a_w_in_d[:, 3 * D:3 * D + 16].rearrange("(dc p) c -> p dc c", p=128), w=["wf"], sem="wf")
                    for i in range(NT):
                        for dc in range(DC):
                            h.mm(pb[2][:, i * 16:(i + 1) * 16], hT[:, dc, i * 128:(i + 1) * 128], wf[:, dc, :],
                                 start=(dc == 0), stop=(dc == DC - 1), r=[("hT", i // 4), "wf"], w=[PK(2)])
                    h.tt("dve", lf[:], pb[2][:, 0:256], bf_rep[:], ALU.add, r=[PK(2), "bf_rep"], w=["lf"])
                    h.act(lf[:], lf[:], AF.Sigmoid, r=["lf"], w=["lf"])
                    h.act(lf[:], lf[:], AF.Ln, r=["lf"], w=["lf"])
                    h.mm(pb[3][:, 0:256], triu_f[:], lf[:], r=["triu_f", "lf"], w=[PK(3)])
                    h.mm(pb[2][:, 0:256], ones_f[:], lf[:], r=["ones_f", "lf"], w=[PK(2)])
                    h.cp("dve", tot[:], pb[2][:, 0:256], r=[PK(2)], w=["tot"])
                    h.memset("dve", off[:, 0:16], 0.0, w=["off"])
                    for i in range(1, NT):
                        h.tt("dve", off[:, i * 16:(i + 1) * 16], off[:, (i - 1) * 16:i * 16], tot[:, (i - 1) * 16:i * 16], ALU.add,
                             r=["off", "tot"], w=["off"])
                    h.tt("dve", cum8[:], pb[3][:, 0:256], off[:], ALU.add, r=[PK(3), "off"], w=["cum8"])
                    h.ts("dve", negcum[:], cum8[:], -1.0, None, ALU.mult, r=["cum8"], w=["negcum"])
                    h.ts("dve", cum8[:], cum8[:], 8.0, None, ALU.mult, r=["cum8"], w=["cum8"])
                    for i in range(NT):
                        bk = 4 + (i // 4) % 2
                        h.tr(pb[bk][0:16, (i % 4) * 128:(i % 4 + 1) * 128], cum8[:, i * 16:(i + 1) * 16], ident[:],
                             r=["cum8", "ident"], w=[PK(bk)])
                        if i % 4 == 3:
                            h.cp("act", cumT[:, (i // 4) * 512:(i // 4 + 1) * 512], pb[bk][0:16, :], r=[PK(bk)], w=["cumT"])
                else:
                    wq = [sb("wq%d" % i, [128, 6, 256], BF16, st) for i in range(2)]
                    wkv = [sb("wkv%d" % i, [128, 2, 256], BF16, st) for i in range(2)]

                g1c = 2 * DC
                w_o_d = a_w_o_d if fox else b_w_o_d
                scale = 0.125 if fox else float(96 ** -0.5)
                mask = triu_b if fox else cmask_b
                Kc = 65 if fox else 128
                cnt = {"ps_s": 0, "pt": 0, "ps_o": 0, "rec": 0, "op": 0}
                NPT = 6

                bgc = {"bk": 0}

                def bgbank():
                    bgc["bk"] += 1
                    return 5 + bgc["bk"] % 3

                def issue_weights(p):
                    wb = p % 2
                    h.dma("pool", wo[p % 3][:], w_o_d[p * 128:(p + 1) * 128, :], w=[("wo", p % 3)], sem=("wo", p % 3))
                    if fox:
                        for part in range(3):
                            c0 = part * D + p * 128
                            h.dma("pool", wpair[wb][:, :, part * 128:(part + 1) * 128],
                                  a_w_in_d[:, c0:c0 + 128].rearrange("(dc p) c -> p dc c", p=128), w=[("wpair", wb, part)], sem=("wpair", wb, part))
                    else:
                        for hh in range(2):
                            hd = 2 * p + hh
                            uq = b_w_uq_d[:, hd * 96:(hd + 1) * 96].rearrange("(kc p) c -> p kc c", p=128)
                            h.dma("pool", wq[wb][:, :, hh * 128:hh * 128 + 64], uq[:, :, 0:64], w=[("wq", wb, hh, 0)], sem=("wq", wb, hh, 0))
                            h.dma("pool", wq[wb][:, :, hh * 128 + 64:hh * 128 + 96], uq[:, :, 64:96], w=[("wq", wb, hh, 1)], sem=("wq", wb, hh, 1))
                            h.dma("pool", wq[wb][:, :, hh * 128 + 96:hh * 128 + 112], uq[:, :, 80:96], w=[("wq", wb, hh, 2)], sem=("wq", wb, hh, 2))
                            h.dma("pool", wq[wb][:, :, hh * 128 + 112:hh * 128 + 128], uq[:, :, 64:80], w=[("wq", wb, hh, 3)], sem=("wq", wb, hh, 3))
                            up = kv_w_up_d[:, hd * 128:(hd + 1) * 128].rearrange("(c p) n -> p c n", p=128)
                            h.dma("pool", wkv[wb][:, :, hh * 64:(hh + 1) * 64], up[:, :, 0:64], w=[("wkv", wb, hh, 0)], sem=("wkv", wb, hh, 0))
                            h.dma("pool", wkv[wb][:, :, 128 + hh * 64:128 + (hh + 1) * 64], up[:, :, 64:128], w=[("wkv", wb, hh, 1)], sem=("wkv", wb, hh, 1))

                def proj_items(p):
                    s_ = p % NSET
                    wb = p % 2
                    items = []

                    def v_item(g):
                        def f():
                            bk = bgbank()
                            for i in range(4 * g, 4 * g + 4):
                                if fox:
                                    for dc in range(DC):
                                        h.mm(pb[bk][:, (i % 4) * 128:(i % 4 + 1) * 128], hT[:, dc, i * 128:(i + 1) * 128], wpair[wb][:, dc, 256:384],
                                             start=(dc == 0), stop=(dc == DC - 1), r=[("wpair", wb, 2), ("hT", i // 4)], w=[PK(bk)])
                                else:
                                    for c in range(2):
                                        h.mm(pb[bk][:, (i % 4) * 128:(i % 4 + 1) * 128], ckvT[:, c, i * 128:(i + 1) * 128], wkv[wb][:, c, 128:256],
                                             start=(c == 0), stop=(c == 1), r=[("wkv", wb, 0, 1), ("wkv", wb, 1, 1), "ckvT"], w=[PK(bk)])
                            pv = pb[bk][:, :].rearrange("p (i c) -> p i c", c=128)
                            h.cp("act", Vp[s_][:, 4 * g:4 * g + 4, 0:64], pv[:, :, 0:64], r=[PK(bk)], w=[("Vp", s_)])
                            h.cp("dve", Vp[s_][:, 4 * g:4 * g + 4, 128:192], pv[:, :, 64:128], r=[PK(bk)], w=[("Vp", s_)])
                        return f

                    if fox:
                        def qk_item(which, tb):
                            def f():
                                nm = "qh" if which == 0 else "kh"
                                dsts = qh[s_] if which == 0 else kh[s_]
                                bk = bgbank()
                                for dc in range(DC):
                                    h.mm(pb[bk][:, :], wpair[wb][:, dc, which * 128:(which + 1) * 128], hT[:, dc, tbs(tb)],
                                         start=(dc == 0), stop=(dc == DC - 1), r=[("wpair", wb, which), ("hT", tb)], w=[PK(bk)])
                                h.cp("act", dsts[0][0:64, tbs(tb)], pb[bk][0:64, :], r=[PK(bk)], w=[(nm, s_, 0)])
                                h.cp("dve", dsts[1][0:64, tbs(tb)], pb[bk][64:128, :], r=[PK(bk)], w=[(nm, s_, 1)])
                            return f

                        def aug_item():
                            for hh in range(2):
                                h.dma("sp", qh[s_][hh][64:65, :], cumT[2 * p + hh:2 * p + hh + 1, :], r=["cumT"], w=[("qh", s_, hh)], sem=("aug", s_, hh))
                        items.append(aug_item)
                        for which in range(2):
                            for tb in range(NB):
                                items.append(qk_item(which, tb))
                    else:
                        def q_item(hh, tb):
                            def f():
                                bk = bgbank()
                                for kc in range(6):
                                    h.mm(pb[bk][:, :], wq[wb][:, kc, hh * 128:(hh + 1) * 128], cqT[:, kc, tbs(tb)],
                                         start=(kc == 0), stop=(kc == 5),
                                         r=[("wq", wb, hh, 0), ("wq", wb, hh, 1), ("wq", wb, hh, 2), ("wq", wb, hh, 3), ("hT", tb)], w=[PK(bk)])
                                h.cp("act", qh[s_][hh][0:64, tbs(tb)], pb[bk][0:64, :], r=[PK(bk)], w=[("qh", s_, hh)])
                                h.tt("dve", qh[s_][hh][64:128, tbs(tb)], pb[bk][64:128, :], csT[64:128, tbs(tb)], ALU.mult,
                                     r=[PK(bk), "rope_tab", "rope_tab2"], w=[("qh", s_, hh)])
                            return f

                        def k_item(tb):
                            def f():
                                bk = bgbank()
                                for c in range(2):
                                    h.mm(pb[bk][:, :], wkv[wb][:, c, 0:128], ckvT[:, c, tbs(tb)], start=(c == 0), stop=(c == 1),
                                         r=[("wkv", wb, 0, 0), ("wkv", wb, 1, 0), "ckvT"], w=[PK(bk)])
                                h.cp("act", kh[s_][0][0:64, tbs(tb)], pb[bk][0:64, :], r=[PK(bk)], w=[("kh", s_, 0)])
                                h.cp("dve", kh[s_][1][0:64, tbs(tb)], pb[bk][64:128, :], r=[PK(bk)], w=[("kh", s_, 1)])
                            return f
                        for hh in range(2):
                            for tb in range(NB):
                                items.append(q_item(hh, tb))
                        for tb in range(NB):
                            items.append(k_item(tb))
                    for g in range(4):
                        items.append(v_item(g))
                    return items

                def outproj_items(p):
                    wb = p % 3
                    ob = p % 2
                    items = []

                    def o_item(tb, dcol):
                        def f():
                            bo = bgbank()
                            h.mm(pb[bo][:, :], wo[wb][:, dcol * 128:(dcol + 1) * 128], oT[ob][:, tbs(tb)],
                                 r=[("wo", wb), ("oT", ob, tb)], w=[PK(bo)])
                            h.stt("dve", xT[:, dcol, tbs(tb)], pb[bo][:, :], modt[:, g1c + dcol:g1c + dcol + 1], xT[:, dcol, tbs(tb)],
                                  ALU.mult, ALU.add, r=[PK(bo), "mod", ("xT", dcol, tb)], w=[("xT", dcol, tb)])
                        return f
                    for tb in range(NB):
                        for dcol in range(DC):
                            items.append(o_item(tb, dcol))
                    return items

                def attn_steps(p):
                    s_ = p % NSET
                    ob = p % 2
                    steps = []
                    for hh in range(2):
                        for qb in range(NB):
                            nkt = 4 * qb + 4
                            ob_k = 3 + cnt["ps_o"] % 2
                            cnt["ps_o"] += 1
                            for kt in range(nkt):
                                steps.append((hh, qb, kt, nkt, ob_k, cnt["ps_s"] % 3, cnt["pt"] % NPT))
                                cnt["ps_s"] += 1
                                cnt["pt"] += 1

                    def emit_S(stp):
                        hh, qb, kt, nkt, ob_k, sk, pk = stp
                        hd = 2 * p + hh
                        qt, kt_ = qh[s_][hh], kh[s_][hh]
                        jj = kt - 4 * qb
                        n0 = max(0, jj) * 128
                        h.mm(pb[sk][:, n0:512], kt_[0:Kc, kt * 128:(kt + 1) * 128], qt[0:Kc, qb * 512 + n0:(qb + 1) * 512],
                             r=[("kh", s_, hh), ("qh", s_, hh)], w=[PK(sk)])
                        if fox:
                            bias = negcum[:, kt * 16 + hd:kt * 16 + hd + 1]
                            rk = [PK(sk), "negcum"]
                        else:
                            bias = zero_bias[:, 0:1]
                            rk = [PK(sk), "zero_bias"]
                        h.act(PT[pk][:, n0:512], pb[sk][:, n0:512], AF.Exp, bias=bias, scale=scale, r=rk, w=[("PT", pk)])
                        if jj >= 0:
                            h.tt("pool", PT[pk][:, n0:n0 + 128], PT[pk][:, n0:n0 + 128], mask[:], ALU.mult,
                                 r=[("PT", pk), "mask"], w=[("PT", pk)])

                    def emit_PV(stp):
                        hh, qb, kt, nkt, ob_k, sk, pk = stp
                        jj = kt - 4 * qb
                        n0 = max(0, jj) * 128
                        vl = Vp[s_][:, kt, 0:128] if hh == 0 else Vp[s_][:, kt, 64:192]
                        h.mm(pb[ob_k][:, n0:512], vl, PT[pk][:, n0:512], start=(kt == 0), stop=(kt == nkt - 1),
                             r=[("Vp", s_), ("PT", pk)], w=[PK(ob_k)])
                        if kt == nkt - 1:
                            rc = cnt["rec"] % 2
                            cnt["rec"] += 1
                            if hh == 0:
                                h.act(rec[rc][0:64, :], pb[ob_k][64:128, :], AF.Ln, r=[PK(ob_k)], w=[("rec", rc)])
                                h.act(rec[rc][0:64, :], rec[rc][0:64, :], AF.Exp, scale=-1.0, r=[("rec", rc)], w=[("rec", rc)])
                                h.tt("dve", oT[ob][0:64, tbs(qb)], pb[ob_k][0:64, :], rec[rc][0:64, :], ALU.mult,
                                     r=[PK(ob_k), ("rec", rc)], w=[("oT", ob, qb)])
                            else:
                                h.act(rec[rc][64:128, :], pb[ob_k][0:64, :], AF.Ln, r=[PK(ob_k)], w=[("rec", rc)])
                                h.act(rec[rc][64:128, :], rec[rc][64:128, :], AF.Exp, scale=-1.0, r=[("rec", rc)], w=[("rec", rc)])
                                h.tt("dve", oT[ob][64:128, tbs(qb)], pb[ob_k][64:128, :], rec[rc][64:128, :], ALU.mult,
                                     r=[PK(ob_k), ("rec", rc)], w=[("oT", ob, qb)])
                    return steps, emit_S, emit_PV

                LOOK = 2
                issue_weights(0)
                for it in proj_items(0):
                    it()
                bg = []
                for p in range(8):
                    if p + 1 < 8:
                        issue_weights(p + 1)
                        bg = bg + proj_items(p + 1)
                    steps, emit_S, emit_PV = attn_steps(p)
                    nst = len(steps)
                    nbg = len(bg)
                    done = 0
                    for i_ in range(nst + LOOK):
                        if i_ < nst:
                            emit_S(steps[i_])
                        if i_ >= LOOK:
                            emit_PV(steps[i_ - LOOK])
                        tgt = (nbg * (i_ + 1)) // (nst + LOOK)
                        while done < tgt:
                            bg[done]()
                            done += 1
                    while done < nbg:
                        bg[done]()
                        done += 1
                    bg = outproj_items(p)
                for it in bg:
                    it()
                h.end()

        def moe_phase(layer):
            modt = mod0 if layer == 0 else mod1
            g2c = 5 * DC
            with ExitStack() as st:
                selE = sb("selE", [16, NE, 128], BF16, st)
                combT = sb("combT", [16, S], BF16, st)
                with ExitStack() as st2:
                    rw = sb("rw", [128, DC, NE], F32, st2)
                    h32 = sb("h32", [128, DC, 512], F32, st2)
                    R = {n: sb("r_" + n, [128, 256], F32, st2) for n in ("sc", "sel", "a", "b", "c", "top2", "w")}
                    gs = sb("gs", [128, 64], F32, st2)
                    gtmp = sb("gtmp", [128, 64], F32, st2)
                    gmax = sb("gmax", [128, 16], F32, st2)
                    ohg = sb("ohg", [128, 64], F32, st2)
                    wsum = sb("wsum", [128, 16], F32, st2)
                    h.begin()
                    h.dma("sp", rw[:], router_w_d[:, :].rearrange("(dc p) e -> p dc e", p=128), w=["rw"], sem="rw")
                    for e_ in range(NE):
                        h.ts("pool", selE[:, e_, :], ones_f[0:16, :], ident[0:16, e_:e_ + 1], None, ALU.mult, r=["ones_f", "ident"], w=["selE"])

                    def router(tb, dc, t):
                        h.ts("dve", h32[:, dc, :], t[:], ncoef[:, dc:dc + 1], modt[:, 3 * DC + dc:3 * DC + dc + 1], ALU.mult, ALU.add,
                             r=[("t2", id(t)), "ncoef", "mod"], w=[("h32", dc)])
                        if dc == DC - 1:
                            for i in range(4):
                                ti_ = tb * 4 + i
                                for d2 in range(DC):
                                    h.mm(pb[2][:, ti_ * 16:(ti_ + 1) * 16], h32[:, d2, i * 128:(i + 1) * 128], rw[:, d2, :],
                                         start=(d2 == 0), stop=(d2 == DC - 1), r=[("h32", d2), "rw"], w=[PK(2)])

                    norm_phase(st2, 1 if layer == 0 else 4, modt, 3 * DC, 4 * DC, router=router)
                    sc, sel, A_, B_, C_, top2, w_ = (R[n] for n in ("sc", "sel", "a", "b", "c", "top2", "w"))
                    h.act(sc[:], pb[2][:, 0:256], AF.Sigmoid, r=[PK(2)], w=["r_sc"])
                    h.tt("dve", sel[:], sc[:], rb_rep[:], ALU.add, r=["r_sc", "rb_rep"], w=["r_sel"])
                    X = sel[:].rearrange("p (t e) -> p t e", e=4)
                    first = True
                    for (a, b) in ((0, 1), (0, 2), (0, 3), (1, 2), (1, 3), (2, 3)):
                        if first:
                            h.tt("dve", gs[:], X[:, :, a], X[:, :, b], ALU.add, r=["r_sel"], w=["gs"])
                            first = False
                        else:
                            h.tt("dve", gtmp[:], X[:, :, a], X[:, :, b], ALU.add, r=["r_sel"], w=["gtmp"])
                            h.tt("dve", gs[:], gs[:], gtmp[:], ALU.max, r=["gs", "gtmp"], w=["gs"])
                    G4 = gs[:].rearrange("p (t g) -> p t g", g=4)
                    h.tt("dve", gmax[:], G4[:, :, 0], G4[:, :, 1], ALU.max, r=["gs"], w=["gmax"])
                    h.tt("dve", gmax[:], gmax[:], G4[:, :, 2], ALU.max, r=["gs", "gmax"], w=["gmax"])
                    h.tt("dve", gmax[:], gmax[:], G4[:, :, 3], ALU.max, r=["gs", "gmax"], w=["gmax"])
                    O4 = ohg[:].rearrange("p (t g) -> p t g", g=4)
                    for g in range(4):
                        h.tt("dve", O4[:, :, g], G4[:, :, g], gmax[:], ALU.is_equal, r=["gs", "gmax"], w=["ohg"])
                    T2 = top2[:].rearrange("p (t e) -> p t e", e=4)
                    A3 = A_[:].rearrange("p (t e) -> p t e", e=4)
                    B3 = B_[:].rearrange("p (t e) -> p t e", e=4)
                    for e_ in range(4):
                        oth = [x for x in range(4) if x != e_]
                        h.tt("dve", A3[:, :, e_], X[:, :, oth[0]], X[:, :, e_], ALU.is_gt, r=["r_sel"], w=["r_a"])
                        h.tt("dve", B3[:, :, e_], X[:, :, oth[1]], X[:, :, e_], ALU.is_gt, r=["r_sel"], w=["r_b"])
                        h.tt("dve", A3[:, :, e_], A3[:, :, e_], B3[:, :, e_], ALU.add, r=["r_a", "r_b"], w=["r_a"])
                        h.tt("dve", B3[:, :, e_], X[:, :, oth[2]], X[:, :, e_], ALU.is_gt, r=["r_sel"], w=["r_b"])
                        h.tt("dve", A3[:, :, e_], A3[:, :, e_], B3[:, :, e_], ALU.add, r=["r_a", "r_b"], w=["r_a"])
                        h.ts("dve", T2[:, :, e_], A3[:, :, e_], 1.5, None, ALU.is_lt, r=["r_a"], w=["r_top2"])
                        h.tt("dve", T2[:, :, e_], T2[:, :, e_], ohg[:], ALU.mult, r=["r_top2", "ohg"], w=["r_top2"])
                    h.tt("dve", w_[:], sc[:], top2[:], ALU.mult, r=["r_sc", "r_top2"], w=["r_w"])
                    h.red(wsum[:], w_[:].rearrange("p (t e) -> p t e", e=16), ALU.add, r=["r_w"], w=["wsum"])
                    h.rcp(wsum[:], wsum[:], r=["wsum"], w=["wsum"])
                    for i in range(NT):
                        h.ts("dve", C_[:, i * 16:(i + 1) * 16], w_[:, i * 16:(i + 1) * 16], wsum[:, i:i + 1], None, ALU.mult,
                             r=["r_w", "wsum"], w=["r_c"])
                    for i in range(NT):
                        bk = 4 + (i // 4) % 2
                        h.tr(pb[bk][0:16, (i % 4) * 128:(i % 4 + 1) * 128], C_[:, i * 16:(i + 1) * 16], ident[:],
                             r=["r_c", "ident"], w=[PK(bk)])
                        if i % 4 == 3:
                            h.cp("act", combT[:, (i // 4) * 512:(i // 4 + 1) * 512], pb[bk][0:16, :], r=[PK(bk)], w=["combT"])
                    h.end()
                if layer == 1 and globals().get("_MOE1_SKIP_B", False):
                    return
                wg = [sb("wg%d" % i, [128, DC, 512], BF16, st) for i in range(2)]
                wu = [sb("wu%d" % i, [128, DC, 512], BF16, st) for i in range(2)]
                wd = [sb("wd%d" % i, [128, 4, D], BF16, st) for i in range(2)]
                aT = [sb("aT%d" % i, [128, 4, 512], BF16, st) for i in range(2)]
                sg = [sb("sg%d" % i, [128, 512], F32, st) for i in range(3)]
                cmb = [sb("cmb%d" % i, [128, 512], F32, st) for i in range(2)]
                h.begin()
                n_g = 0
                n_y = 0
                n_it = 0
                mod_items = []
                if layer == 0:
                    wmc = [sb("wmc%d" % i, [128, 1024], BF16, st) for i in range(2)]
                    for (wsrc, nblk, modt_, mkey) in ((w_mod_d[1], 6, mod1, "mod1acc"), (kv_w_mod_d, 2, modkv, "modkvacc")):
                        for blk in range(nblk):
                            for kc in range(DC):
                                mod_items.append((wsrc, blk, kc, modt_, mkey))

                def issue_mod_dma(n):
                    wsrc, blk, kc, modt_, mkey = mod_items[n]
                    wkey = ("wmc", n % 2)
                    h.dma("pool", wmc[n % 2][:], wsrc[kc * 128:(kc + 1) * 128, blk * 1024:(blk + 1) * 1024], w=[wkey], sem=wkey)

                if mod_items:
                    issue_mod_dma(0)

                def run_mod_item(n):
                    wsrc, blk, kc, modt_, mkey = mod_items[n]
                    wt = wmc[n % 2]
                    wkey = ("wmc", n % 2)
                    if n + 1 < len(mod_items):
                        issue_mod_dma(n + 1)
                    for j in range(8):
                        h.mm(pb[0][:, j:j + 1], wt[:, j * 128:(j + 1) * 128], cact[:, kc:kc + 1], r=[wkey, "cact"], w=[PK(0)])
                    h.tt("dve", modt_[:, blk * 8:(blk + 1) * 8], modt_[:, blk * 8:(blk + 1) * 8], pb[0][:, 0:8], ALU.add,
                         r=[PK(0), mkey], w=[mkey])

                def issue_expert(e2):
                    w2 = e2 % 2
                    h.dma("pool", wg[w2][:], wg_d[layer][e2].rearrange("(dc p) f -> p dc f", p=128), w=[("wg", w2)], sem=("wg", w2))
                    h.dma("pool", wu[w2][:], wu_d[layer][e2].rearrange("(dc p) f -> p dc f", p=128), w=[("wu", w2)], sem=("wu", w2))
                    h.dma("pool", wd[w2][:], wd_d[layer][e2].rearrange("(fc p) d -> p fc d", p=128), w=[("wd", w2)], sem=("wd", w2))

                issue_expert(0)
                for e_ in range(NE):
                    wb = e_ % 2
                    if e_ + 1 < NE:
                        issue_expert(e_ + 1)
                    for tb in range(NB):
                        ab = n_it % 2
                        n_it += 1
                        h.mm(pb[0][:, :], selE[:, e_, :], combT[:, tbs(tb)], r=["selE", "combT"], w=[PK(0)])
                        h.cp("act", cmb[ab][:], pb[0][:, :], r=[PK(0)], w=[("cmb", ab)])
                        for fc in range(4):
                            bg = 1 + n_g % 2
                            bu = 3 + n_g % 2
                            sgi = n_g % 3
                            n_g += 1
                            for dc in range(DC):
                                h.mm(pb[bg][:, :], wg[wb][:, dc, fc * 128:(fc + 1) * 128], hT[:, dc, tbs(tb)],
                                     start=(dc == 0), stop=(dc == DC - 1), r=[("wg", wb), ("hT", tb)], w=[PK(bg)])
                            for dc in range(DC):
                                h.mm(pb[bu][:, :], wu[wb][:, dc, fc * 128:(fc + 1) * 128], hT[:, dc, tbs(tb)],
                                     start=(dc == 0), stop=(dc == DC - 1), r=[("wu", wb), ("hT", tb)], w=[PK(bu)])
                            h.act(sg[sgi][:], pb[bg][:, :], AF.Silu, r=[PK(bg)], w=[("sg", sgi)])
                            h.tt("dve", sg[sgi][:], sg[sgi][:], cmb[ab][:], ALU.mult, r=[("sg", sgi), ("cmb", ab)], w=[("sg", sgi)])
                            h.tt("dve", aT[ab][:, fc, :], pb[bu][:, :], sg[sgi][:], ALU.mult, r=[PK(bu), ("sg", sgi)], w=[("aT", ab, fc)])
                            if fc == 1 and n_it - 1 < len(mod_items):
                                run_mod_item(n_it - 1)
                        for dcol in range(DC):
                            by = 5 + n_y % 3
                            n_y += 1
                            for fc in range(4):
                                h.mm(pb[by][:, :], wd[wb][:, fc, dcol * 128:(dcol + 1) * 128], aT[ab][:, fc, :],
                                     start=(fc == 0), stop=(fc == 3), r=[("wd", wb), ("aT", ab, fc)], w=[PK(by)])
                            h.stt("dve", xT[:, dcol, tbs(tb)], pb[by][:, :], modt[:, g2c + dcol:g2c + dcol + 1], xT[:, dcol, tbs(tb)],
                                  ALU.mult, ALU.add, r=[PK(by), "mod", ("xT", dcol, tb)], w=[("xT", dcol, tb)])
                h.end()

        def kv_phase():
            with ExitStack() as st:
                wkd = sb("wkd", [128, DC, 256], BF16, st)
                wkr = sb("wkr", [128, DC, 128], BF16, st)
                craw = sb("kraw", [128, 2, 512], F32, st)
                csq = [sb("ksq%d" % i, [128, 512], BF16, st) for i in range(2)]
                crs = sb("krs", [128, 512], F32, st)
                ra = [sb("kra%d" % i, [128, 512], F32, st) for i in range(2)]
                rb = [sb("krb%d" % i, [128, 512], F32, st) for i in range(2)]
                h.begin()
                h.memset("pool", wkr[:, :, 0:64], 0.0, w=["wkr_z"])
                dn = kv_w_down_d[:, :].rearrange("(dc p) c -> p dc c", p=128)
                h.dma("pool", wkd[:], dn[:, :, 0:256], w=["wkd"], sem="wkd")
                h.dma("pool", wkr[:, :, 64:96], dn[:, :, 256:288], w=["wkr0"], sem="wkr0")
                h.dma("pool", wkr[:, :, 96:112], dn[:, :, 272:288], w=["wkr1"], sem="wkr1")
                h.dma("pool", wkr[:, :, 112:128], dn[:, :, 256:272], w=["wkr2"], sem="wkr2")
                norm_phase(st, 2, modkv, 0, DC)
                for tb in range(NB):
                    for c in range(2):
                        bk = 2 + c
                        for dc in range(DC):
                            h.mm(pb[bk][:, :], wkd[:, dc, c * 128:(c + 1) * 128], hT[:, dc, tbs(tb)], start=(dc == 0), stop=(dc == DC - 1),
                                 r=["wkd", ("hT", tb)], w=[PK(bk)])
                        h.cp("act", craw[:, c, :], pb[bk][:, :], r=[PK(bk)], w=[("kraw", c)])
                        h.tt("dve", csq[c][:], craw[:, c, :], craw[:, c, :], ALU.mult, r=[("kraw", c)], w=[("ksq", c)])
                        h.mm(pb[4][:, :], ones_b[:, :], csq[c][:], start=(c == 0), stop=(c == 1), r=[("ksq", c), "ones_b"], w=[PK(4)])
                    h.act(crs[:], pb[4][:, :], AF.Ln, bias=epst[:, 0:1], scale=1.0 / 256, r=[PK(4), "epst"], w=["krs"])
                    h.act(crs[:], crs[:], AF.Exp, scale=-0.5, r=["krs"], w=["krs"])
                    for c in range(2):
                        h.stt("dve", ckvT[:, c, tbs(tb)], craw[:, c, :], lat_g[:, c:c + 1], crs[:], ALU.mult, ALU.mult,
                              r=[("kraw", c), "krs", "lat_g"], w=["ckvT"])
                    bk = 5 + tb % 2
                    for dc in range(DC):
                        h.mm(pb[bk][:, :], wkr[:, dc, :], hT[:, dc, tbs(tb)], start=(dc == 0), stop=(dc == DC - 1),
                             r=["wkr_z", "wkr0", "wkr1", "wkr2", ("hT", tb)], w=[PK(bk)])
                    cs_ = tbs(tb)
                    a_, b_ = ra[tb % 2], rb[tb % 2]
                    h.tt("dve", a_[64:128, :], pb[bk][64:128, :], csT[64:128, cs_], ALU.mult, r=[PK(bk), "rope_tab", "rope_tab2"], w=[("kra", tb % 2)])
                    h.cp("act", b_[64:96, :], a_[96:128, :], r=[("kra", tb % 2)], w=[("krb", tb % 2)])
                    h.tt("dve", krope[64:96, cs_], a_[64:96, :], b_[64:96, :], ALU.add, r=[("kra", tb % 2), ("krb", tb % 2)], w=[("krope", tb)])
                    h.cp("act", krope[96:128, cs_], krope[64:96, cs_], r=[("krope", tb)], w=[("kropeB", tb)])
                norm_phase(st, 3, mod1, 0, DC)
                wdq = sb("wdq", [128, DC, 768], BF16, st)
                craw = sb("craw", [128, 6, 512], F32, st)
                csq = [sb("csq%d" % i, [128, 512], BF16, st) for i in range(2)]
                crs = sb("crs", [128, 512], F32, st)
                h.dma("pool", wdq[:], b_w_dq_d[:, :].rearrange("(dc p) c -> p dc c", p=128), w=["wdq"], sem="wdq")
                for tb in range(NB):
                    pend = None
                    for c in range(6):
                        bk = 2 + c % 2
                        for dc in range(DC):
                            h.mm(pb[bk][:, :], wdq[:, dc, c * 128:(c + 1) * 128], hT[:, dc, tbs(tb)],
                                 start=(dc == 0), stop=(dc == DC - 1), r=["wdq", ("hT", tb)], w=[PK(bk)])
                        if pend is not None:
                            pend()
                        h.cp("act", craw[:, c, :], pb[bk][:, :], r=[PK(bk)], w=[("craw", c)])
                        q_ = csq[c % 2]
                        h.tt("dve", q_[:], craw[:, c, :], craw[:, c, :], ALU.mult, r=[("craw", c)], w=[("csq", c % 2)])

                        def pend(c=c, q_=q_):
                            h.mm(pb[4][:, :], ones_b[:, :], q_[:], start=(c == 0), stop=(c == 5), r=[("csq", c % 2), "ones_b"], w=[PK(4)])
                    pend()
                    h.act(crs[:], pb[4][:, :], AF.Ln, bias=epst[:, 0:1], scale=1.0 / 768, r=[PK(4), "epst"], w=["crs"])
                    h.act(crs[:], crs[:], AF.Exp, scale=-0.5, r=["crs"], w=["crs"])
                    for c in range(6):
                        h.stt("dve", hT[:, c, tbs(tb)], craw[:, c, :], q_g[:, c:c + 1], crs[:], ALU.mult, ALU.mult,
                              r=[("craw", c), "crs", "q_g"], w=[("hT", tb)])
                h.end()

        def final_phase(do_norm):
            with ExitStack() as st:
                sq = [sb("fsq%d" % i, [128, 512], BF16, st) for i in range(3)]
                rs = [sb("frs%d" % i, [128, 512], F32, st) for i in range(2)]
                ob = [sb("fob%d" % i, [128, 512], F32, st) for i in range(4)]
                h.begin()
                for _i in range(globals().get("_DUMMY", 0)):
                    _de = globals().get("_DUMMY_ENG", "pe")
                    if _de == "pe":
                        h.mm(pb[7][:, 0:16], ones_b[:, :], ones_b[:, 0:16], r=["ones_b"], w=[PK(7)])
                    elif _de == "dve":
                        h.memset("dve", sq[0][:, 0:8], 0.0, w=[])
                    elif _de == "actbig":
                        h.act(hT[:, 0, :], xT[:, 0, :], AF.Copy, r=[], w=[])
                    elif _de == "pooldma":
                        h.dma("pool", sq[2][:, 0:16], a_w_o_d[0:128, 0:16], w=["dummy_dma"], sem="dummy_dma")
                    else:
                        h.act(sq[1][:, 0:8], ones_b[:, 0:8], AF.Copy, r=[], w=[])
                n = 0
                for tb in range(NB):
                    if do_norm:
                        pss = pb[tb % 2]
                        for dc in range(DC):
                            q = sq[n % 3]
                            n += 1
                            if dc % 2 == 0:
                                h.act(q[:], xT[:, dc, tbs(tb)], AF.Square, r=[("xT", dc, tb)], w=[("sq", id(q))])
                            else:
                                h.tt("dve", q[:], xT[:, dc, tbs(tb)], xT[:, dc, tbs(tb)], ALU.mult, r=[("xT", dc, tb)], w=[("sq", id(q))])
                            h.mm(pss[:, :], ones_b[:, :], q[:], start=(dc == 0), stop=(dc == DC - 1), r=[("sq", id(q)), "ones_b"], w=[PK(tb % 2)])
                        r_ = rs[tb % 2]
                        h.act(r_[:], pss[:, :], AF.Ln, bias=epst[:, 0:1], scale=1.0 / D, r=[PK(tb % 2), "epst"], w=[("rs", tb % 2)])
                        h.act(r_[:], r_[:], AF.Exp, scale=-0.5, r=[("rs", tb % 2)], w=[("rs", tb % 2)])
                    for dc in range(DC):
                        o = ob[n % 4]
                        ok = ("ob", n % 4)
                        n += 1
                        if do_norm:
                            h.stt("dve", o[:], xT[:, dc, tbs(tb)], gvec[:, 5 * DC + dc:5 * DC + dc + 1], r_[:], ALU.mult, ALU.mult,
                                  r=[("xT", dc, tb), "gvec", ("rs", tb % 2)], w=[ok])
                        else:
                            h.cp("dve", o[:], xT[:, dc, tbs(tb)], r=[("xT", dc, tb)], w=[ok])
                        h.dma("sp", outT_d[dc * 128:(dc + 1) * 128, tbs(tb)], o[:], r=[ok], w=[("outd", dc, tb)], sem=ok)
                h.end()

        fns = [lambda: attn_phase(0), lambda: moe_phase(0), kv_phase, lambda: attn_phase(1), lambda: moe_phase(1)]
        for i_ in range(5):
            if lo <= i_ <= hi and i_ not in globals().get("_SKIP", []):
                fns[i_]()
        for _i in range(globals().get("_DUMMY_BLOCKS", 0)):
            h.begin()
            h.memset("dve", ncoef[:, 8:9], 0.0, w=["x"])
            h.end()
        final_phase(hi >= 5)
    return nc


_CACHE = {}


def _lay(v, k):
    return np.ascontiguousarray(np.asarray(v, np.float32).reshape(k, 128).T)


def kernel(x, c, positions, a_norm_g, a_w_in, a_b_f, a_w_o, kv_norm_g, kv_w_mod, kv_b_mod,
           kv_w_down, kv_latent_g, kv_w_up, b_norm_g, b_w_dq, b_q_norm_g, b_w_uq, b_w_o,
           w_mod, b_mod, ffn_norm_g, router_w, router_bias, exp_w_gate, exp_w_up, exp_w_down,
           final_norm_g):
    f = lambda a: np.ascontiguousarray(np.asarray(a, np.float32))
    x = f(x)
    B = x.shape[0]
    if "nc" not in _CACHE:
        _CACHE["nc"] = build_program(0, 5)
    gvec = np.concatenate([_lay(a_norm_g[0], 8), _lay(ffn_norm_g[0], 8), _lay(kv_norm_g, 8), _lay(b_norm_g[0], 8),
                           _lay(ffn_norm_g[1], 8), _lay(final_norm_g, 8)], axis=1)
    bmod = np.concatenate([_lay(b_mod[0], 48), _lay(b_mod[1], 48), _lay(kv_b_mod, 16)], axis=1)
    bf_rep = np.ascontiguousarray(np.tile(np.asarray(a_b_f[0], np.float32)[None, :], (128, 16)))
    rb_rep = np.ascontiguousarray(np.tile(np.asarray(router_bias, np.float32)[None, :], (128, 16)))
    half = 16
    inv_freq = (10000.0 ** (-np.arange(half, dtype=np.float32) / half)).astype(np.float32)
    invf = np.zeros((128, 2), np.float32)
    for p in range(128):
        invf[p, 0] = inv_freq[p % 16]
        invf[p, 1] = -1.0 if (p % 32) < 16 else 1.0
    shared = {
        "invf": invf, "gvec": np.ascontiguousarray(gvec), "bmod": np.ascontiguousarray(bmod), "bf_rep": bf_rep, "rb_rep": rb_rep,
        "lat_g": _lay(kv_latent_g, 2), "q_g": _lay(b_q_norm_g[0], 6),
        "w_mod": f(w_mod), "kv_w_mod": f(kv_w_mod), "a_w_in": f(a_w_in[0]), "a_w_o": f(a_w_o[0]),
        "kv_w_down": f(kv_w_down), "kv_w_up": f(kv_w_up), "b_w_dq": f(b_w_dq[0]), "b_w_uq": f(b_w_uq[0]),
        "b_w_o": f(b_w_o[0]), "router_w": f(router_w),
        "exp_w_gate0": f(exp_w_gate[0]), "exp_w_gate1": f(exp_w_gate[1]), "exp_w_up0": f(exp_w_up[0]), "exp_w_up1": f(exp_w_up[1]),
        "exp_w_down0": f(exp_w_down[0]), "exp_w_down1": f(exp_w_down[1]),
    }
    in_maps = []
    for b in range(B):
        m = dict(shared)
        m["xT"] = np.ascontiguousarray(x[b].T)
        m["c_l"] = _lay(c[b], 8)
        m["pos"] = np.ascontiguousarray(np.asarray(positions[b], np.int32)[None, :])
        in_maps.append(m)
    res = run_bass_kernel_spmd(_CACHE["nc"], in_maps, core_ids=list(range(B)))
    out = np.stack([np.asarray(r["outT"], np.float32).T for r in res.results], axis=0)
    return np.ascontiguousarray(out)
```

```python
import numpy as np
from contextlib import ExitStack
import concourse.bass as bass
import concourse.mybir as mybir
from concourse.bass_utils import run_bass_kernel_spmd

F32 = mybir.dt.float32
BF16 = mybir.dt.bfloat16
I32 = mybir.dt.int32
AF = mybir.ActivationFunctionType
ALU = mybir.AluOpType
AX = mybir.AxisListType

S = 2048
D = 1024
NT = 16
NB = 4
DC = 8
NE = 16
EPS = 1e-6
SAME_ENGINE_SYNC = True
STOP_AFTER = "final"


class _Op:
    __slots__ = ("eng", "idx", "fn", "deps", "need_sig", "sig_val", "dma", "dma_val")

    def __init__(self, eng, idx, fn, dma):
        self.eng = eng
        self.idx = idx
        self.fn = fn
        self.deps = []
        self.need_sig = False
        self.sig_val = 0
        self.dma = dma
        self.dma_val = 0


class Prog:
    ENGS = ("pe", "act", "dve", "pool", "sp")

    def __init__(self, nc):
        self.nc = nc
        self.ops = {e: [] for e in self.ENGS}
        self.last_w = {}
        self.readers = {}
        self.dma_cnt = {}

    def op(self, eng, fn, r=(), w=(), dma=None):
        o = _Op(eng, len(self.ops[eng]), fn, dma)
        deps = {}

        def add(d):
            if d.dma is not None:
                key = ("d", d.dma)
                if key not in deps or deps[key].dma_val < d.dma_val:
                    deps[key] = d
            else:
                if d.eng == eng and (eng == "pe" or not SAME_ENGINE_SYNC):
                    return
                key = ("c", d.eng)
                if key not in deps or deps[key].idx < d.idx:
                    deps[key] = d

        for k in r:
            d = self.last_w.get(k)
            if d is not None:
                add(d)
        for k in w:
            d = self.last_w.get(k)
            if d is not None:
                add(d)
            rd = self.readers.get(k)
            if rd:
                for d in rd.values():
                    add(d)
        for d in deps.values():
            if d.dma is None:
                d.need_sig = True
            o.deps.append(d)
        for k in w:
            self.last_w[k] = o
            self.readers[k] = {}
        for k in r:
            rd = self.readers.setdefault(k, {})
            if dma is not None:
                rd[("d", dma, o.idx)] = o
            else:
                rd[("c", eng)] = o
        if dma is not None:
            c = self.dma_cnt.get(dma, 0) + 1
            self.dma_cnt[dma] = c
            o.dma_val = 16 * c
        self.ops[eng].append(o)
        return o

    def emit(self, pool, final_waits=()):
        nc = self.nc
        for e in self.ENGS:
            c = 0
            for o in self.ops[e]:
                if o.dma is None and o.need_sig:
                    c += 1
                    o.sig_val = pool.ebase[e] + c
            pool.ebase[e] += c
        swk = set()
        for o in self.ops["pool"]:
            if o.dma is not None:
                swk.add(o.dma)
        slot = {}
        n_sw, n_hw = 0, 0
        for k in self.dma_cnt:
            if k in swk:
                slot[k] = n_sw
                n_sw += 1
            else:
                slot[k] = pool.n_sw + n_hw
                n_hw += 1
        assert n_sw <= pool.n_sw and n_hw <= len(pool.dsem) - pool.n_sw, (n_sw, n_hw)
        for e in self.ENGS:
            for o in self.ops[e]:
                if o.dma is not None:
                    o.dma_val += pool.dbase[slot[o.dma]]
        esem = pool.esem
        dsem = {k: pool.dsem[i] for k, i in slot.items()}
        all_final = {k: pool.dbase[slot[k]] + 16 * self.dma_cnt[k] for k in slot}
        for k, i in slot.items():
            pool.dbase[i] += 16 * self.dma_cnt[k]
        with nc.Block() as block:

            def run(e, engobj):
                waited = {}
                for o in self.ops[e]:
                    ws = []
                    for d in o.deps:
                        if d.dma is not None:
                            sem, val, sk = dsem[d.dma], d.dma_val, ("d", d.dma)
                        else:
                            sem, val, sk = esem[d.eng], d.sig_val, ("c", d.eng)
                        if waited.get(sk, 0) >= val:
                            continue
                        waited[sk] = val
                        ws.append((sem, val))
                    for sem, val in ws[:-1]:
                        engobj.wait_ge(sem, val)
                    ins = o.fn(engobj)
                    if ws:
                        ins._wait_ge(ws[-1][0], ws[-1][1])
                    if o.dma is not None:
                        ins.then_inc(dsem[o.dma], 16)
                    elif o.need_sig:
                        ins.then_inc(esem[e], 1)
                if e == "sp":
                    for k, v in all_final.items():
                        engobj.wait_ge(dsem[k], v)

            @block.tensor
            def _(pe):
                run("pe", pe)

            @block.scalar
            def _(act):
                run("act", act)

            @block.vector
            def _(dve):
                run("dve", dve)

            @block.gpsimd
            def _(pool_):
                run("pool", pool_)

            @block.sync
            def _(sp):
                run("sp", sp)


class SemPool:
    def __init__(self, nc, st, n_dma=54, n_sw=32):
        self.n_sw = n_sw
        self.esem = {e: st.enter_context(nc.semaphore("g_" + e)) for e in Prog.ENGS}
        self.ebase = {e: 0 for e in Prog.ENGS}
        self.dsem = [st.enter_context(nc.semaphore("gd%d" % i)) for i in range(n_dma)]
        self.dbase = [0] * n_dma


class H:
    def __init__(self, nc):
        self.nc = nc
        self.P = None
        self.pool = None

    def begin(self):
        self.P = Prog(self.nc)

    def end(self, final_waits=()):
        self.P.emit(self.pool, final_waits)
        self.P = None

    def mm(self, out, lhsT, rhs, start=True, stop=True, r=(), w=()):
        self.P.op("pe", lambda e: e.matmul(out, lhsT, rhs, start=start, stop=stop), r, w)

    def tr(self, out, in_, ident, r=(), w=()):
        self.P.op("pe", lambda e: e.transpose(out, in_, ident), r, w)

    def act(self, out, in_, func, bias=0.0, scale=1.0, r=(), w=()):
        self.P.op("act", lambda e: e.activation(out, in_, func, bias=bias, scale=scale), r, w)

    def tt(self, eng, out, in0, in1, op, r=(), w=()):
        self.P.op(eng, lambda e: e.tensor_tensor(out, in0, in1, op), r, w)

    def ts(self, eng, out, in0, s1, s2, op0, op1=None, r=(), w=()):
        if op1 is None:
            self.P.op(eng, lambda e: e.tensor_scalar(out, in0, s1, None, op0), r, w)
        else:
            self.P.op(eng, lambda e: e.tensor_scalar(out, in0, s1, s2, op0, op1), r, w)

    def stt(self, eng, out, in0, sc, in1, op0, op1, r=(), w=()):
        self.P.op(eng, lambda e: e.scalar_tensor_tensor(out, in0, sc, in1, op0, op1), r, w)

    def cp(self, eng, out, in_, r=(), w=()):
        if eng == "act":
            self.P.op("act", lambda e: e.copy(out, in_), r, w)
        else:
            self.P.op(eng, lambda e: e.tensor_copy(out, in_), r, w)

    def rcp(self, out, in_, r=(), w=()):
        self.P.op("dve", lambda e: e.reciprocal(out, in_), r, w)

    def red(self, out, in_, op, r=(), w=()):
        self.P.op("dve", lambda e: e.tensor_reduce(out, in_, AX.X, op), r, w)

    def memset(self, eng, ap, val, w=()):
        self.P.op(eng, lambda e: e.memset(ap, val), (), w)

    def dma(self, q, out, in_, r=(), w=(), sem=None):
        self.P.op(q, lambda e: e.dma_start(out=out, in_=in_), r, w, dma=sem)


def build_program(lo=0, hi=5):
    nc = bass.Bass("TRN2", target_bir_lowering=False)
    h = H(nc)

    def din(name, shape, dt=F32):
        return nc.dram_tensor(name, list(shape), dt, kind="ExternalInput").ap()

    xT_d = din("xT", [D, S])
    c_d = din("c_l", [128, DC])
    pos_d = din("pos", [1, S], I32)
    invf_d = din("invf", [128, 2])
    gvec_d = din("gvec", [128, 6 * DC])
    bmod_d = din("bmod", [128, 112])
    bf_d = din("bf_rep", [128, 256])
    rb_d = din("rb_rep", [128, 256])
    lg_d = din("lat_g", [128, 2])
    qg_d = din("q_g", [128, 6])
    w_mod_d = din("w_mod", [2, D, 6 * D])
    kv_w_mod_d = din("kv_w_mod", [D, 2 * D])
    a_w_in_d = din("a_w_in", [D, 3 * D + 16])
    a_w_o_d = din("a_w_o", [D, D])
    kv_w_down_d = din("kv_w_down", [D, 288])
    kv_w_up_d = din("kv_w_up", [256, 2048])
    b_w_dq_d = din("b_w_dq", [D, 768])
    b_w_uq_d = din("b_w_uq", [768, 1536])
    b_w_o_d = din("b_w_o", [D, D])
    router_w_d = din("router_w", [D, NE])
    wg_d = [din("exp_w_gate%d" % l_, [NE, D, 512]) for l_ in range(2)]
    wu_d = [din("exp_w_up%d" % l_, [NE, D, 512]) for l_ in range(2)]
    wd_d = [din("exp_w_down%d" % l_, [NE, 512, D]) for l_ in range(2)]
    outT_d = nc.dram_tensor("outT", [D, S], F32, kind="ExternalOutput").ap()

    with ExitStack() as G:
        _uid = [0]

        def sb(name, shape, dt, st=G):
            _uid[0] += 1
            return st.enter_context(nc.sbuf_tensor("s%d_%s" % (_uid[0], name), list(shape), dt))

        pb = [G.enter_context(nc.psum_tensor("pb%d" % i, [128, 512], F32)) for i in range(8)]
        h.pool = SemPool(nc, G)

        def PK(i):
            return ("ps", i)

        xT = sb("xT", [128, DC, S], F32)
        hT = sb("hT", [128, DC, S], BF16)
        ident = sb("ident", [128, 128], F32)
        ones_f = sb("ones_f", [128, 128], F32)
        ones_b = sb("ones_b", [128, 128], BF16)
        triu_f = sb("triu_f", [128, 128], F32)
        triu_b = sb("triu_b", [128, 128], BF16)
        cmask_b = sb("cmask_b", [128, 128], BF16)
        epst = sb("epst", [128, 1], F32)
        mod0 = sb("mod0", [128, 48], F32)
        mod1 = sb("mod1", [128, 48], F32)
        modkv = sb("modkv", [128, 16], F32)
        gvec = sb("gvec_s", [128, 6 * DC], F32)
        bmod = sb("bmod_s", [128, 112], F32)
        bf_rep = sb("bf_rep_s", [128, 256], F32)
        rb_rep = sb("rb_rep_s", [128, 256], F32)
        lat_g = sb("lat_g_s", [128, 2], F32)
        q_g = sb("q_g_s", [128, 6], F32)
        csT = sb("csT", [128, S], BF16)
        ckvT = sb("ckvT", [128, 2, S], BF16)
        krope = sb("krope", [128, S], BF16)
        ncoef = sb("ncoef", [128, 2 * DC], F32)
        cact = sb("cact", [128, DC], BF16)

        def tbs(tb):
            return slice(tb * 512, (tb + 1) * 512)

        def norm_phase(st, gidx, modt, sh0, sc0, nchunks=DC, src=None, dst=None, dst_keyf=None,
                       router=None):
            sq = [sb("sq%d" % i, [128, 512], BF16, st) for i in range(4)]
            rs = [sb("rs%d" % i, [128, 512], F32, st) for i in range(2)]
            t2 = [sb("t2_%d" % i, [128, 512], F32, st) for i in range(3)]
            h.stt("dve", ncoef[:, 0:DC], modt[:, sc0:sc0 + DC], 1.0, gvec[:, gidx * DC:(gidx + 1) * DC],
                  ALU.add, ALU.mult, r=["mod", "gvec"], w=["ncoef"])
            n = 0
            for tb in range(NB):
                pss = pb[tb % 2]
                for dc in range(DC):
                    q = sq[n % 4]
                    n += 1
                    if dc % 2 == 0:
                        h.act(q[:], xT[:, dc, tbs(tb)], AF.Square, r=[("xT", dc, tb)], w=[("sq", id(q))])
                    else:
                        h.tt("dve", q[:], xT[:, dc, tbs(tb)], xT[:, dc, tbs(tb)], ALU.mult, r=[("xT", dc, tb)], w=[("sq", id(q))])
                    h.mm(pss[:, :], ones_b[:, :], q[:], start=(dc == 0), stop=(dc == DC - 1),
                         r=[("sq", id(q)), "ones_b"], w=[PK(tb % 2)])
                r_ = rs[tb % 2]
                h.act(r_[:], pss[:, :], AF.Ln, bias=epst[:, 0:1], scale=1.0 / D, r=[PK(tb % 2), "epst"], w=[("rs", tb % 2)])
                h.act(r_[:], r_[:], AF.Exp, scale=-0.5, r=[("rs", tb % 2)], w=[("rs", tb % 2)])
                for dc in range(DC):
                    t = t2[n % 3]
                    n += 1
                    h.tt("dve", t[:], xT[:, dc, tbs(tb)], r_[:], ALU.mult, r=[("xT", dc, tb), ("rs", tb % 2)], w=[("t2", id(t))])
                    h.act(hT[:, dc, tbs(tb)], t[:], AF.Identity, bias=modt[:, sh0 + dc:sh0 + dc + 1],
                          scale=ncoef[:, dc:dc + 1], r=[("t2", id(t)), "ncoef", "mod"], w=[("hT", tb)])
                    if router is not None:
                        router(tb, dc, t)

        with ExitStack() as st:
            cs = sb("c_s", [128, DC], F32, st)
            wm = [sb("wm%d" % i, [128, 6 * D], BF16, st) for i in range(3)]
            posi = sb("posi", [128, S], I32, st)
            invf = sb("invf_s", [128, 2], F32, st)
            sinT = sb("sinT", [128, S], BF16, st)
            cosT = sb("cosT", [128, S], BF16, st)
            ang = sb("ang", [128, S], F32, st)
            ta = sb("ta", [128, S], F32, st)
            tb_ = sb("tb_", [128, S], F32, st)
            ti = sb("ti", [128, S], I32, st)
            h.begin()
            h.dma("sp", cs[:], c_d[:, :], w=["cs"], sem="cs")
            h.dma("sp", gvec[:], gvec_d[:, :], w=["gvec"], sem="ld_gvec")
            h.dma("sp", bmod[:], bmod_d[:, :], w=["bmod"], sem="ld_bmod")
            h.dma("sp", bf_rep[:], bf_d[:, :], w=["bf_rep"], sem="ld_bf_rep")
            h.dma("sp", rb_rep[:], rb_d[:, :], w=["rb_rep"], sem="ld_rb_rep")
            h.dma("sp", lat_g[:], lg_d[:, :], w=["lat_g"], sem="ld_lat_g")
            h.dma("sp", q_g[:], qg_d[:, :], w=["q_g"], sem="ld_q_g")
            h.dma("sp", invf[:], invf_d[:, :], w=["invf"], sem="ld_invf")
            h.dma("sp", posi[:], pos_d.partition_broadcast(128), w=["posi"], sem="ld_posi")
            h.memset("pool", ones_f[:], 1.0, w=["ones_f"])
            h.memset("pool", ones_b[:], 1.0, w=["ones_b"])
            h.memset("pool", epst[:], EPS, w=["epst"])
            h.P.op("pool", lambda e: e.affine_select(out=ident[:], in_=ones_f[:], pattern=[[-1, 128]], compare_op=ALU.is_equal,
                                                     fill=0.0, base=0, channel_multiplier=1), ["ones_f"], ["ident"])
            h.P.op("pool", lambda e: e.affine_select(out=triu_f[:], in_=ones_f[:], pattern=[[1, 128]], compare_op=ALU.is_ge,
                                                     fill=0.0, base=0, channel_multiplier=-1), ["ones_f"], ["triu_f"])
            h.cp("pool", triu_b[:], triu_f[:], r=["triu_f"], w=["triu_b"])
            h.memset("pool", cmask_b[:], 1.0, w=["cmask_b"])
            h.memset("pool", cmask_b[64:128, 0:64], 0.0, w=["cmask_b"])
            h.memset("pool", krope[:], 0.0, w=["krope"])
            for dc in range(DC):
                h.dma("sp", xT[:, dc, :], xT_d[dc * 128:(dc + 1) * 128, :], w=[("xT", dc, t) for t in range(NB)], sem=("xload", dc))
            h.act(cact[:], cs[:], AF.Silu, r=["cs"], w=["cact"])
            h.cp("dve", ang[:], posi[:], r=["posi"], w=["ang"])
            h.ts("dve", ang[:], ang[:], invf[:, 0:1], None, ALU.mult, r=["ang", "invf"], w=["ang"])
            for (dst, shift) in ((sinT, 0.0), (cosT, float(np.pi / 2))):
                if shift != 0.0:
                    h.ts("dve", ang[:], ang[:], shift, None, ALU.add, r=["ang"], w=["ang"])
                h.ts("dve", ta[:], ang[:], float(1.0 / (2 * np.pi)), None, ALU.mult, r=["ang"], w=["ta"])
                h.cp("dve", ti[:], ta[:], r=["ta"], w=["ti"])
                h.cp("dve", ta[:], ti[:], r=["ti"], w=["ta"])
                h.stt("dve", tb_[:], ta[:], float(-2 * np.pi), ang[:], ALU.mult, ALU.add, r=["ta", "ang"], w=["tb_"])
                h.ts("dve", ta[:], tb_[:], float(np.pi), float(-2 * np.pi), ALU.is_gt, ALU.mult, r=["tb_"], w=["ta"])
                h.tt("dve", tb_[:], tb_[:], ta[:], ALU.add, r=["tb_", "ta"], w=["tb_"])
                h.ts("dve", ta[:], tb_[:], float(-np.pi), float(2 * np.pi), ALU.is_lt, ALU.mult, r=["tb_"], w=["ta"])
                h.tt("dve", tb_[:], tb_[:], ta[:], ALU.add, r=["tb_", "ta"], w=["tb_"])
                h.act(dst[:], tb_[:], AF.Sin, r=["tb_"], w=[("trig", id(dst))])
            wi = 0
            h.cp("dve", mod1[:, :], bmod[:, 48:96], r=["bmod"], w=["mod1acc"])
            h.cp("dve", modkv[:, :], bmod[:, 96:112], r=["bmod"], w=["modkvacc"])
            for (wsrc, ncol, modt, boff) in ((w_mod_d[0], 48, mod0, 0),):
                psm = pb[2 + (wi % 2)]
                for kc in range(DC):
                    wt = wm[wi % 3]
                    wkey = ("wm", wi % 3)
                    wi += 1
                    h.dma("pool", wt[:, 0:ncol * 128], wsrc[kc * 128:(kc + 1) * 128, :], w=[wkey], sem=wkey)
                    for j in range(ncol):
                        h.mm(psm[:, kc * ncol + j:kc * ncol + j + 1], wt[:, j * 128:(j + 1) * 128], cact[:, kc:kc + 1],
                             r=[wkey, "cact"], w=[("psm", id(psm))])
                h.red(modt[:, 0:ncol], psm[:, 0:DC * ncol].rearrange("p (k j) -> p j k", k=DC), ALU.add, r=[("psm", id(psm))], w=["mod"])
                h.tt("dve", modt[:, 0:ncol], modt[:, 0:ncol], bmod[:, boff:boff + ncol], ALU.add, r=["mod", "bmod"], w=["mod"])
            h.memset("pool", csT[0:64, :], 0.0, w=["cs_lo"])
            h.cp("pool", csT[64:96, :], cosT[64:96, :], r=[("trig", id(cosT))], w=["rope_tab"])
            h.ts("dve", csT[96:128, :], sinT[96:128, :], invf[96:128, 1:2], None, ALU.mult, r=[("trig", id(sinT)), "invf"], w=["rope_tab2"])
            h.end()

        def attn_phase(layer):
            fox = layer == 0
            modt = mod0 if fox else mod1
            if fox:
                with ExitStack() as st0:
                    h.begin()
                    norm_phase(st0, 0, modt, 0, DC)
                    h.end()
            with ExitStack() as st:
                h.begin()
                cqT = hT
                NSET = 2
                qh = [[sb("qh%d_%d" % (s_, i), [128, S], BF16, st) for i in range(2)] for s_ in range(NSET)]
                kh = [[sb("kh%d_%d" % (s_, i), [128, S], BF16, st) for i in range(2)] for s_ in range(NSET)]
                Vp = [sb("Vp%d" % s_, [128, NT, 192], BF16, st) for s_ in range(NSET)]
                oT = [sb("oT%d" % i, [128, S], BF16, st) for i in range(2)]
                wo = [sb("wo%d" % i, [128, D], BF16, st) for i in range(3)]
                PT = [sb("PT%d" % i, [128, 512], BF16, st) for i in range(6)]
                rec = [sb("rec%d" % i, [128, 512], F32, st) for i in range(2)]
                zero_bias = sb("zero_bias", [128, 1], F32, st)
                h.memset("pool", zero_bias[:], 0.0, w=["zero_bias"])
                for s_ in range(NSET):
                    h.memset("pool", Vp[s_][:, :, 64:128], 1.0, w=[("Vp", s_)])
                    for i in range(2):
                        if fox:
                            h.memset("pool", kh[s_][i][64:65, :], 1.0, w=[("kh", s_, i)])
                        else:
                            h.cp("act" if i == 0 else "dve", kh[s_][i][64:128, :], krope[64:128, :], r=["krope"], w=[("kh", s_, i)])
                if fox:
                    wpair = [sb("wpair%d" % i, [128, DC, 384], BF16, st) for i in range(2)]
                    wf = sb("wf", [128, DC, 16], BF16, st)
                    lf = sb("lf", [128, 256], F32, st)
                    tot = sb("tot", [128, 256], F32, st)
                    off = sb("off", [128, 256], F32, st)
                    negcum = sb("negcum", [128, 256], F32, st)
                    cum8 = sb("cum8", [128, 256], F32, st)
                    cumT = sb("cumT", [16, S], BF16, st)
                    h.dma("pool", wf[:], a_w_in_d[:, 3 * D:3 * D + 16].rearrange("(dc p) c -> p dc c", p=128), w=["wf"], sem="wf")
                    for i in range(NT):
                        for dc in range(DC):
                            h.mm(pb[2][:, i * 16:(i + 1) * 16], hT[:, dc, i * 128:(i + 1) * 128], wf[:, dc, :],
                                 start=(dc == 0), stop=(dc == DC - 1), r=[("hT", i // 4), "wf"], w=[PK(2)])
                    h.tt("dve", lf[:], pb[2][:, 0:256], bf_rep[:], ALU.add, r=[PK(2), "bf_rep"], w=["lf"])
                    h.act(lf[:], lf[:], AF.Sigmoid, r=["lf"], w=["lf"])
                    h.act(lf[:], lf[:], AF.Ln, r=["lf"], w=["lf"])
                    h.mm(pb[3][:, 0:256], triu_f[:], lf[:], r=["triu_f", "lf"], w=[PK(3)])
                    h.mm(pb[2][:, 0:256], ones_f[:], lf[:], r=["ones_f", "lf"], w=[PK(2)])
                    h.cp("dve", tot[:], pb[2][:, 0:256], r=[PK(2)], w=["tot"])
                    h.memset("dve", off[:, 0:16], 0.0, w=["off"])
                    for i in range(1, NT):
                        h.tt("dve", off[:, i * 16:(i + 1) * 16], off[:, (i - 1) * 16:i * 16], tot[:, (i - 1) * 16:i * 16], ALU.add,
                             r=["off", "tot"], w=["off"])
                    h.tt("dve", cum8[:], pb[3][:, 0:256], off[:], ALU.add, r=[PK(3), "off"], w=["cum8"])
                    h.ts("dve", negcum[:], cum8[:], -1.0, None, ALU.mult, r=["cum8"], w=["negcum"])
                    h.ts("dve", cum8[:], cum8[:], 8.0, None, ALU.mult, r=["cum8"], w=["cum8"])
                    for i in range(NT):
                        bk = 4 + (i // 4) % 2
                        h.tr(pb[bk][0:16, (i % 4) * 128:(i % 4 + 1) * 128], cum8[:, i * 16:(i + 1) * 16], ident[:],
                             r=["cum8", "ident"], w=[PK(bk)])
                        if i % 4 == 3:
                            h.cp("act", cumT[:, (i // 4) * 512:(i // 4 + 1) * 512], pb[bk][0:16, :], r=[PK(bk)], w=["cumT"])
                else:
                    wq = [sb("wq%d" % i, [128, 6, 256], BF16, st) for i in range(2)]
                    wkv = [sb("wkv%d" % i, [128, 2, 256], BF16, st) for i in range(2)]

                g1c = 2 * DC
                w_o_d = a_w_o_d if fox else b_w_o_d
                scale = 0.125 if fox else float(96 ** -0.5)
                mask = triu_b if fox else cmask_b
                Kc = 65 if fox else 128
                cnt = {"ps_s": 0, "pt": 0, "ps_o": 0, "rec": 0, "op": 0}
                NPT = 6

                bgc = {"bk": 0}

                def bgbank():
                    bgc["bk"] += 1
                    return 5 + bgc["bk"] % 3

                def issue_weights(p):
                    wb = p % 2
                    h.dma("pool", wo[p % 3][:], w_o_d[p * 128:(p + 1) * 128, :], w=[("wo", p % 3)], sem=("wo", p % 3))
                    if fox:
                        for part in range(3):
                            c0 = part * D + p * 128
                            h.dma("pool", wpair[wb][:, :, part * 128:(part + 1) * 128],
                                  a_w_in_d[:, c0:c0 + 128].rearrange("(dc p) c -> p dc c", p=128), w=[("wpair", wb, part)], sem=("wpair", wb, part))
                    else:
                        for hh in range(2):
                            hd = 2 * p + hh
                            uq = b_w_uq_d[:, hd * 96:(hd + 1) * 96].rearrange("(kc p) c -> p kc c", p=128)
                            h.dma("pool", wq[wb][:, :, hh * 128:hh * 128 + 64], uq[:, :, 0:64], w=[("wq", wb, hh, 0)], sem=("wq", wb, hh, 0))
                            h.dma("pool", wq[wb][:, :, hh * 128 + 64:hh * 128 + 96], uq[:, :, 64:96], w=[("wq", wb, hh, 1)], sem=("wq", wb, hh, 1))
                            h.dma("pool", wq[wb][:, :, hh * 128 + 96:hh * 128 + 112], uq[:, :, 80:96], w=[("wq", wb, hh, 2)], sem=("wq", wb, hh, 2))
                            h.dma("pool", wq[wb][:, :, hh * 128 + 112:hh * 128 + 128], uq[:, :, 64:80], w=[("wq", wb, hh, 3)], sem=("wq", wb, hh, 3))
                            up = kv_w_up_d[:, hd * 128:(hd + 1) * 128].rearrange("(c p) n -> p c n", p=128)
                            h.dma("pool", wkv[wb][:, :, hh * 64:(hh + 1) * 64], up[:, :, 0:64], w=[("wkv", wb, hh, 0)], sem=("wkv", wb, hh, 0))
                            h.dma("pool", wkv[wb][:, :, 128 + hh * 64:128 + (hh + 1) * 64], up[:, :, 64:128], w=[("wkv", wb, hh, 1)], sem=("wkv", wb, hh, 1))

                def proj_items(p):
                    s_ = p % NSET
                    wb = p % 2
                    items = []

                    def v_item(g):
                        def f():
                            bk = bgbank()
                            for i in range(4 * g, 4 * g + 4):
                                if fox:
                                    for dc in range(DC):
                                        h.mm(pb[bk][:, (i % 4) * 128:(i % 4 + 1) * 128], hT[:, dc, i * 128:(i + 1) * 128], wpair[wb][:, dc, 256:384],
                                             start=(dc == 0), stop=(dc == DC - 1), r=[("wpair", wb, 2), ("hT", i // 4)], w=[PK(bk)])
                                else:
                                    for c in range(2):
                                        h.mm(pb[bk][:, (i % 4) * 128:(i % 4 + 1) * 128], ckvT[:, c, i * 128:(i + 1) * 128], wkv[wb][:, c, 128:256],
                                             start=(c == 0), stop=(c == 1), r=[("wkv", wb, 0, 1), ("wkv", wb, 1, 1), "ckvT"], w=[PK(bk)])
                            pv = pb[bk][:, :].rearrange("p (i c) -> p i c", c=128)
                            h.cp("act", Vp[s_][:, 4 * g:4 * g + 4, 0:64], pv[:, :, 0:64], r=[PK(bk)], w=[("Vp", s_)])
                            h.cp("dve", Vp[s_][:, 4 * g:4 * g + 4, 128:192], pv[:, :, 64:128], r=[PK(bk)], w=[("Vp", s_)])
                        return f

                    if fox:
                        def qk_item(which, tb):
                            def f():
                                nm = "qh" if which == 0 else "kh"
                                dsts = qh[s_] if which == 0 else kh[s_]
                                bk = bgbank()
                                for dc in range(DC):
                                    h.mm(pb[bk][:, :], wpair[wb][:, dc, which * 128:(which + 1) * 128], hT[:, dc, tbs(tb)],
                                         start=(dc == 0), stop=(dc == DC - 1), r=[("wpair", wb, which), ("hT", tb)], w=[PK(bk)])
                                h.cp("act", dsts[0][0:64, tbs(tb)], pb[bk][0:64, :], r=[PK(bk)], w=[(nm, s_, 0)])
                                h.cp("dve", dsts[1][0:64, tbs(tb)], pb[bk][64:128, :], r=[PK(bk)], w=[(nm, s_, 1)])
                            return f

                        def aug_item():
                            for hh in range(2):
                                h.dma("sp", qh[s_][hh][64:65, :], cumT[2 * p + hh:2 * p + hh + 1, :], r=["cumT"], w=[("qh", s_, hh)], sem=("aug", s_, hh))
                        items.append(aug_item)
                        for which in range(2):
                            for tb in range(NB):
                                items.append(qk_item(which, tb))
                    else:
                        def q_item(hh, tb):
                            def f():
                                bk = bgbank()
                                for kc in range(6):
                                    h.mm(pb[bk][:, :], wq[wb][:, kc, hh * 128:(hh + 1) * 128], cqT[:, kc, tbs(tb)],
                                         start=(kc == 0), stop=(kc == 5),
                                         r=[("wq", wb, hh, 0), ("wq", wb, hh, 1), ("wq", wb, hh, 2), ("wq", wb, hh, 3), ("hT", tb)], w=[PK(bk)])
                                h.cp("act", qh[s_][hh][0:64, tbs(tb)], pb[bk][0:64, :], r=[PK(bk)], w=[("qh", s_, hh)])
                                h.tt("dve", qh[s_][hh][64:128, tbs(tb)], pb[bk][64:128, :], csT[64:128, tbs(tb)], ALU.mult,
                                     r=[PK(bk), "rope_tab", "rope_tab2"], w=[("qh", s_, hh)])
                            return f

                        def k_item(tb):
                            def f():
                                bk = bgbank()
                                for c in range(2):
                                    h.mm(pb[bk][:, :], wkv[wb][:, c, 0:128], ckvT[:, c, tbs(tb)], start=(c == 0), stop=(c == 1),
                                         r=[("wkv", wb, 0, 0), ("wkv", wb, 1, 0), "ckvT"], w=[PK(bk)])
                                h.cp("act", kh[s_][0][0:64, tbs(tb)], pb[bk][0:64, :], r=[PK(bk)], w=[("kh", s_, 0)])
                                h.cp("dve", kh[s_][1][0:64, tbs(tb)], pb[bk][64:128, :], r=[PK(bk)], w=[("kh", s_, 1)])
                            return f
                        for hh in range(2):
                            for tb in range(NB):
                                items.append(q_item(hh, tb))
                        for tb in range(NB):
                            items.append(k_item(tb))
                    for g in range(4):
                        items.append(v_item(g))
                    return items

                def outproj_items(p):
                    wb = p % 3
                    ob = p % 2
                    items = []

                    def o_item(tb, dcol):
                        def f():
                            bo = bgbank()
                            h.mm(pb[bo][:, :], wo[wb][:, dcol * 128:(dcol + 1) * 128], oT[ob][:, tbs(tb)],
                                 r=[("wo", wb), ("oT", ob, tb)], w=[PK(bo)])
                            h.stt("dve", xT[:, dcol, tbs(tb)], pb[bo][:, :], modt[:, g1c + dcol:g1c + dcol + 1], xT[:, dcol, tbs(tb)],
                                  ALU.mult, ALU.add, r=[PK(bo), "mod", ("xT", dcol, tb)], w=[("xT", dcol, tb)])
                        return f
                    for tb in range(NB):
                        for dcol in range(DC):
                            items.append(o_item(tb, dcol))
                    return items

                def attn_steps(p):
                    s_ = p % NSET
                    ob = p % 2
                    steps = []
                    for hh in range(2):
                        for qb in range(NB):
                            nkt = 4 * qb + 4
                            ob_k = 3 + cnt["ps_o"] % 2
                            cnt["ps_o"] += 1
                            for kt in range(nkt):
                                steps.append((hh, qb, kt, nkt, ob_k, cnt["ps_s"] % 3, cnt["pt"] % NPT))
                                cnt["ps_s"] += 1
                                cnt["pt"] += 1

                    def emit_S(stp):
                        hh, qb, kt, nkt, ob_k, sk, pk = stp
                        hd = 2 * p + hh
                        qt, kt_ = qh[s_][hh], kh[s_][hh]
                        jj = kt - 4 * qb
                        n0 = max(0, jj) * 128
                        h.mm(pb[sk][:, n0:512], kt_[0:Kc, kt * 128:(kt + 1) * 128], qt[0:Kc, qb * 512 + n0:(qb + 1) * 512],
                             r=[("kh", s_, hh), ("qh", s_, hh)], w=[PK(sk)])
                        if fox:
                            bias = negcum[:, kt * 16 + hd:kt * 16 + hd + 1]
                            rk = [PK(sk), "negcum"]
                        else:
                            bias = zero_bias[:, 0:1]
                            rk = [PK(sk), "zero_bias"]
                        h.act(PT[pk][:, n0:512], pb[sk][:, n0:512], AF.Exp, bias=bias, scale=scale, r=rk, w=[("PT", pk)])
                        if jj >= 0:
                            h.tt("pool", PT[pk][:, n0:n0 + 128], PT[pk][:, n0:n0 + 128], mask[:], ALU.mult,
                                 r=[("PT", pk), "mask"], w=[("PT", pk)])

                    def emit_PV(stp):
                        hh, qb, kt, nkt, ob_k, sk, pk = stp
                        jj = kt - 4 * qb
                        n0 = max(0, jj) * 128
                        vl = Vp[s_][:, kt, 0:128] if hh == 0 else Vp[s_][:, kt, 64:192]
                        h.mm(pb[ob_k][:, n0:512], vl, PT[pk][:, n0:512], start=(kt == 0), stop=(kt == nkt - 1),
                             r=[("Vp", s_), ("PT", pk)], w=[PK(ob_k)])
                        if kt == nkt - 1:
                            rc = cnt["rec"] % 2
                            cnt["rec"] += 1
                            if hh == 0:
                                h.act(rec[rc][0:64, :], pb[ob_k][64:128, :], AF.Ln, r=[PK(ob_k)], w=[("rec", rc)])
                                h.act(rec[rc][0:64, :], rec[rc][0:64, :], AF.Exp, scale=-1.0, r=[("rec", rc)], w=[("rec", rc)])
                                h.tt("dve", oT[ob][0:64, tbs(qb)], pb[ob_k][0:64, :], rec[rc][0:64, :], ALU.mult,
                                     r=[PK(ob_k), ("rec", rc)], w=[("oT", ob, qb)])
                            else:
                                h.act(rec[rc][64:128, :], pb[ob_k][0:64, :], AF.Ln, r=[PK(ob_k)], w=[("rec", rc)])
                                h.act(rec[rc][64:128, :], rec[rc][64:128, :], AF.Exp, scale=-1.0, r=[("rec", rc)], w=[("rec", rc)])
                                h.tt("dve", oT[ob][64:128, tbs(qb)], pb[ob_k][64:128, :], rec[rc][64:128, :], ALU.mult,
                                     r=[PK(ob_k), ("rec", rc)], w=[("oT", ob, qb)])
                    return steps, emit_S, emit_PV

                LOOK = 2
                issue_weights(0)
                for it in proj_items(0):
                    it()
                bg = []
                for p in range(8):
                    if p + 1 < 8:
                        issue_weights(p + 1)
                        bg = bg + proj_items(p + 1)
                    steps, emit_S, emit_PV = attn_steps(p)
                    nst = len(steps)
                    nbg = len(bg)
                    done = 0
                    for i_ in range(nst + LOOK):
                        if i_ < nst:
                            emit_S(steps[i_])
                        if i_ >= LOOK:
                            emit_PV(steps[i_ - LOOK])
                        tgt = (nbg * (i_ + 1)) // (nst + LOOK)
                        while done < tgt:
                            bg[done]()
                            done += 1
                    while done < nbg:
                        bg[done]()
                        done += 1
                    bg = outproj_items(p)
                for it in bg:
                    it()
                h.end()

        def moe_phase(layer):
            modt = mod0 if layer == 0 else mod1
            g2c = 5 * DC
            with ExitStack() as st:
                selE = sb("selE", [16, NE, 128], BF16, st)
                combT = sb("combT", [16, S], BF16, st)
                with ExitStack() as st2:
                    rw = sb("rw", [128, DC, NE], F32, st2)
                    h32 = sb("h32", [128, DC, 512], F32, st2)
                    R = {n: sb("r_" + n, [128, 256], F32, st2) for n in ("sc", "sel", "a", "b", "c", "top2", "w")}
                    gs = sb("gs", [128, 64], F32, st2)
                    gtmp = sb("gtmp", [128, 64], F32, st2)
                    gmax = sb("gmax", [128, 16], F32, st2)
                    ohg = sb("ohg", [128, 64], F32, st2)
                    wsum = sb("wsum", [128, 16], F32, st2)
                    h.begin()
                    h.dma("sp", rw[:], router_w_d[:, :].rearrange("(dc p) e -> p dc e", p=128), w=["rw"], sem="rw")
                    for e_ in range(NE):
                        h.ts("pool", selE[:, e_, :], ones_f[0:16, :], ident[0:16, e_:e_ + 1], None, ALU.mult, r=["ones_f", "ident"], w=["selE"])

                    def router(tb, dc, t):
                        h.ts("dve", h32[:, dc, :], t[:], ncoef[:, dc:dc + 1], modt[:, 3 * DC + dc:3 * DC + dc + 1], ALU.mult, ALU.add,
                             r=[("t2", id(t)), "ncoef", "mod"], w=[("h32", dc)])
                        if dc == DC - 1:
                            for i in range(4):
                                ti_ = tb * 4 + i
                                for d2 in range(DC):
                                    h.mm(pb[2][:, ti_ * 16:(ti_ + 1) * 16], h32[:, d2, i * 128:(i + 1) * 128], rw[:, d2, :],
                                         start=(d2 == 0), stop=(d2 == DC - 1), r=[("h32", d2), "rw"], w=[PK(2)])

                    norm_phase(st2, 1 if layer == 0 else 4, modt, 3 * DC, 4 * DC, router=router)
                    sc, sel, A_, B_, C_, top2, w_ = (R[n] for n in ("sc", "sel", "a", "b", "c", "top2", "w"))
                    h.act(sc[:], pb[2][:, 0:256], AF.Sigmoid, r=[PK(2)], w=["r_sc"])
                    h.tt("dve", sel[:], sc[:], rb_rep[:], ALU.add, r=["r_sc", "rb_rep"], w=["r_sel"])
                    X = sel[:].rearrange("p (t e) -> p t e", e=4)
                    first = True
                    for (a, b) in ((0, 1), (0, 2), (0, 3), (1, 2), (1, 3), (2, 3)):
                        if first:
                            h.tt("dve", gs[:], X[:, :, a], X[:, :, b], ALU.add, r=["r_sel"], w=["gs"])
                            first = False
                        else:
                            h.tt("dve", gtmp[:], X[:, :, a], X[:, :, b], ALU.add, r=["r_sel"], w=["gtmp"])
                            h.tt("dve", gs[:], gs[:], gtmp[:], ALU.max, r=["gs", "gtmp"], w=["gs"])
                    G4 = gs[:].rearrange("p (t g) -> p t g", g=4)
                    h.tt("dve", gmax[:], G4[:, :, 0], G4[:, :, 1], ALU.max, r=["gs"], w=["gmax"])
                    h.tt("dve", gmax[:], gmax[:], G4[:, :, 2], ALU.max, r=["gs", "gmax"], w=["gmax"])
                    h.tt("dve", gmax[:], gmax[:], G4[:, :, 3], ALU.max, r=["gs", "gmax"], w=["gmax"])
                    O4 = ohg[:].rearrange("p (t g) -> p t g", g=4)
                    for g in range(4):
                        h.tt("dve", O4[:, :, g], G4[:, :, g], gmax[:], ALU.is_equal, r=["gs", "gmax"], w=["ohg"])
                    T2 = top2[:].rearrange("p (t e) -> p t e", e=4)
                    A3 = A_[:].rearrange("p (t e) -> p t e", e=4)
                    B3 = B_[:].rearrange("p (t e) -> p t e", e=4)
                    for e_ in range(4):
                        oth = [x for x in range(4) if x != e_]
                        h.tt("dve", A3[:, :, e_], X[:, :, oth[0]], X[:, :, e_], ALU.is_gt, r=["r_sel"], w=["r_a"])
                        h.tt("dve", B3[:, :, e_], X[:, :, oth[1]], X[:, :, e_], ALU.is_gt, r=["r_sel"], w=["r_b"])
                        h.tt("dve", A3[:, :, e_], A3[:, :, e_], B3[:, :, e_], ALU.add, r=["r_a", "r_b"], w=["r_a"])
                        h.tt("dve", B3[:, :, e_], X[:, :, oth[2]], X[:, :, e_], ALU.is_gt, r=["r_sel"], w=["r_b"])
                        h.tt("dve", A3[:, :, e_], A3[:, :, e_], B3[:, :, e_], ALU.add, r=["r_a", "r_b"], w=["r_a"])
                        h.ts("dve", T2[:, :, e_], A3[:, :, e_], 1.5, None, ALU.is_lt, r=["r_a"], w=["r_top2"])
                        h.tt("dve", T2[:, :, e_], T2[:, :, e_], ohg[:], ALU.mult, r=["r_top2", "ohg"], w=["r_top2"])
                    h.tt("dve", w_[:], sc[:], top2[:], ALU.mult, r=["r_sc", "r_top2"], w=["r_w"])
                    h.red(wsum[:], w_[:].rearrange("p (t e) -> p t e", e=16), ALU.add, r=["r_w"], w=["wsum"])
                    h.rcp(wsum[:], wsum[:], r=["wsum"], w=["wsum"])
                    for i in range(NT):
                        h.ts("dve", C_[:, i * 16:(i + 1) * 16], w_[:, i * 16:(i + 1) * 16], wsum[:, i:i + 1], None, ALU.mult,
                             r=["r_w", "wsum"], w=["r_c"])
                    for i in range(NT):
                        bk = 4 + (i // 4) % 2
                        h.tr(pb[bk][0:16, (i % 4) * 128:(i % 4 + 1) * 128], C_[:, i * 16:(i + 1) * 16], ident[:],
                             r=["r_c", "ident"], w=[PK(bk)])
                        if i % 4 == 3:
                            h.cp("act", combT[:, (i // 4) * 512:(i // 4 + 1) * 512], pb[bk][0:16, :], r=[PK(bk)], w=["combT"])
                    h.end()
                if layer == 1 and globals().get("_MOE1_SKIP_B", False):
                    return
                wg = [sb("wg%d" % i, [128, DC, 512], BF16, st) for i in range(2)]
                wu = [sb("wu%d" % i, [128, DC, 512], BF16, st) for i in range(2)]
                wd = [sb("wd%d" % i, [128, 4, D], BF16, st) for i in range(2)]
                aT = [sb("aT%d" % i, [128, 4, 512], BF16, st) for i in range(2)]
                sg = [sb("sg%d" % i, [128, 512], F32, st) for i in range(3)]
                cmb = [sb("cmb%d" % i, [128, 512], F32, st) for i in range(2)]
                h.begin()
                n_g = 0
                n_y = 0
                n_it = 0
                mod_items = []
                if layer == 0:
                    wmc = [sb("wmc%d" % i, [128, 1024], BF16, st) for i in range(2)]
                    for (wsrc, nblk, modt_, mkey) in ((w_mod_d[1], 6, mod1, "mod1acc"), (kv_w_mod_d, 2, modkv, "modkvacc")):
                        for blk in range(nblk):
                            for kc in range(DC):
                                mod_items.append((wsrc, blk, kc, modt_, mkey))

                def issue_mod_dma(n):
                    wsrc, blk, kc, modt_, mkey = mod_items[n]
                    wkey = ("wmc", n % 2)
                    h.dma("pool", wmc[n % 2][:], wsrc[kc * 128:(kc + 1) * 128, blk * 1024:(blk + 1) * 1024], w=[wkey], sem=wkey)

                if mod_items:
                    issue_mod_dma(0)

                def run_mod_item(n):
                    wsrc, blk, kc, modt_, mkey = mod_items[n]
                    wt = wmc[n % 2]
                    wkey = ("wmc", n % 2)
                    if n + 1 < len(mod_items):
                        issue_mod_dma(n + 1)
                    for j in range(8):
                        h.mm(pb[0][:, j:j + 1], wt[:, j * 128:(j + 1) * 128], cact[:, kc:kc + 1], r=[wkey, "cact"], w=[PK(0)])
                    h.tt("dve", modt_[:, blk * 8:(blk + 1) * 8], modt_[:, blk * 8:(blk + 1) * 8], pb[0][:, 0:8], ALU.add,
                         r=[PK(0), mkey], w=[mkey])

                def issue_expert(e2):
                    w2 = e2 % 2
                    h.dma("pool", wg[w2][:], wg_d[layer][e2].rearrange("(dc p) f -> p dc f", p=128), w=[("wg", w2)], sem=("wg", w2))
                    h.dma("pool", wu[w2][:], wu_d[layer][e2].rearrange("(dc p) f -> p dc f", p=128), w=[("wu", w2)], sem=("wu", w2))
                    h.dma("pool", wd[w2][:], wd_d[layer][e2].rearrange("(fc p) d -> p fc d", p=128), w=[("wd", w2)], sem=("wd", w2))

                issue_expert(0)
                for e_ in range(NE):
                    wb = e_ % 2
                    if e_ + 1 < NE:
                        issue_expert(e_ + 1)
                    for tb in range(NB):
                        ab = n_it % 2
                        n_it += 1
                        h.mm(pb[0][:, :], selE[:, e_, :], combT[:, tbs(tb)], r=["selE", "combT"], w=[PK(0)])
                        h.cp("act", cmb[ab][:], pb[0][:, :], r=[PK(0)], w=[("cmb", ab)])
                        for fc in range(4):
                            bg = 1 + n_g % 2
                            bu = 3 + n_g % 2
                            sgi = n_g % 3
                            n_g += 1
                            for dc in range(DC):
                                h.mm(pb[bg][:, :], wg[wb][:, dc, fc * 128:(fc + 1) * 128], hT[:, dc, tbs(tb)],
                                     start=(dc == 0), stop=(dc == DC - 1), r=[("wg", wb), ("hT", tb)], w=[PK(bg)])
                            for dc in range(DC):
                                h.mm(pb[bu][:, :], wu[wb][:, dc, fc * 128:(fc + 1) * 128], hT[:, dc, tbs(tb)],
                                     start=(dc == 0), stop=(dc == DC - 1), r=[("wu", wb), ("hT", tb)], w=[PK(bu)])
                            h.act(sg[sgi][:], pb[bg][:, :], AF.Silu, r=[PK(bg)], w=[("sg", sgi)])
                            h.tt("dve", sg[sgi][:], sg[sgi][:], cmb[ab][:], ALU.mult, r=[("sg", sgi), ("cmb", ab)], w=[("sg", sgi)])
                            h.tt("dve", aT[ab][:, fc, :], pb[bu][:, :], sg[sgi][:], ALU.mult, r=[PK(bu), ("sg", sgi)], w=[("aT", ab, fc)])
                            if fc == 1 and n_it - 1 < len(mod_items):
                                run_mod_item(n_it - 1)
                        for dcol in range(DC):
                            by = 5 + n_y % 3
                            n_y += 1
                            for fc in range(4):
                                h.mm(pb[by][:, :], wd[wb][:, fc, dcol * 128:(dcol + 1) * 128], aT[ab][:, fc, :],
                                     start=(fc == 0), stop=(fc == 3), r=[("wd", wb), ("aT", ab, fc)], w=[PK(by)])
                            h.stt("dve", xT[:, dcol, tbs(tb)], pb[by][:, :], modt[:, g2c + dcol:g2c + dcol + 1], xT[:, dcol, tbs(tb)],
                                  ALU.mult, ALU.add, r=[PK(by), "mod", ("xT", dcol, tb)], w=[("xT", dcol, tb)])
                h.end()

        def kv_phase():
            with ExitStack() as st:
                wkd = sb("wkd", [128, DC, 256], BF16, st)
                wkr = sb("wkr", [128, DC, 128], BF16, st)
                craw = sb("kraw", [128, 2, 512], F32, st)
                csq = [sb("ksq%d" % i, [128, 512], BF16, st) for i in range(2)]
                crs = sb("krs", [128, 512], F32, st)
                ra = [sb("kra%d" % i, [128, 512], F32, st) for i in range(2)]
                rb = [sb("krb%d" % i, [128, 512], F32, st) for i in range(2)]
                h.begin()
                h.memset("pool", wkr[:, :, 0:64], 0.0, w=["wkr_z"])
                dn = kv_w_down_d[:, :].rearrange("(dc p) c -> p dc c", p=128)
                h.dma("pool", wkd[:], dn[:, :, 0:256], w=["wkd"], sem="wkd")
                h.dma("pool", wkr[:, :, 64:96], dn[:, :, 256:288], w=["wkr0"], sem="wkr0")
                h.dma("pool", wkr[:, :, 96:112], dn[:, :, 272:288], w=["wkr1"], sem="wkr1")
                h.dma("pool", wkr[:, :, 112:128], dn[:, :, 256:272], w=["wkr2"], sem="wkr2")
                norm_phase(st, 2, modkv, 0, DC)
                for tb in range(NB):
                    for c in range(2):
                        bk = 2 + c
                        for dc in range(DC):
                            h.mm(pb[bk][:, :], wkd[:, dc, c * 128:(c + 1) * 128], hT[:, dc, tbs(tb)], start=(dc == 0), stop=(dc == DC - 1),
                                 r=["wkd", ("hT", tb)], w=[PK(bk)])
                        h.cp("act", craw[:, c, :], pb[bk][:, :], r=[PK(bk)], w=[("kraw", c)])
                        h.tt("dve", csq[c][:], craw[:, c, :], craw[:, c, :], ALU.mult, r=[("kraw", c)], w=[("ksq", c)])
                        h.mm(pb[4][:, :], ones_b[:, :], csq[c][:], start=(c == 0), stop=(c == 1), r=[("ksq", c), "ones_b"], w=[PK(4)])
                    h.act(crs[:], pb[4][:, :], AF.Ln, bias=epst[:, 0:1], scale=1.0 / 256, r=[PK(4), "epst"], w=["krs"])
                    h.act(crs[:], crs[:], AF.Exp, scale=-0.5, r=["krs"], w=["krs"])
                    for c in range(2):
                        h.stt("dve", ckvT[:, c, tbs(tb)], craw[:, c, :], lat_g[:, c:c + 1], crs[:], ALU.mult, ALU.mult,
                              r=[("kraw", c), "krs", "lat_g"], w=["ckvT"])
                    bk = 5 + tb % 2
                    for dc in range(DC):
                        h.mm(pb[bk][:, :], wkr[:, dc, :], hT[:, dc, tbs(tb)], start=(dc == 0), stop=(dc == DC - 1),
                             r=["wkr_z", "wkr0", "wkr1", "wkr2", ("hT", tb)], w=[PK(bk)])
                    cs_ = tbs(tb)
                    a_, b_ = ra[tb % 2], rb[tb % 2]
                    h.tt("dve", a_[64:128, :], pb[bk][64:128, :], csT[64:128, cs_], ALU.mult, r=[PK(bk), "rope_tab", "rope_tab2"], w=[("kra", tb % 2)])
                    h.cp("act", b_[64:96, :], a_[96:128, :], r=[("kra", tb % 2)], w=[("krb", tb % 2)])
                    h.tt("dve", krope[64:96, cs_], a_[64:96, :], b_[64:96, :], ALU.add, r=[("kra", tb % 2), ("krb", tb % 2)], w=[("krope", tb)])
                    h.cp("act", krope[96:128, cs_], krope[64:96, cs_], r=[("krope", tb)], w=[("kropeB", tb)])
                norm_phase(st, 3, mod1, 0, DC)
                wdq = sb("wdq", [128, DC, 768], BF16, st)
                craw = sb("craw", [128, 6, 512], F32, st)
                csq = [sb("csq%d" % i, [128, 512], BF16, st) for i in range(2)]
                crs = sb("crs", [128, 512], F32, st)
                h.dma("pool", wdq[:], b_w_dq_d[:, :].rearrange("(dc p) c -> p dc c", p=128), w=["wdq"], sem="wdq")
                for tb in range(NB):
                    pend = None
                    for c in range(6):
                        bk = 2 + c % 2
                        for dc in range(DC):
                            h.mm(pb[bk][:, :], wdq[:, dc, c * 128:(c + 1) * 128], hT[:, dc, tbs(tb)],
                                 start=(dc == 0), stop=(dc == DC - 1), r=["wdq", ("hT", tb)], w=[PK(bk)])
                        if pend is not None:
                            pend()
                        h.cp("act", craw[:, c, :], pb[bk][:, :], r=[PK(bk)], w=[("craw", c)])
                        q_ = csq[c % 2]
                        h.tt("dve", q_[:], craw[:, c, :], craw[:, c, :], ALU.mult, r=[("craw", c)], w=[("csq", c % 2)])

                        def pend(c=c, q_=q_):
                            h.mm(pb[4][:, :], ones_b[:, :], q_[:], start=(c == 0), stop=(c == 5), r=[("csq", c % 2), "ones_b"], w=[PK(4)])
                    pend()
                    h.act(crs[:], pb[4][:, :], AF.Ln, bias=epst[:, 0:1], scale=1.0 / 768, r=[PK(4), "epst"], w=["crs"])
                    h.act(crs[:], crs[:], AF.Exp, scale=-0.5, r=["crs"], w=["crs"])
                    for c in range(6):
                        h.stt("dve", hT[:, c, tbs(tb)], craw[:, c, :], q_g[:, c:c + 1], crs[:], ALU.mult, ALU.mult,
                              r=[("craw", c), "crs", "q_g"], w=[("hT", tb)])
                h.end()

        def final_phase(do_norm):
            with ExitStack() as st:
                sq = [sb("fsq%d" % i, [128, 512], BF16, st) for i in range(3)]
                rs = [sb("frs%d" % i, [128, 512], F32, st) for i in range(2)]
                ob = [sb("fob%d" % i, [128, 512], F32, st) for i in range(4)]
                h.begin()
                for _i in range(globals().get("_DUMMY", 0)):
                    _de = globals().get("_DUMMY_ENG", "pe")
                    if _de == "pe":
                        h.mm(pb[7][:, 0:16], ones_b[:, :], ones_b[:, 0:16], r=["ones_b"], w=[PK(7)])
                    elif _de == "dve":
                        h.memset("dve", sq[0][:, 0:8], 0.0, w=[])
                    elif _de == "actbig":
                        h.act(hT[:, 0, :], xT[:, 0, :], AF.Copy, r=[], w=[])
                    elif _de == "pooldma":
                        h.dma("pool", sq[2][:, 0:16], a_w_o_d[0:128, 0:16], w=["dummy_dma"], sem="dummy_dma")
                    else:
                        h.act(sq[1][:, 0:8], ones_b[:, 0:8], AF.Copy, r=[], w=[])
                n = 0
                for tb in range(NB):
                    if do_norm:
                        pss = pb[tb % 2]
                        for dc in range(DC):
                            q = sq[n % 3]
                            n += 1
                            if dc % 2 == 0:
                                h.act(q[:], xT[:, dc, tbs(tb)], AF.Square, r=[("xT", dc, tb)], w=[("sq", id(q))])
                            else:
                                h.tt("dve", q[:], xT[:, dc, tbs(tb)], xT[:, dc, tbs(tb)], ALU.mult, r=[("xT", dc, tb)], w=[("sq", id(q))])
                            h.mm(pss[:, :], ones_b[:, :], q[:], start=(dc == 0), stop=(dc == DC - 1), r=[("sq", id(q)), "ones_b"], w=[PK(tb % 2)])
                        r_ = rs[tb % 2]
                        h.act(r_[:], pss[:, :], AF.Ln, bias=epst[:, 0:1], scale=1.0 / D, r=[PK(tb % 2), "epst"], w=[("rs", tb % 2)])
                        h.act(r_[:], r_[:], AF.Exp, scale=-0.5, r=[("rs", tb % 2)], w=[("rs", tb % 2)])
                    for dc in range(DC):
                        o = ob[n % 4]
                        ok = ("ob", n % 4)
                        n += 1
                        if do_norm:
                            h.stt("dve", o[:], xT[:, dc, tbs(tb)], gvec[:, 5 * DC + dc:5 * DC + dc + 1], r_[:], ALU.mult, ALU.mult,
                                  r=[("xT", dc, tb), "gvec", ("rs", tb % 2)], w=[ok])
                        else:
                            h.cp("dve", o[:], xT[:, dc, tbs(tb)], r=[("xT", dc, tb)], w=[ok])
                        h.dma("sp", outT_d[dc * 128:(dc + 1) * 128, tbs(tb)], o[:], r=[ok], w=[("outd", dc, tb)], sem=ok)
                h.end()

        fns = [lambda: attn_phase(0), lambda: moe_phase(0), kv_phase, lambda: attn_phase(1), lambda: moe_phase(1)]
        for i_ in range(5):
            if lo <= i_ <= hi and i_ not in globals().get("_SKIP", []):
                fns[i_]()
        for _i in range(globals().get("_DUMMY_BLOCKS", 0)):
            h.begin()
            h.memset("dve", ncoef[:, 8:9], 0.0, w=["x"])
            h.end()
        final_phase(hi >= 5)
    return nc


_CACHE = {}


def _lay(v, k):
    return np.ascontiguousarray(np.asarray(v, np.float32).reshape(k, 128).T)


def kernel(x, c, positions, a_norm_g, a_w_in, a_b_f, a_w_o, kv_norm_g, kv_w_mod, kv_b_mod,
           kv_w_down, kv_latent_g, kv_w_up, b_norm_g, b_w_dq, b_q_norm_g, b_w_uq, b_w_o,
           w_mod, b_mod, ffn_norm_g, router_w, router_bias, exp_w_gate, exp_w_up, exp_w_down,
           final_norm_g):
    f = lambda a: np.ascontiguousarray(np.asarray(a, np.float32))
    x = f(x)
    B = x.shape[0]
    if "nc" not in _CACHE:
        _CACHE["nc"] = build_program(0, 5)
    gvec = np.concatenate([_lay(a_norm_g[0], 8), _lay(ffn_norm_g[0], 8), _lay(kv_norm_g, 8), _lay(b_norm_g[0], 8),
                           _lay(ffn_norm_g[1], 8), _lay(final_norm_g, 8)], axis=1)
    bmod = np.concatenate([_lay(b_mod[0], 48), _lay(b_mod[1], 48), _lay(kv_b_mod, 16)], axis=1)
    bf_rep = np.ascontiguousarray(np.tile(np.asarray(a_b_f[0], np.float32)[None, :], (128, 16)))
    rb_rep = np.ascontiguousarray(np.tile(np.asarray(router_bias, np.float32)[None, :], (128, 16)))
    half = 16
    inv_freq = (10000.0 ** (-np.arange(half, dtype=np.float32) / half)).astype(np.float32)
    invf = np.zeros((128, 2), np.float32)
    for p in range(128):
        invf[p, 0] = inv_freq[p % 16]
        invf[p, 1] = -1.0 if (p % 32) < 16 else 1.0
    shared = {
        "invf": invf, "gvec": np.ascontiguousarray(gvec), "bmod": np.ascontiguousarray(bmod), "bf_rep": bf_rep, "rb_rep": rb_rep,
        "lat_g": _lay(kv_latent_g, 2), "q_g": _lay(b_q_norm_g[0], 6),
        "w_mod": f(w_mod), "kv_w_mod": f(kv_w_mod), "a_w_in": f(a_w_in[0]), "a_w_o": f(a_w_o[0]),
        "kv_w_down": f(kv_w_down), "kv_w_up": f(kv_w_up), "b_w_dq": f(b_w_dq[0]), "b_w_uq": f(b_w_uq[0]),
        "b_w_o": f(b_w_o[0]), "router_w": f(router_w),
        "exp_w_gate0": f(exp_w_gate[0]), "exp_w_gate1": f(exp_w_gate[1]), "exp_w_up0": f(exp_w_up[0]), "exp_w_up1": f(exp_w_up[1]),
        "exp_w_down0": f(exp_w_down[0]), "exp_w_down1": f(exp_w_down[1]),
    }
    in_maps = []
    for b in range(B):
        m = dict(shared)
        m["xT"] = np.ascontiguousarray(x[b].T)
        m["c_l"] = _lay(c[b], 8)
        m["pos"] = np.ascontiguousarray(np.asarray(positions[b], np.int32)[None, :])
        in_maps.append(m)
    res = run_bass_kernel_spmd(_CACHE["nc"], in_maps, core_ids=list(range(B)))
    out = np.stack([np.asarray(r["outT"], np.float32).T for r in res.results], axis=0)
    return np.ascontiguousarray(out)
```

```python
import numpy as np
from contextlib import ExitStack
import concourse.bass as bass
import concourse.mybir as mybir
from concourse.bass_utils import run_bass_kernel_spmd

F32 = mybir.dt.float32
BF16 = mybir.dt.bfloat16
I32 = mybir.dt.int32
AF = mybir.ActivationFunctionType
ALU = mybir.AluOpType
AX = mybir.AxisListType

S = 2048
D = 1024
NT = 16
NB = 4
DC = 8
NE = 16
EPS = 1e-6
C1_2PI = float(np.float32(2 * np.pi))
C2_2PI = float(2 * np.pi - C1_2PI)
SAME_ENGINE_SYNC = True
STOP_AFTER = "final"


class _Op:
    __slots__ = ("eng", "idx", "fn", "deps", "need_sig", "sig_val", "dma", "dma_val")

    def __init__(self, eng, idx, fn, dma):
        self.eng = eng
        self.idx = idx
        self.fn = fn
        self.deps = []
        self.need_sig = False
        self.sig_val = 0
        self.dma = dma
        self.dma_val = 0


class Prog:
    ENGS = ("pe", "act", "dve", "pool", "sp")

    def __init__(self, nc):
        self.nc = nc
        self.ops = {e: [] for e in self.ENGS}
        self.last_w = {}
        self.readers = {}
        self.dma_cnt = {}

    def op(self, eng, fn, r=(), w=(), dma=None):
        o = _Op(eng, len(self.ops[eng]), fn, dma)
        deps = {}

        def add(d):
            if d.dma is not None:
                key = ("d", d.dma)
                if key not in deps or deps[key].dma_val < d.dma_val:
                    deps[key] = d
            else:
                if d.eng == eng and (eng == "pe" or not SAME_ENGINE_SYNC):
                    return
                key = ("c", d.eng)
                if key not in deps or deps[key].idx < d.idx:
                    deps[key] = d

        for k in r:
            d = self.last_w.get(k)
            if d is not None:
                add(d)
        for k in w:
            d = self.last_w.get(k)
            if d is not None:
                add(d)
            rd = self.readers.get(k)
            if rd:
                for d in rd.values():
                    add(d)
        for d in deps.values():
            if d.dma is None:
                d.need_sig = True
            o.deps.append(d)
        for k in w:
            self.last_w[k] = o
            self.readers[k] = {}
        for k in r:
            rd = self.readers.setdefault(k, {})
            if dma is not None:
                rd[("d", dma, o.idx)] = o
            else:
                rd[("c", eng)] = o
        if dma is not None:
            c = self.dma_cnt.get(dma, 0) + 1
            self.dma_cnt[dma] = c
            o.dma_val = 16 * c
        self.ops[eng].append(o)
        return o

    def emit(self, pool, final_waits=()):
        nc = self.nc
        for e in self.ENGS:
            c = 0
            for o in self.ops[e]:
                if o.dma is None and o.need_sig:
                    c += 1
                    o.sig_val = pool.ebase[e] + c
            pool.ebase[e] += c
        swk = set()
        for o in self.ops["pool"]:
            if o.dma is not None:
                swk.add(o.dma)
        slot = {}
        n_sw, n_hw = 0, 0
        for k in self.dma_cnt:
            if k in swk:
                slot[k] = n_sw
                n_sw += 1
            else:
                slot[k] = pool.n_sw + n_hw
                n_hw += 1
        assert n_sw <= pool.n_sw and n_hw <= len(pool.dsem) - pool.n_sw, (n_sw, n_hw)
        for e in self.ENGS:
            for o in self.ops[e]:
                if o.dma is not None:
                    o.dma_val += pool.dbase[slot[o.dma]]
        esem = pool.esem
        dsem = {k: pool.dsem[i] for k, i in slot.items()}
        all_final = {k: pool.dbase[slot[k]] + 16 * self.dma_cnt[k] for k in slot}
        for k, i in slot.items():
            pool.dbase[i] += 16 * self.dma_cnt[k]
        with nc.Block() as block:

            def run(e, engobj):
                waited = {}
                for o in self.ops[e]:
                    ws = []
                    for d in o.deps:
                        if d.dma is not None:
                            sem, val, sk = dsem[d.dma], d.dma_val, ("d", d.dma)
                        else:
                            sem, val, sk = esem[d.eng], d.sig_val, ("c", d.eng)
                        if waited.get(sk, 0) >= val:
                            continue
                        waited[sk] = val
                        ws.append((sem, val))
                    for sem, val in ws[:-1]:
                        engobj.wait_ge(sem, val)
                    ins = o.fn(engobj)
                    if ws:
                        ins._wait_ge(ws[-1][0], ws[-1][1])
                    if o.dma is not None:
                        ins.then_inc(dsem[o.dma], 16)
                    elif o.need_sig:
                        ins.then_inc(esem[e], 1)
                if e == "sp":
                    for k, v in all_final.items():
                        engobj.wait_ge(dsem[k], v)

            @block.tensor
            def _(pe):
                run("pe", pe)

            @block.scalar
            def _(act):
                run("act", act)

            @block.vector
            def _(dve):
                run("dve", dve)

            @block.gpsimd
            def _(pool_):
                run("pool", pool_)

            @block.sync
            def _(sp):
                run("sp", sp)


class SemPool:
    def __init__(self, nc, st, n_dma=54, n_sw=32):
        self.n_sw = n_sw
        self.esem = {e: st.enter_context(nc.semaphore("g_" + e)) for e in Prog.ENGS}
        self.ebase = {e: 0 for e in Prog.ENGS}
        self.dsem = [st.enter_context(nc.semaphore("gd%d" % i)) for i in range(n_dma)]
        self.dbase = [0] * n_dma


class H:
    def __init__(self, nc):
        self.nc = nc
        self.P = None
        self.pool = None

    def begin(self):
        self.P = Prog(self.nc)

    def end(self, final_waits=()):
        self.P.emit(self.pool, final_waits)
        self.P = None

    def mm(self, out, lhsT, rhs, start=True, stop=True, r=(), w=()):
        self.P.op("pe", lambda e: e.matmul(out, lhsT, rhs, start=start, stop=stop), r, w)

    def tr(self, out, in_, ident, r=(), w=()):
        self.P.op("pe", lambda e: e.transpose(out, in_, ident), r, w)

    def act(self, out, in_, func, bias=0.0, scale=1.0, r=(), w=()):
        self.P.op("act", lambda e: e.activation(out, in_, func, bias=bias, scale=scale), r, w)

    def tt(self, eng, out, in0, in1, op, r=(), w=()):
        self.P.op(eng, lambda e: e.tensor_tensor(out, in0, in1, op), r, w)

    def ts(self, eng, out, in0, s1, s2, op0, op1=None, r=(), w=()):
        if op1 is None:
            self.P.op(eng, lambda e: e.tensor_scalar(out, in0, s1, None, op0), r, w)
        else:
            self.P.op(eng, lambda e: e.tensor_scalar(out, in0, s1, s2, op0, op1), r, w)

    def stt(self, eng, out, in0, sc, in1, op0, op1, r=(), w=()):
        self.P.op(eng, lambda e: e.scalar_tensor_tensor(out, in0, sc, in1, op0, op1), r, w)

    def cp(self, eng, out, in_, r=(), w=()):
        if eng == "act":
            self.P.op("act", lambda e: e.copy(out, in_), r, w)
        else:
            self.P.op(eng, lambda e: e.tensor_copy(out, in_), r, w)

    def rcp(self, out, in_, r=(), w=()):
        self.P.op("dve", lambda e: e.reciprocal(out, in_), r, w)

    def red(self, out, in_, op, r=(), w=()):
        self.P.op("dve", lambda e: e.tensor_reduce(out, in_, AX.X, op), r, w)

    def memset(self, eng, ap, val, w=()):
        self.P.op(eng, lambda e: e.memset(ap, val), (), w)

    def dma(self, q, out, in_, r=(), w=(), sem=None):
        self.P.op(q, lambda e: e.dma_start(out=out, in_=in_), r, w, dma=sem)


def build_program(lo=0, hi=5):
    nc = bass.Bass("TRN2", target_bir_lowering=False)
    h = H(nc)

    def din(name, shape, dt=F32):
        return nc.dram_tensor(name, list(shape), dt, kind="ExternalInput").ap()

    xT_d = din("xT", [D, S])
    c_d = din("c_l", [128, DC])
    pos_d = din("pos", [1, S], I32)
    invf_d = din("invf", [128, 2])
    gvec_d = din("gvec", [128, 6 * DC])
    bmod_d = din("bmod", [128, 112])
    bf_d = din("bf_rep", [128, 256])
    rb_d = din("rb_rep", [128, 256])
    lg_d = din("lat_g", [128, 2])
    qg_d = din("q_g", [128, 6])
    w_mod_d = din("w_mod", [2, D, 6 * D])
    kv_w_mod_d = din("kv_w_mod", [D, 2 * D])
    a_w_in_d = din("a_w_in", [D, 3 * D + 16])
    a_w_o_d = din("a_w_o", [D, D])
    kv_w_down_d = din("kv_w_down", [D, 288])
    kv_w_up_d = din("kv_w_up", [256, 2048])
    b_w_dq_d = din("b_w_dq", [D, 768])
    b_w_uq_d = din("b_w_uq", [768, 1536])
    b_w_o_d = din("b_w_o", [D, D])
    router_w_d = din("router_w", [D, NE])
    wg_d = [din("exp_w_gate%d" % l_, [NE, D, 512]) for l_ in range(2)]
    wu_d = [din("exp_w_up%d" % l_, [NE, D, 512]) for l_ in range(2)]
    wd_d = [din("exp_w_down%d" % l_, [NE, 512, D]) for l_ in range(2)]
    outT_d = nc.dram_tensor("outT", [D, S], F32, kind="ExternalOutput").ap()

    with ExitStack() as G:
        _uid = [0]

        def sb(name, shape, dt, st=G):
            _uid[0] += 1
            return st.enter_context(nc.sbuf_tensor("s%d_%s" % (_uid[0], name), list(shape), dt))

        pb = [G.enter_context(nc.psum_tensor("pb%d" % i, [128, 512], F32)) for i in range(8)]
        h.pool = SemPool(nc, G)

        def PK(i):
            return ("ps", i)

        xT = sb("xT", [128, DC, S], F32)
        hT = sb("hT", [128, DC, S], BF16)
        ident = sb("ident", [128, 128], F32)
        ones_f = sb("ones_f", [128, 128], F32)
        ones_b = sb("ones_b", [128, 128], BF16)
        triu_f = sb("triu_f", [128, 128], F32)
        triu_b = sb("triu_b", [128, 128], BF16)
        cmask_b = sb("cmask_b", [128, 128], BF16)
        epst = sb("epst", [128, 1], F32)
        mod0 = sb("mod0", [128, 48], F32)
        mod1 = sb("mod1", [128, 48], F32)
        modkv = sb("modkv", [128, 16], F32)
        gvec = sb("gvec_s", [128, 6 * DC], F32)
        bmod = sb("bmod_s", [128, 112], F32)
        bf_rep = sb("bf_rep_s", [128, 256], F32)
        rb_rep = sb("rb_rep_s", [128, 256], F32)
        lat_g = sb("lat_g_s", [128, 2], F32)
        q_g = sb("q_g_s", [128, 6], F32)
        csT = sb("csT", [128, S], BF16)
        ckvT = sb("ckvT", [128, 2, S], BF16)
        krope = sb("krope", [128, S], BF16)
        ncoef = sb("ncoef", [128, 2 * DC], F32)
        cact = sb("cact", [128, DC], BF16)

        def tbs(tb):
            return slice(tb * 512, (tb + 1) * 512)

        def norm_phase(st, gidx, modt, sh0, sc0, nchunks=DC, src=None, dst=None, dst_keyf=None,
                       router=None):
            sq = [sb("sq%d" % i, [128, 512], BF16, st) for i in range(4)]
            rs = [sb("rs%d" % i, [128, 512], F32, st) for i in range(2)]
            t2 = [sb("t2_%d" % i, [128, 512], F32, st) for i in range(3)]
            h.stt("dve", ncoef[:, 0:DC], modt[:, sc0:sc0 + DC], 1.0, gvec[:, gidx * DC:(gidx + 1) * DC],
                  ALU.add, ALU.mult, r=["mod", "gvec"], w=["ncoef"])
            n = 0
            for tb in range(NB):
                pss = pb[tb % 2]
                for dc in range(DC):
                    q = sq[n % 4]
                    n += 1
                    if dc % 2 == 0:
                        h.act(q[:], xT[:, dc, tbs(tb)], AF.Square, r=[("xT", dc, tb)], w=[("sq", id(q))])
                    else:
                        h.tt("dve", q[:], xT[:, dc, tbs(tb)], xT[:, dc, tbs(tb)], ALU.mult, r=[("xT", dc, tb)], w=[("sq", id(q))])
                    h.mm(pss[:, :], ones_b[:, :], q[:], start=(dc == 0), stop=(dc == DC - 1),
                         r=[("sq", id(q)), "ones_b"], w=[PK(tb % 2)])
                r_ = rs[tb % 2]
                h.act(r_[:], pss[:, :], AF.Ln, bias=epst[:, 0:1], scale=1.0 / D, r=[PK(tb % 2), "epst"], w=[("rs", tb % 2)])
                h.act(r_[:], r_[:], AF.Exp, scale=-0.5, r=[("rs", tb % 2)], w=[("rs", tb % 2)])
                for dc in range(DC):
                    t = t2[n % 3]
                    n += 1
                    h.tt("dve", t[:], xT[:, dc, tbs(tb)], r_[:], ALU.mult, r=[("xT", dc, tb), ("rs", tb % 2)], w=[("t2", id(t))])
                    h.act(hT[:, dc, tbs(tb)], t[:], AF.Identity, bias=modt[:, sh0 + dc:sh0 + dc + 1],
                          scale=ncoef[:, dc:dc + 1], r=[("t2", id(t)), "ncoef", "mod"], w=[("hT", tb)])
                    if router is not None:
                        router(tb, dc, t)

        with ExitStack() as st:
            cs = sb("c_s", [128, DC], F32, st)
            wm = [sb("wm%d" % i, [128, 6 * D], BF16, st) for i in range(3)]
            posi = sb("posi", [128, S], I32, st)
            invf = sb("invf_s", [128, 2], F32, st)
            sinT = sb("sinT", [128, S], BF16, st)
            cosT = sb("cosT", [128, S], BF16, st)
            ang = sb("ang", [128, S], F32, st)
            ta = sb("ta", [128, S], F32, st)
            tb_ = sb("tb_", [128, S], F32, st)
            ti = sb("ti", [128, S], I32, st)
            h.begin()
            h.dma("sp", cs[:], c_d[:, :], w=["cs"], sem="cs")
            h.dma("sp", gvec[:], gvec_d[:, :], w=["gvec"], sem="ld_gvec")
            h.dma("sp", bmod[:], bmod_d[:, :], w=["bmod"], sem="ld_bmod")
            h.dma("sp", bf_rep[:], bf_d[:, :], w=["bf_rep"], sem="ld_bf_rep")
            h.dma("sp", rb_rep[:], rb_d[:, :], w=["rb_rep"], sem="ld_rb_rep")
            h.dma("sp", lat_g[:], lg_d[:, :], w=["lat_g"], sem="ld_lat_g")
            h.dma("sp", q_g[:], qg_d[:, :], w=["q_g"], sem="ld_q_g")
            h.dma("sp", invf[:], invf_d[:, :], w=["invf"], sem="ld_invf")
            h.dma("sp", posi[:], pos_d.partition_broadcast(128), w=["posi"], sem="ld_posi")
            h.memset("pool", ones_f[:], 1.0, w=["ones_f"])
            h.memset("pool", ones_b[:], 1.0, w=["ones_b"])
            h.memset("pool", epst[:], EPS, w=["epst"])
            h.P.op("pool", lambda e: e.affine_select(out=ident[:], in_=ones_f[:], pattern=[[-1, 128]], compare_op=ALU.is_equal,
                                                     fill=0.0, base=0, channel_multiplier=1), ["ones_f"], ["ident"])
            h.P.op("pool", lambda e: e.affine_select(out=triu_f[:], in_=ones_f[:], pattern=[[1, 128]], compare_op=ALU.is_ge,
                                                     fill=0.0, base=0, channel_multiplier=-1), ["ones_f"], ["triu_f"])
            h.cp("pool", triu_b[:], triu_f[:], r=["triu_f"], w=["triu_b"])
            h.memset("pool", cmask_b[:], 1.0, w=["cmask_b"])
            h.memset("pool", cmask_b[64:128, 0:64], 0.0, w=["cmask_b"])
            h.memset("pool", krope[:], 0.0, w=["krope"])
            for dc in range(DC):
                h.dma("sp", xT[:, dc, :], xT_d[dc * 128:(dc + 1) * 128, :], w=[("xT", dc, t) for t in range(NB)], sem=("xload", dc))
            h.act(cact[:], cs[:], AF.Silu, r=["cs"], w=["cact"])
            h.cp("dve", ang[:], posi[:], r=["posi"], w=["ang"])
            h.ts("dve", ang[:], ang[:], invf[:, 0:1], None, ALU.mult, r=["ang", "invf"], w=["ang"])
            for (dst, shift) in ((sinT, 0.0), (cosT, float(np.pi / 2))):
                if shift != 0.0:
                    h.ts("dve", ang[:], ang[:], shift, None, ALU.add, r=["ang"], w=["ang"])
                h.ts("dve", ta[:], ang[:], float(1.0 / (2 * np.pi)), None, ALU.mult, r=["ang"], w=["ta"])
                h.cp("dve", ti[:], ta[:], r=["ta"], w=["ti"])
                h.cp("dve", ta[:], ti[:], r=["ti"], w=["ta"])
                h.stt("dve", tb_[:], ta[:], -C1_2PI, ang[:], ALU.mult, ALU.add, r=["ta", "ang"], w=["tb_"])
                h.stt("dve", tb_[:], ta[:], -C2_2PI, tb_[:], ALU.mult, ALU.add, r=["ta", "tb_"], w=["tb_"])
                h.ts("dve", ta[:], tb_[:], float(np.pi), float(-2 * np.pi), ALU.is_gt, ALU.mult, r=["tb_"], w=["ta"])
                h.tt("dve", tb_[:], tb_[:], ta[:], ALU.add, r=["tb_", "ta"], w=["tb_"])
                h.ts("dve", ta[:], tb_[:], float(-np.pi), float(2 * np.pi), ALU.is_lt, ALU.mult, r=["tb_"], w=["ta"])
                h.tt("dve", tb_[:], tb_[:], ta[:], ALU.add, r=["tb_", "ta"], w=["tb_"])
                h.act(dst[:], tb_[:], AF.Sin, r=["tb_"], w=[("trig", id(dst))])
            wi = 0
            h.cp("dve", mod1[:, :], bmod[:, 48:96], r=["bmod"], w=["mod1acc"])
            h.cp("dve", modkv[:, :], bmod[:, 96:112], r=["bmod"], w=["modkvacc"])
            for (wsrc, ncol, modt, boff) in ((w_mod_d[0], 48, mod0, 0),):
                psm = pb[2 + (wi % 2)]
                for kc in range(DC):
                    wt = wm[wi % 3]
                    wkey = ("wm", wi % 3)
                    wi += 1
                    h.dma("pool", wt[:, 0:ncol * 128], wsrc[kc * 128:(kc + 1) * 128, :], w=[wkey], sem=wkey)
                    for j in range(ncol):
                        h.mm(psm[:, kc * ncol + j:kc * ncol + j + 1], wt[:, j * 128:(j + 1) * 128], cact[:, kc:kc + 1],
                             r=[wkey, "cact"], w=[("psm", id(psm))])
                h.red(modt[:, 0:ncol], psm[:, 0:DC * ncol].rearrange("p (k j) -> p j k", k=DC), ALU.add, r=[("psm", id(psm))], w=["mod"])
                h.tt("dve", modt[:, 0:ncol], modt[:, 0:ncol], bmod[:, boff:boff + ncol], ALU.add, r=["mod", "bmod"], w=["mod"])
            h.memset("pool", csT[0:64, :], 0.0, w=["cs_lo"])
            h.cp("pool", csT[64:96, :], cosT[64:96, :], r=[("trig", id(cosT))], w=["rope_tab"])
            h.ts("dve", csT[96:128, :], sinT[96:128, :], invf[96:128, 1:2], None, ALU.mult, r=[("trig", id(sinT)), "invf"], w=["rope_tab2"])
            h.end()

        def attn_phase(layer):
            fox = layer == 0
            modt = mod0 if fox else mod1
            if fox:
                with ExitStack() as st0:
                    h.begin()
                    norm_phase(st0, 0, modt, 0, DC)
                    h.end()
            with ExitStack() as st:
                h.begin()
                cqT = hT
                NSET = 2
                qh = [[sb("qh%d_%d" % (s_, i), [128, S], BF16, st) for i in range(2)] for s_ in range(NSET)]
                kh = [[sb("kh%d_%d" % (s_, i), [128, S], BF16, st) for i in range(2)] for s_ in range(NSET)]
                Vp = [sb("Vp%d" % s_, [128, NT, 192], BF16, st) for s_ in range(NSET)]
                oT = [sb("oT%d" % i, [128, S], BF16, st) for i in range(2)]
                wo = [sb("wo%d" % i, [128, D], BF16, st) for i in range(3)]
                PT = [sb("PT%d" % i, [128, 512], BF16, st) for i in range(6)]
                rec = [sb("rec%d" % i, [128, 512], F32, st) for i in range(2)]
                zero_bias = sb("zero_bias", [128, 1], F32, st)
                h.memset("pool", zero_bias[:], 0.0, w=["zero_bias"])
                for s_ in range(NSET):
                    h.memset("pool", Vp[s_][:, :, 64:128], 1.0, w=[("Vp", s_)])
                    for i in range(2):
                        if fox:
                            h.memset("pool", kh[s_][i][64:65, :], 1.0, w=[("kh", s_, i)])
                        else:
                            h.cp("act" if i == 0 else "dve", kh[s_][i][64:128, :], krope[64:128, :], r=["krope"], w=[("kh", s_, i)])
                if fox:
                    wpair = [sb("wpair%d" % i, [128, DC, 384], BF16, st) for i in range(2)]
                    wf = sb("wf", [128, DC, 16], BF16, st)
                    lf = sb("lf", [128, 256], F32, st)
                    tot = sb("tot", [128, 256], F32, st)
                    off = sb("off", [128, 256], F32, st)
                    negcum = sb("negcum", [128, 256], F32, st)
                    cum8 = sb("cum8", [128, 256], F32, st)
                    cumT = sb("cumT", [16, S], BF16, st)
                    h.dma("pool", wf[:], a_w_in_d[:, 3 * D:3 * D + 16].rearrange("(dc p) c -> p dc c", p=128), w=["wf"], sem="wf")
                    for i in range(NT):
                        for dc in range(DC):
                            h.mm(pb[2][:, i * 16:(i + 1) * 16], hT[:, dc, i * 128:(i + 1) * 128], wf[:, dc, :],
                                 start=(dc == 0), stop=(dc == DC - 1), r=[("hT", i // 4), "wf"], w=[PK(2)])
                    h.tt("dve", lf[:], pb[2][:, 0:256], bf_rep[:], ALU.add, r=[PK(2), "bf_rep"], w=["lf"])
                    h.act(lf[:], lf[:], AF.Sigmoid, r=["lf"], w=["lf"])
                    h.act(lf[:], lf[:], AF.Ln, r=["lf"], w=["lf"])
                    h.mm(pb[3][:, 0:256], triu_f[:], lf[:], r=["triu_f", "lf"], w=[PK(3)])
                    h.mm(pb[2][:, 0:256], ones_f[:], lf[:], r=["ones_f", "lf"], w=[PK(2)])
                    h.cp("dve", tot[:], pb[2][:, 0:256], r=[PK(2)], w=["tot"])
                    h.memset("dve", off[:, 0:16], 0.0, w=["off"])
                    for i in range(1, NT):
                        h.tt("dve", off[:, i * 16:(i + 1) * 16], off[:, (i - 1) * 16:i * 16], tot[:, (i - 1) * 16:i * 16], ALU.add,
                             r=["off", "tot"], w=["off"])
                    h.tt("dve", cum8[:], pb[3][:, 0:256], off[:], ALU.add, r=[PK(3), "off"], w=["cum8"])
                    h.ts("dve", negcum[:], cum8[:], -1.0, None, ALU.mult, r=["cum8"], w=["negcum"])
                    h.ts("dve", cum8[:], cum8[:], 8.0, None, ALU.mult, r=["cum8"], w=["cum8"])
                    for i in range(NT):
                        bk = 4 + (i // 4) % 2
                        h.tr(pb[bk][0:16, (i % 4) * 128:(i % 4 + 1) * 128], cum8[:, i * 16:(i + 1) * 16], ident[:],
                             r=["cum8", "ident"], w=[PK(bk)])
                        if i % 4 == 3:
                            h.cp("act", cumT[:, (i // 4) * 512:(i // 4 + 1) * 512], pb[bk][0:16, :], r=[PK(bk)], w=["cumT"])
                else:
                    wq = [sb("wq%d" % i, [128, 6, 256], BF16, st) for i in range(2)]
                    wkv = [sb("wkv%d" % i, [128, 2, 256], BF16, st) for i in range(2)]

                g1c = 2 * DC
                w_o_d = a_w_o_d if fox else b_w_o_d
                scale = 0.125 if fox else float(96 ** -0.5)
                mask = triu_b if fox else cmask_b
                Kc = 65 if fox else 128
                cnt = {"ps_s": 0, "pt": 0, "ps_o": 0, "rec": 0, "op": 0}
                NPT = 6

                bgc = {"bk": 0}

                def bgbank():
                    bgc["bk"] += 1
                    return 5 + bgc["bk"] % 3

                def issue_weights(p):
                    wb = p % 2
                    h.dma("pool", wo[p % 3][:], w_o_d[p * 128:(p + 1) * 128, :], w=[("wo", p % 3)], sem=("wo", p % 3))
                    if fox:
                        for part in range(3):
                            c0 = part * D + p * 128
                            h.dma("pool", wpair[wb][:, :, part * 128:(part + 1) * 128],
                                  a_w_in_d[:, c0:c0 + 128].rearrange("(dc p) c -> p dc c", p=128), w=[("wpair", wb, part)], sem=("wpair", wb, part))
                    else:
                        for hh in range(2):
                            hd = 2 * p + hh
                            uq = b_w_uq_d[:, hd * 96:(hd + 1) * 96].rearrange("(kc p) c -> p kc c", p=128)
                            h.dma("pool", wq[wb][:, :, hh * 128:hh * 128 + 64], uq[:, :, 0:64], w=[("wq", wb, hh, 0)], sem=("wq", wb, hh, 0))
                            h.dma("pool", wq[wb][:, :, hh * 128 + 64:hh * 128 + 96], uq[:, :, 64:96], w=[("wq", wb, hh, 1)], sem=("wq", wb, hh, 1))
                            h.dma("pool", wq[wb][:, :, hh * 128 + 96:hh * 128 + 112], uq[:, :, 80:96], w=[("wq", wb, hh, 2)], sem=("wq", wb, hh, 2))
                            h.dma("pool", wq[wb][:, :, hh * 128 + 112:hh * 128 + 128], uq[:, :, 64:80], w=[("wq", wb, hh, 3)], sem=("wq", wb, hh, 3))
                            up = kv_w_up_d[:, hd * 128:(hd + 1) * 128].rearrange("(c p) n -> p c n", p=128)
                            h.dma("pool", wkv[wb][:, :, hh * 64:(hh + 1) * 64], up[:, :, 0:64], w=[("wkv", wb, hh, 0)], sem=("wkv", wb, hh, 0))
                            h.dma("pool", wkv[wb][:, :, 128 + hh * 64:128 + (hh + 1) * 64], up[:, :, 64:128], w=[("wkv", wb, hh, 1)], sem=("wkv", wb, hh, 1))

                def proj_items(p):
                    s_ = p % NSET
                    wb = p % 2
                    items = []

                    def v_item(g):
                        def f():
                            bk = bgbank()
                            for i in range(4 * g, 4 * g + 4):
                                if fox:
                                    for dc in range(DC):
                                        h.mm(pb[bk][:, (i % 4) * 128:(i % 4 + 1) * 128], hT[:, dc, i * 128:(i + 1) * 128], wpair[wb][:, dc, 256:384],
                                             start=(dc == 0), stop=(dc == DC - 1), r=[("wpair", wb, 2), ("hT", i // 4)], w=[PK(bk)])
                                else:
                                    for c in range(2):
                                        h.mm(pb[bk][:, (i % 4) * 128:(i % 4 + 1) * 128], ckvT[:, c, i * 128:(i + 1) * 128], wkv[wb][:, c, 128:256],
                                             start=(c == 0), stop=(c == 1), r=[("wkv", wb, 0, 1), ("wkv", wb, 1, 1), "ckvT"], w=[PK(bk)])
                            pv = pb[bk][:, :].rearrange("p (i c) -> p i c", c=128)
                            h.cp("act", Vp[s_][:, 4 * g:4 * g + 4, 0:64], pv[:, :, 0:64], r=[PK(bk)], w=[("Vp", s_)])
                            h.cp("dve", Vp[s_][:, 4 * g:4 * g + 4, 128:192], pv[:, :, 64:128], r=[PK(bk)], w=[("Vp", s_)])
                        return f

                    if fox:
                        def qk_item(which, tb):
                            def f():
                                nm = "qh" if which == 0 else "kh"
                                dsts = qh[s_] if which == 0 else kh[s_]
                                bk = bgbank()
                                for dc in range(DC):
                                    h.mm(pb[bk][:, :], wpair[wb][:, dc, which * 128:(which + 1) * 128], hT[:, dc, tbs(tb)],
                                         start=(dc == 0), stop=(dc == DC - 1), r=[("wpair", wb, which), ("hT", tb)], w=[PK(bk)])
                                h.cp("act", dsts[0][0:64, tbs(tb)], pb[bk][0:64, :], r=[PK(bk)], w=[(nm, s_, 0)])
                                h.cp("dve", dsts[1][0:64, tbs(tb)], pb[bk][64:128, :], r=[PK(bk)], w=[(nm, s_, 1)])
                            return f

                        def aug_item():
                            for hh in range(2):
                                h.dma("sp", qh[s_][hh][64:65, :], cumT[2 * p + hh:2 * p + hh + 1, :], r=["cumT"], w=[("qh", s_, hh)], sem=("aug", s_, hh))
                        items.append(aug_item)
                        for which in range(2):
                            for tb in range(NB):
                                items.append(qk_item(which, tb))
                    else:
                        def q_item(hh, tb):
                            def f():
                                bk = bgbank()
                                for kc in range(6):
                                    h.mm(pb[bk][:, :], wq[wb][:, kc, hh * 128:(hh + 1) * 128], cqT[:, kc, tbs(tb)],
                                         start=(kc == 0), stop=(kc == 5),
                                         r=[("wq", wb, hh, 0), ("wq", wb, hh, 1), ("wq", wb, hh, 2), ("wq", wb, hh, 3), ("hT", tb)], w=[PK(bk)])
                                h.cp("act", qh[s_][hh][0:64, tbs(tb)], pb[bk][0:64, :], r=[PK(bk)], w=[("qh", s_, hh)])
                                h.tt("dve", qh[s_][hh][64:128, tbs(tb)], pb[bk][64:128, :], csT[64:128, tbs(tb)], ALU.mult,
                                     r=[PK(bk), "rope_tab", "rope_tab2"], w=[("qh", s_, hh)])
                            return f

                        def k_item(tb):
                            def f():
                                bk = bgbank()
                                for c in range(2):
                                    h.mm(pb[bk][:, :], wkv[wb][:, c, 0:128], ckvT[:, c, tbs(tb)], start=(c == 0), stop=(c == 1),
                                         r=[("wkv", wb, 0, 0), ("wkv", wb, 1, 0), "ckvT"], w=[PK(bk)])
                                h.cp("act", kh[s_][0][0:64, tbs(tb)], pb[bk][0:64, :], r=[PK(bk)], w=[("kh", s_, 0)])
                                h.cp("dve", kh[s_][1][0:64, tbs(tb)], pb[bk][64:128, :], r=[PK(bk)], w=[("kh", s_, 1)])
                            return f
                        for hh in range(2):
                            for tb in range(NB):
                                items.append(q_item(hh, tb))
                        for tb in range(NB):
                            items.append(k_item(tb))
                    for g in range(4):
                        items.append(v_item(g))
                    return items

                def outproj_items(p):
                    wb = p % 3
                    ob = p % 2
                    items = []

                    def o_item(tb, dcol):
                        def f():
                            bo = bgbank()
                            h.mm(pb[bo][:, :], wo[wb][:, dcol * 128:(dcol + 1) * 128], oT[ob][:, tbs(tb)],
                                 r=[("wo", wb), ("oT", ob, tb)], w=[PK(bo)])
                            h.stt("dve", xT[:, dcol, tbs(tb)], pb[bo][:, :], modt[:, g1c + dcol:g1c + dcol + 1], xT[:, dcol, tbs(tb)],
                                  ALU.mult, ALU.add, r=[PK(bo), "mod", ("xT", dcol, tb)], w=[("xT", dcol, tb)])
                        return f
                    for tb in range(NB):
                        for dcol in range(DC):
                            items.append(o_item(tb, dcol))
                    return items

                def attn_steps(p):
                    s_ = p % NSET
                    ob = p % 2
                    steps = []
                    for hh in range(2):
                        for qb in range(NB):
                            nkt = 4 * qb + 4
                            ob_k = 3 + cnt["ps_o"] % 2
                            cnt["ps_o"] += 1
                            for kt in range(nkt):
                                steps.append((hh, qb, kt, nkt, ob_k, cnt["ps_s"] % 3, cnt["pt"] % NPT))
                                cnt["ps_s"] += 1
                                cnt["pt"] += 1

                    def emit_S(stp):
                        hh, qb, kt, nkt, ob_k, sk, pk = stp
                        hd = 2 * p + hh
                        qt, kt_ = qh[s_][hh], kh[s_][hh]
                        jj = kt - 4 * qb
                        n0 = max(0, jj) * 128
                        h.mm(pb[sk][:, n0:512], kt_[0:Kc, kt * 128:(kt + 1) * 128], qt[0:Kc, qb * 512 + n0:(qb + 1) * 512],
                             r=[("kh", s_, hh), ("qh", s_, hh)], w=[PK(sk)])
                        if fox:
                            bias = negcum[:, kt * 16 + hd:kt * 16 + hd + 1]
                            rk = [PK(sk), "negcum"]
                        else:
                            bias = zero_bias[:, 0:1]
                            rk = [PK(sk), "zero_bias"]
                        h.act(PT[pk][:, n0:512], pb[sk][:, n0:512], AF.Exp, bias=bias, scale=scale, r=rk, w=[("PT", pk)])
                        if jj >= 0:
                            h.tt("pool", PT[pk][:, n0:n0 + 128], PT[pk][:, n0:n0 + 128], mask[:], ALU.mult,
                                 r=[("PT", pk), "mask"], w=[("PT", pk)])

                    def emit_PV(stp):
                        hh, qb, kt, nkt, ob_k, sk, pk = stp
                        jj = kt - 4 * qb
                        n0 = max(0, jj) * 128
                        vl = Vp[s_][:, kt, 0:128] if hh == 0 else Vp[s_][:, kt, 64:192]
                        h.mm(pb[ob_k][:, n0:512], vl, PT[pk][:, n0:512], start=(kt == 0), stop=(kt == nkt - 1),
                             r=[("Vp", s_), ("PT", pk)], w=[PK(ob_k)])
                        if kt == nkt - 1:
                            rc = cnt["rec"] % 2
                            cnt["rec"] += 1
                            if hh == 0:
                                h.act(rec[rc][0:64, :], pb[ob_k][64:128, :], AF.Ln, r=[PK(ob_k)], w=[("rec", rc)])
                                h.act(rec[rc][0:64, :], rec[rc][0:64, :], AF.Exp, scale=-1.0, r=[("rec", rc)], w=[("rec", rc)])
                                h.tt("dve", oT[ob][0:64, tbs(qb)], pb[ob_k][0:64, :], rec[rc][0:64, :], ALU.mult,
                                     r=[PK(ob_k), ("rec", rc)], w=[("oT", ob, qb)])
                            else:
                                h.act(rec[rc][64:128, :], pb[ob_k][0:64, :], AF.Ln, r=[PK(ob_k)], w=[("rec", rc)])
                                h.act(rec[rc][64:128, :], rec[rc][64:128, :], AF.Exp, scale=-1.0, r=[("rec", rc)], w=[("rec", rc)])
                                h.tt("dve", oT[ob][64:128, tbs(qb)], pb[ob_k][64:128, :], rec[rc][64:128, :], ALU.mult,
                                     r=[PK(ob_k), ("rec", rc)], w=[("oT", ob, qb)])
                    return steps, emit_S, emit_PV

                LOOK = 2
                issue_weights(0)
                for it in proj_items(0):
                    it()
                bg = []
                for p in range(8):
                    if p + 1 < 8:
                        issue_weights(p + 1)
                        bg = bg + proj_items(p + 1)
                    steps, emit_S, emit_PV = attn_steps(p)
                    nst = len(steps)
                    nbg = len(bg)
                    done = 0
                    for i_ in range(nst + LOOK):
                        if i_ < nst:
                            emit_S(steps[i_])
                        if i_ >= LOOK:
                            emit_PV(steps[i_ - LOOK])
                        tgt = (nbg * (i_ + 1)) // (nst + LOOK)
                        while done < tgt:
                            bg[done]()
                            done += 1
                    while done < nbg:
                        bg[done]()
                        done += 1
                    bg = outproj_items(p)
                for it in bg:
                    it()
                h.end()

        def moe_phase(layer):
            modt = mod0 if layer == 0 else mod1
            g2c = 5 * DC
            with ExitStack() as st:
                selE = sb("selE", [16, NE, 128], BF16, st)
                combT = sb("combT", [16, S], BF16, st)
                with ExitStack() as st2:
                    rw = sb("rw", [128, DC, NE], F32, st2)
                    h32 = sb("h32", [128, DC, 512], F32, st2)
                    R = {n: sb("r_" + n, [128, 256], F32, st2) for n in ("sc", "sel", "a", "b", "c", "top2", "w")}
                    gs = sb("gs", [128, 64], F32, st2)
                    gtmp = sb("gtmp", [128, 64], F32, st2)
                    gmax = sb("gmax", [128, 16], F32, st2)
                    ohg = sb("ohg", [128, 64], F32, st2)
                    wsum = sb("wsum", [128, 16], F32, st2)
                    h.begin()
                    h.dma("sp", rw[:], router_w_d[:, :].rearrange("(dc p) e -> p dc e", p=128), w=["rw"], sem="rw")
                    for e_ in range(NE):
                        h.ts("pool", selE[:, e_, :], ones_f[0:16, :], ident[0:16, e_:e_ + 1], None, ALU.mult, r=["ones_f", "ident"], w=["selE"])

                    def router(tb, dc, t):
                        h.ts("dve", h32[:, dc, :], t[:], ncoef[:, dc:dc + 1], modt[:, 3 * DC + dc:3 * DC + dc + 1], ALU.mult, ALU.add,
                             r=[("t2", id(t)), "ncoef", "mod"], w=[("h32", dc)])
                        if dc == DC - 1:
                            for i in range(4):
                                ti_ = tb * 4 + i
                                for d2 in range(DC):
                                    h.mm(pb[2][:, ti_ * 16:(ti_ + 1) * 16], h32[:, d2, i * 128:(i + 1) * 128], rw[:, d2, :],
                                         start=(d2 == 0), stop=(d2 == DC - 1), r=[("h32", d2), "rw"], w=[PK(2)])

                    norm_phase(st2, 1 if layer == 0 else 4, modt, 3 * DC, 4 * DC, router=router)
                    sc, sel, A_, B_, C_, top2, w_ = (R[n] for n in ("sc", "sel", "a", "b", "c", "top2", "w"))
                    h.act(sc[:], pb[2][:, 0:256], AF.Sigmoid, r=[PK(2)], w=["r_sc"])
                    h.tt("dve", sel[:], sc[:], rb_rep[:], ALU.add, r=["r_sc", "rb_rep"], w=["r_sel"])
                    X = sel[:].rearrange("p (t e) -> p t e", e=4)
                    first = True
                    for (a, b) in ((0, 1), (0, 2), (0, 3), (1, 2), (1, 3), (2, 3)):
                        if first:
                            h.tt("dve", gs[:], X[:, :, a], X[:, :, b], ALU.add, r=["r_sel"], w=["gs"])
                            first = False
                        else:
                            h.tt("dve", gtmp[:], X[:, :, a], X[:, :, b], ALU.add, r=["r_sel"], w=["gtmp"])
                            h.tt("dve", gs[:], gs[:], gtmp[:], ALU.max, r=["gs", "gtmp"], w=["gs"])
                    G4 = gs[:].rearrange("p (t g) -> p t g", g=4)
                    h.tt("dve", gmax[:], G4[:, :, 0], G4[:, :, 1], ALU.max, r=["gs"], w=["gmax"])
                    h.tt("dve", gmax[:], gmax[:], G4[:, :, 2], ALU.max, r=["gs", "gmax"], w=["gmax"])
                    h.tt("dve", gmax[:], gmax[:], G4[:, :, 3], ALU.max, r=["gs", "gmax"], w=["gmax"])
                    O4 = ohg[:].rearrange("p (t g) -> p t g", g=4)
                    for g in range(4):
                        h.tt("dve", O4[:, :, g], G4[:, :, g], gmax[:], ALU.is_equal, r=["gs", "gmax"], w=["ohg"])
                    T2 = top2[:].rearrange("p (t e) -> p t e", e=4)
                    A3 = A_[:].rearrange("p (t e) -> p t e", e=4)
                    B3 = B_[:].rearrange("p (t e) -> p t e", e=4)
                    for e_ in range(4):
                        oth = [x for x in range(4) if x != e_]
                        h.tt("dve", A3[:, :, e_], X[:, :, oth[0]], X[:, :, e_], ALU.is_gt, r=["r_sel"], w=["r_a"])
                        h.tt("dve", B3[:, :, e_], X[:, :, oth[1]], X[:, :, e_], ALU.is_gt, r=["r_sel"], w=["r_b"])
                        h.tt("dve", A3[:, :, e_], A3[:, :, e_], B3[:, :, e_], ALU.add, r=["r_a", "r_b"], w=["r_a"])
                        h.tt("dve", B3[:, :, e_], X[:, :, oth[2]], X[:, :, e_], ALU.is_gt, r=["r_sel"], w=["r_b"])
                        h.tt("dve", A3[:, :, e_], A3[:, :, e_], B3[:, :, e_], ALU.add, r=["r_a", "r_b"], w=["r_a"])
                        h.ts("dve", T2[:, :, e_], A3[:, :, e_], 1.5, None, ALU.is_lt, r=["r_a"], w=["r_top2"])
                        h.tt("dve", T2[:, :, e_], T2[:, :, e_], ohg[:], ALU.mult, r=["r_top2", "ohg"], w=["r_top2"])
                    h.tt("dve", w_[:], sc[:], top2[:], ALU.mult, r=["r_sc", "r_top2"], w=["r_w"])
                    h.red(wsum[:], w_[:].rearrange("p (t e) -> p t e", e=16), ALU.add, r=["r_w"], w=["wsum"])
                    h.rcp(wsum[:], wsum[:], r=["wsum"], w=["wsum"])
                    for i in range(NT):
                        h.ts("dve", C_[:, i * 16:(i + 1) * 16], w_[:, i * 16:(i + 1) * 16], wsum[:, i:i + 1], None, ALU.mult,
                             r=["r_w", "wsum"], w=["r_c"])
                    for i in range(NT):
                        bk = 4 + (i // 4) % 2
                        h.tr(pb[bk][0:16, (i % 4) * 128:(i % 4 + 1) * 128], C_[:, i * 16:(i + 1) * 16], ident[:],
                             r=["r_c", "ident"], w=[PK(bk)])
                        if i % 4 == 3:
                            h.cp("act", combT[:, (i // 4) * 512:(i // 4 + 1) * 512], pb[bk][0:16, :], r=[PK(bk)], w=["combT"])
                    h.end()
                if layer == 1 and globals().get("_MOE1_SKIP_B", False):
                    return
                wg = [sb("wg%d" % i, [128, DC, 512], BF16, st) for i in range(2)]
                wu = [sb("wu%d" % i, [128, DC, 512], BF16, st) for i in range(2)]
                wd = [sb("wd%d" % i, [128, 4, D], BF16, st) for i in range(2)]
                aT = [sb("aT%d" % i, [128, 4, 512], BF16, st) for i in range(2)]
                sg = [sb("sg%d" % i, [128, 512], F32, st) for i in range(3)]
                cmb = [sb("cmb%d" % i, [128, 512], F32, st) for i in range(2)]
                h.begin()
                n_g = 0
                n_y = 0
                n_it = 0
                mod_items = []
                if layer == 0:
                    wmc = [sb("wmc%d" % i, [128, 1024], BF16, st) for i in range(2)]
                    for (wsrc, nblk, modt_, mkey) in ((w_mod_d[1], 6, mod1, "mod1acc"), (kv_w_mod_d, 2, modkv, "modkvacc")):
                        for blk in range(nblk):
                            for kc in range(DC):
                                mod_items.append((wsrc, blk, kc, modt_, mkey))

                def issue_mod_dma(n):
                    wsrc, blk, kc, modt_, mkey = mod_items[n]
                    wkey = ("wmc", n % 2)
                    h.dma("pool", wmc[n % 2][:], wsrc[kc * 128:(kc + 1) * 128, blk * 1024:(blk + 1) * 1024], w=[wkey], sem=wkey)

                if mod_items:
                    issue_mod_dma(0)

                def run_mod_item(n):
                    wsrc, blk, kc, modt_, mkey = mod_items[n]
                    wt = wmc[n % 2]
                    wkey = ("wmc", n % 2)
                    if n + 1 < len(mod_items):
                        issue_mod_dma(n + 1)
                    for j in range(8):
                        h.mm(pb[0][:, j:j + 1], wt[:, j * 128:(j + 1) * 128], cact[:, kc:kc + 1], r=[wkey, "cact"], w=[PK(0)])
                    h.tt("dve", modt_[:, blk * 8:(blk + 1) * 8], modt_[:, blk * 8:(blk + 1) * 8], pb[0][:, 0:8], ALU.add,
                         r=[PK(0), mkey], w=[mkey])

                def issue_expert(e2):
                    w2 = e2 % 2
                    h.dma("pool", wg[w2][:], wg_d[layer][e2].rearrange("(dc p) f -> p dc f", p=128), w=[("wg", w2)], sem=("wg", w2))
                    h.dma("pool", wu[w2][:], wu_d[layer][e2].rearrange("(dc p) f -> p dc f", p=128), w=[("wu", w2)], sem=("wu", w2))
                    h.dma("pool", wd[w2][:], wd_d[layer][e2].rearrange("(fc p) d -> p fc d", p=128), w=[("wd", w2)], sem=("wd", w2))

                issue_expert(0)
                for e_ in range(NE):
                    wb = e_ % 2
                    if e_ + 1 < NE:
                        issue_expert(e_ + 1)
                    for tb in range(NB):
                        ab = n_it % 2
                        n_it += 1
                        h.mm(pb[0][:, :], selE[:, e_, :], combT[:, tbs(tb)], r=["selE", "combT"], w=[PK(0)])
                        h.cp("act", cmb[ab][:], pb[0][:, :], r=[PK(0)], w=[("cmb", ab)])
                        for fc in range(4):
                            bg = 1 + n_g % 2
                            bu = 3 + n_g % 2
                            sgi = n_g % 3
                            n_g += 1
                            for dc in range(DC):
                                h.mm(pb[bg][:, :], wg[wb][:, dc, fc * 128:(fc + 1) * 128], hT[:, dc, tbs(tb)],
                                     start=(dc == 0), stop=(dc == DC - 1), r=[("wg", wb), ("hT", tb)], w=[PK(bg)])
                            for dc in range(DC):
                                h.mm(pb[bu][:, :], wu[wb][:, dc, fc * 128:(fc + 1) * 128], hT[:, dc, tbs(tb)],
                                     start=(dc == 0), stop=(dc == DC - 1), r=[("wu", wb), ("hT", tb)], w=[PK(bu)])
                            h.act(sg[sgi][:], pb[bg][:, :], AF.Silu, r=[PK(bg)], w=[("sg", sgi)])
                            h.tt("dve", sg[sgi][:], sg[sgi][:], cmb[ab][:], ALU.mult, r=[("sg", sgi), ("cmb", ab)], w=[("sg", sgi)])
                            h.tt("dve", aT[ab][:, fc, :], pb[bu][:, :], sg[sgi][:], ALU.mult, r=[PK(bu), ("sg", sgi)], w=[("aT", ab, fc)])
                            if fc == 1 and n_it - 1 < len(mod_items):
                                run_mod_item(n_it - 1)
                        for dcol in range(DC):
                            by = 5 + n_y % 3
                            n_y += 1
                            for fc in range(4):
                                h.mm(pb[by][:, :], wd[wb][:, fc, dcol * 128:(dcol + 1) * 128], aT[ab][:, fc, :],
                                     start=(fc == 0), stop=(fc == 3), r=[("wd", wb), ("aT", ab, fc)], w=[PK(by)])
                            h.stt("dve", xT[:, dcol, tbs(tb)], pb[by][:, :], modt[:, g2c + dcol:g2c + dcol + 1], xT[:, dcol, tbs(tb)],
                                  ALU.mult, ALU.add, r=[PK(by), "mod", ("xT", dcol, tb)], w=[("xT", dcol, tb)])
                h.end()

        def kv_phase():
            with ExitStack() as st:
                wkd = sb("wkd", [128, DC, 256], BF16, st)
                wkr = sb("wkr", [128, DC, 128], BF16, st)
                craw = sb("kraw", [128, 2, 512], F32, st)
                csq = [sb("ksq%d" % i, [128, 512], BF16, st) for i in range(2)]
                crs = sb("krs", [128, 512], F32, st)
                ra = [sb("kra%d" % i, [128, 512], F32, st) for i in range(2)]
                rb = [sb("krb%d" % i, [128, 512], F32, st) for i in range(2)]
                h.begin()
                h.memset("pool", wkr[:, :, 0:64], 0.0, w=["wkr_z"])
                dn = kv_w_down_d[:, :].rearrange("(dc p) c -> p dc c", p=128)
                h.dma("pool", wkd[:], dn[:, :, 0:256], w=["wkd"], sem="wkd")
                h.dma("pool", wkr[:, :, 64:96], dn[:, :, 256:288], w=["wkr0"], sem="wkr0")
                h.dma("pool", wkr[:, :, 96:112], dn[:, :, 272:288], w=["wkr1"], sem="wkr1")
                h.dma("pool", wkr[:, :, 112:128], dn[:, :, 256:272], w=["wkr2"], sem="wkr2")
                norm_phase(st, 2, modkv, 0, DC)
                for tb in range(NB):
                    for c in range(2):
                        bk = 2 + c
                        for dc in range(DC):
                            h.mm(pb[bk][:, :], wkd[:, dc, c * 128:(c + 1) * 128], hT[:, dc, tbs(tb)], start=(dc == 0), stop=(dc == DC - 1),
                                 r=["wkd", ("hT", tb)], w=[PK(bk)])
                        h.cp("act", craw[:, c, :], pb[bk][:, :], r=[PK(bk)], w=[("kraw", c)])
                        h.tt("dve", csq[c][:], craw[:, c, :], craw[:, c, :], ALU.mult, r=[("kraw", c)], w=[("ksq", c)])
                        h.mm(pb[4][:, :], ones_b[:, :], csq[c][:], start=(c == 0), stop=(c == 1), r=[("ksq", c), "ones_b"], w=[PK(4)])
                    h.act(crs[:], pb[4][:, :], AF.Ln, bias=epst[:, 0:1], scale=1.0 / 256, r=[PK(4), "epst"], w=["krs"])
                    h.act(crs[:], crs[:], AF.Exp, scale=-0.5, r=["krs"], w=["krs"])
                    for c in range(2):
                        h.stt("dve", ckvT[:, c, tbs(tb)], craw[:, c, :], lat_g[:, c:c + 1], crs[:], ALU.mult, ALU.mult,
                              r=[("kraw", c), "krs", "lat_g"], w=["ckvT"])
                    bk = 5 + tb % 2
                    for dc in range(DC):
                        h.mm(pb[bk][:, :], wkr[:, dc, :], hT[:, dc, tbs(tb)], start=(dc == 0), stop=(dc == DC - 1),
                             r=["wkr_z", "wkr0", "wkr1", "wkr2", ("hT", tb)], w=[PK(bk)])
                    cs_ = tbs(tb)
                    a_, b_ = ra[tb % 2], rb[tb % 2]
                    h.tt("dve", a_[64:128, :], pb[bk][64:128, :], csT[64:128, cs_], ALU.mult, r=[PK(bk), "rope_tab", "rope_tab2"], w=[("kra", tb % 2)])
                    h.cp("act", b_[64:96, :], a_[96:128, :], r=[("kra", tb % 2)], w=[("krb", tb % 2)])
                    h.tt("dve", krope[64:96, cs_], a_[64:96, :], b_[64:96, :], ALU.add, r=[("kra", tb % 2), ("krb", tb % 2)], w=[("krope", tb)])
                    h.cp("act", krope[96:128, cs_], krope[64:96, cs_], r=[("krope", tb)], w=[("kropeB", tb)])
                norm_phase(st, 3, mod1, 0, DC)
                wdq = sb("wdq", [128, DC, 768], BF16, st)
                craw = sb("craw", [128, 6, 512], F32, st)
                csq = [sb("csq%d" % i, [128, 512], BF16, st) for i in range(2)]
                crs = sb("crs", [128, 512], F32, st)
                h.dma("pool", wdq[:], b_w_dq_d[:, :].rearrange("(dc p) c -> p dc c", p=128), w=["wdq"], sem="wdq")
                for tb in range(NB):
                    pend = None
                    for c in range(6):
                        bk = 2 + c % 2
                        for dc in range(DC):
                            h.mm(pb[bk][:, :], wdq[:, dc, c * 128:(c + 1) * 128], hT[:, dc, tbs(tb)],
                                 start=(dc == 0), stop=(dc == DC - 1), r=["wdq", ("hT", tb)], w=[PK(bk)])
                        if pend is not None:
                            pend()
                        h.cp("act", craw[:, c, :], pb[bk][:, :], r=[PK(bk)], w=[("craw", c)])
                        q_ = csq[c % 2]
                        h.tt("dve", q_[:], craw[:, c, :], craw[:, c, :], ALU.mult, r=[("craw", c)], w=[("csq", c % 2)])

                        def pend(c=c, q_=q_):
                            h.mm(pb[4][:, :], ones_b[:, :], q_[:], start=(c == 0), stop=(c == 5), r=[("csq", c % 2), "ones_b"], w=[PK(4)])
                    pend()
                    h.act(crs[:], pb[4][:, :], AF.Ln, bias=epst[:, 0:1], scale=1.0 / 768, r=[PK(4), "epst"], w=["crs"])
                    h.act(crs[:], crs[:], AF.Exp, scale=-0.5, r=["crs"], w=["crs"])
                    for c in range(6):
                        h.stt("dve", hT[:, c, tbs(tb)], craw[:, c, :], q_g[:, c:c + 1], crs[:], ALU.mult, ALU.mult,
                              r=[("craw", c), "crs", "q_g"], w=[("hT", tb)])
                h.end()

        def final_phase(do_norm):
            with ExitStack() as st:
                sq = [sb("fsq%d" % i, [128, 512], BF16, st) for i in range(3)]
                rs = [sb("frs%d" % i, [128, 512], F32, st) for i in range(2)]
                ob = [sb("fob%d" % i, [128, 512], F32, st) for i in range(4)]
                h.begin()
                for _i in range(globals().get("_DUMMY", 0)):
                    _de = globals().get("_DUMMY_ENG", "pe")
                    if _de == "pe":
                        h.mm(pb[7][:, 0:16], ones_b[:, :], ones_b[:, 0:16], r=["ones_b"], w=[PK(7)])
                    elif _de == "dve":
                        h.memset("dve", sq[0][:, 0:8], 0.0, w=[])
                    elif _de == "actbig":
                        h.act(hT[:, 0, :], xT[:, 0, :], AF.Copy, r=[], w=[])
                    elif _de == "pooldma":
                        h.dma("pool", sq[2][:, 0:16], a_w_o_d[0:128, 0:16], w=["dummy_dma"], sem="dummy_dma")
                    else:
                        h.act(sq[1][:, 0:8], ones_b[:, 0:8], AF.Copy, r=[], w=[])
                n = 0
                for tb in range(NB):
                    if do_norm:
                        pss = pb[tb % 2]
                        for dc in range(DC):
                            q = sq[n % 3]
                            n += 1
                            if dc % 2 == 0:
                                h.act(q[:], xT[:, dc, tbs(tb)], AF.Square, r=[("xT", dc, tb)], w=[("sq", id(q))])
                            else:
                                h.tt("dve", q[:], xT[:, dc, tbs(tb)], xT[:, dc, tbs(tb)], ALU.mult, r=[("xT", dc, tb)], w=[("sq", id(q))])
                            h.mm(pss[:, :], ones_b[:, :], q[:], start=(dc == 0), stop=(dc == DC - 1), r=[("sq", id(q)), "ones_b"], w=[PK(tb % 2)])
                        r_ = rs[tb % 2]
                        h.act(r_[:], pss[:, :], AF.Ln, bias=epst[:, 0:1], scale=1.0 / D, r=[PK(tb % 2), "epst"], w=[("rs", tb % 2)])
                        h.act(r_[:], r_[:], AF.Exp, scale=-0.5, r=[("rs", tb % 2)], w=[("rs", tb % 2)])
                    for dc in range(DC):
                        o = ob[n % 4]
                        ok = ("ob", n % 4)
                        n += 1
                        if do_norm:
                            h.stt("dve", o[:], xT[:, dc, tbs(tb)], gvec[:, 5 * DC + dc:5 * DC + dc + 1], r_[:], ALU.mult, ALU.mult,
                                  r=[("xT", dc, tb), "gvec", ("rs", tb % 2)], w=[ok])
                        else:
                            h.cp("dve", o[:], xT[:, dc, tbs(tb)], r=[("xT", dc, tb)], w=[ok])
                        h.dma("sp", outT_d[dc * 128:(dc + 1) * 128, tbs(tb)], o[:], r=[ok], w=[("outd", dc, tb)], sem=ok)
                h.end()

        fns = [lambda: attn_phase(0), lambda: moe_phase(0), kv_phase, lambda: attn_phase(1), lambda: moe_phase(1)]
        for i_ in range(5):
            if lo <= i_ <= hi and i_ not in globals().get("_SKIP", []):
                fns[i_]()
        for _i in range(globals().get("_DUMMY_BLOCKS", 0)):
            h.begin()
            h.memset("dve", ncoef[:, 8:9], 0.0, w=["x"])
            h.end()
        final_phase(hi >= 5)
    return nc


_CACHE = {}


def _lay(v, k):
    return np.ascontiguousarray(np.asarray(v, np.float32).reshape(k, 128).T)


def kernel(x, c, positions, a_norm_g, a_w_in, a_b_f, a_w_o, kv_norm_g, kv_w_mod, kv_b_mod,
           kv_w_down, kv_latent_g, kv_w_up, b_norm_g, b_w_dq, b_q_norm_g, b_w_uq, b_w_o,
           w_mod, b_mod, ffn_norm_g, router_w, router_bias, exp_w_gate, exp_w_up, exp_w_down,
           final_norm_g):
    f = lambda a: np.ascontiguousarray(np.asarray(a, np.float32))
    x = f(x)
    B = x.shape[0]
    if "nc" not in _CACHE:
        _CACHE["nc"] = build_program(0, 5)
    gvec = np.concatenate([_lay(a_norm_g[0], 8), _lay(ffn_norm_g[0], 8), _lay(kv_norm_g, 8), _lay(b_norm_g[0], 8),
                           _lay(ffn_norm_g[1], 8), _lay(final_norm_g, 8)], axis=1)
    bmod = np.concatenate([_lay(b_mod[0], 48), _lay(b_mod[1], 48), _lay(kv_b_mod, 16)], axis=1)
    bf_rep = np.ascontiguousarray(np.tile(np.asarray(a_b_f[0], np.float32)[None, :], (128, 16)))
    rb_rep = np.ascontiguousarray(np.tile(np.asarray(router_bias, np.float32)[None, :], (128, 16)))
    half = 16
    inv_freq = (10000.0 ** (-np.arange(half, dtype=np.float32) / half)).astype(np.float32)
    invf = np.zeros((128, 2), np.float32)
    for p in range(128):
        invf[p, 0] = inv_freq[p % 16]
        invf[p, 1] = -1.0 if (p % 32) < 16 else 1.0
    shared = {
        "invf": invf, "gvec": np.ascontiguousarray(gvec), "bmod": np.ascontiguousarray(bmod), "bf_rep": bf_rep, "rb_rep": rb_rep,
        "lat_g": _lay(kv_latent_g, 2), "q_g": _lay(b_q_norm_g[0], 6),
        "w_mod": f(w_mod), "kv_w_mod": f(kv_w_mod), "a_w_in": f(a_w_in[0]), "a_w_o": f(a_w_o[0]),
        "kv_w_down": f(kv_w_down), "kv_w_up": f(kv_w_up), "b_w_dq": f(b_w_dq[0]), "b_w_uq": f(b_w_uq[0]),
        "b_w_o": f(b_w_o[0]), "router_w": f(router_w),
        "exp_w_gate0": f(exp_w_gate[0]), "exp_w_gate1": f(exp_w_gate[1]), "exp_w_up0": f(exp_w_up[0]), "exp_w_up1": f(exp_w_up[1]),
        "exp_w_down0": f(exp_w_down[0]), "exp_w_down1": f(exp_w_down[1]),
    }
    in_maps = []
    for b in range(B):
        m = dict(shared)
        m["xT"] = np.ascontiguousarray(x[b].T)
        m["c_l"] = _lay(c[b], 8)
        m["pos"] = np.ascontiguousarray(np.asarray(positions[b], np.int32)[None, :])
        in_maps.append(m)
    res = run_bass_kernel_spmd(_CACHE["nc"], in_maps, core_ids=list(range(B)))
    out = np.stack([np.asarray(r["outT"], np.float32).T for r in res.results], axis=0)
    return np.ascontiguousarray(out)
```
